# Optimizing a Trainium2 kernel written in Bass

```python
import jax
import jax.numpy as jnp
from jax import lax
import numpy as np

D_MODEL = 2048
BATCH = 2
SEQ = 8192
DEPTH = 2

GRID_W = 64
CTX_LEN = 256
N_MOD = 6
CONV_CH = 1024
CONV_K = 31
NA_HEADS = 8
HEAD_DIM = 128
NA_DIM = NA_HEADS * HEAD_DIM
MIX_DIM = CONV_CH + NA_DIM
PROJ_DIM = 2 * CONV_CH + 3 * NA_DIM
Q_OFF = 2 * CONV_CH
K_OFF = Q_OFF + NA_DIM
V_OFF = K_OFF + NA_DIM
NA_KR = 8
NA_KC = 16
Q_COLS = 16
BAND_C = 32
ROPE_BASE = 10000.0
N_EXPERTS = 32
TOP_K = 4
D_EXPERT = 2048
SWIGLU_ALPHA = 1.702
SWIGLU_LIMIT = 7.0
MOE_BLOCK = 128
EPS = 1e-6
NEG_INF = -1e30

kernel_name = 'hybrid_conv_natten_moe_dit'


def rms_norm(x, g):
    xf = x.astype(jnp.float32)
    y = xf * lax.rsqrt(jnp.mean(xf * xf, axis=-1, keepdims=True) + EPS)
    return (y * g.astype(jnp.float32)).astype(x.dtype)


def layer_norm(x, g, b):
    xf = x.astype(jnp.float32)
    mu = jnp.mean(xf, axis=-1, keepdims=True)
    var = jnp.mean(jnp.square(xf - mu), axis=-1, keepdims=True)
    y = (xf - mu) * lax.rsqrt(var + EPS) * g.astype(jnp.float32) + b.astype(jnp.float32)
    return y.astype(x.dtype)


def modulate(x, g, shift, scale):
    return rms_norm(x, g) * (1 + scale) + shift


def to_heads(t):
    B, L, _ = t.shape
    return t.reshape(B, L, NA_HEADS, HEAD_DIM).transpose(0, 2, 1, 3)


def from_heads(o):
    B, H, L, dh = o.shape
    return o.transpose(0, 2, 1, 3).reshape(B, L, H * dh)


def rope_2d(n_tokens):
    t = jnp.arange(n_tokens, dtype=jnp.int32)
    row = (t // GRID_W).astype(jnp.float32)
    col = (t % GRID_W).astype(jnp.float32)
    n_freq = HEAD_DIM // 4
    inv = ROPE_BASE ** (-jnp.arange(n_freq, dtype=jnp.float32) / n_freq)
    ang = jnp.concatenate([row[:, None] * inv, col[:, None] * inv], axis=-1)
    return jnp.cos(ang), jnp.sin(ang)


def apply_rope_2d(x, cos, sin):
    xf = x.astype(jnp.float32)
    n_freq = HEAD_DIM // 4
    half = HEAD_DIM // 2

    def rot(xa, c, s):
        x1, x2 = xa[..., :n_freq], xa[..., n_freq:]
        return jnp.concatenate([x1 * c - x2 * s, x2 * c + x1 * s], axis=-1)

    out = jnp.concatenate([rot(xf[..., :half], cos[:, :n_freq], sin[:, :n_freq]),
                           rot(xf[..., half:], cos[:, n_freq:], sin[:, n_freq:])], axis=-1)
    return out.astype(x.dtype)


def conv_module(u, w_dw, b_dw, ln_g, ln_b):
    a, gate = jnp.split(u, 2, axis=-1)
    v = a * jax.nn.sigmoid(gate)
    v = lax.conv_general_dilated(v, w_dw[:, None, :], window_strides=(1,),
                                 padding=((CONV_K // 2, CONV_K // 2),),
                                 dimension_numbers=('NWC', 'WIO', 'NWC'),
                                 feature_group_count=CONV_CH) + b_dw
    return jax.nn.silu(layer_norm(v, ln_g, ln_b))


def dense_attention(q, k, v):
    s = jnp.einsum('bhqd,bhkd->bhqk', q, k).astype(jnp.float32) * (HEAD_DIM ** -0.5)
    p = jax.nn.softmax(s, axis=-1).astype(v.dtype)
    return jnp.einsum('bhqk,bhkd->bhqd', p, v)


def neighbourhood_attention(q, k, v, k_ctx, v_ctx, rpb):
    B, H, L, dh = q.shape
    rows = L // GRID_W
    kr = min(NA_KR, rows)
    n_cb = GRID_W // Q_COLS
    scale = dh ** -0.5
    q_col = np.arange(GRID_W).reshape(n_cb, Q_COLS)
    win_start = np.clip(q_col - NA_KC // 2, 0, GRID_W - NA_KC)
    band_start = np.clip(np.arange(n_cb) * Q_COLS - NA_KC // 2, 0, GRID_W - BAND_C)
    k_col = band_start[:, None] + np.arange(BAND_C)
    ws = win_start[:, :, None]
    kc3 = k_col[:, None, :]
    in_win = (kc3 >= ws) & (kc3 < ws + NA_KC)
    mask = jnp.asarray(np.broadcast_to(in_win[:, :, None, :], (n_cb, Q_COLS, kr, BAND_C))
                       .reshape(n_cb, Q_COLS, kr * BAND_C))
    dc_idx = jnp.asarray(np.clip(kc3 - q_col[:, :, None], -(NA_KC - 1), NA_KC - 1) + NA_KC - 1)[:, :, None, :]
    k_rows = k.reshape(B, H, rows, GRID_W, dh)
    v_rows = v.reshape(B, H, rows, GRID_W, dh)
    q_rows = jnp.moveaxis(q.reshape(B, H, rows, n_cb, Q_COLS, dh), 2, 0)
    n_loc = kr * BAND_C

    def one_row(args):
        r, q_r = args
        rs = jnp.clip(r - kr // 2, 0, rows - kr)

        def band(t):
            t = lax.dynamic_slice_in_dim(t, rs, kr, axis=2)[:, :, :, k_col, :]
            return jnp.moveaxis(t, 3, 2).reshape(B, H, n_cb, n_loc, dh)

        kb, vb = band(k_rows), band(v_rows)
        dr_idx = (rs + jnp.arange(kr) - r + NA_KR - 1)[None, None, :, None]
        bias = rpb[:, dr_idx, dc_idx].reshape(H, n_cb, Q_COLS, n_loc)
        s_loc = jnp.einsum('bhnqd,bhnkd->bhnqk', q_r, kb).astype(jnp.float32) * scale + bias
        s_loc = jnp.where(mask, s_loc, NEG_INF)
        s_ctx = jnp.einsum('bhnqd,bhkd->bhnqk', q_r, k_ctx).astype(jnp.float32) * scale
        p = jax.nn.softmax(jnp.concatenate([s_loc, s_ctx], axis=-1), axis=-1).astype(v.dtype)
        return (jnp.einsum('bhnqk,bhnkd->bhnqd', p[..., :n_loc], vb)
                + jnp.einsum('bhnqk,bhkd->bhnqd', p[..., n_loc:], v_ctx))

    out = lax.map(one_row, (jnp.arange(rows, dtype=jnp.int32), q_rows))
    return jnp.moveaxis(out, 0, 2).reshape(B, H, L, dh)


def clamped_swiglu(gu):
    x_glu, x_lin = jnp.split(gu, 2, axis=-1)
    x_glu = jnp.minimum(x_glu, SWIGLU_LIMIT)
    x_lin = jnp.clip(x_lin, -SWIGLU_LIMIT, SWIGLU_LIMIT)
    return x_glu * jax.nn.sigmoid(SWIGLU_ALPHA * x_glu) * (x_lin + 1)


def moe(h, w_router, b_router, w_gu, b_gu, w_dn, b_dn):
    N = h.shape[0]
    logits = (h @ w_router + b_router).astype(jnp.float32)
    top_v, top_i = lax.top_k(logits, TOP_K)
    gates = jax.nn.softmax(top_v, axis=-1)
    A = N * TOP_K
    flat_e = top_i.reshape(-1)
    flat_t = jnp.repeat(jnp.arange(N, dtype=jnp.int32), TOP_K)
    flat_w = gates.reshape(-1)
    order = jnp.argsort(flat_e)
    e_sorted = flat_e[order]
    counts = jnp.bincount(flat_e, length=N_EXPERTS)
    padded = (counts + MOE_BLOCK - 1) // MOE_BLOCK * MOE_BLOCK
    start = jnp.cumsum(counts) - counts
    p_end = jnp.cumsum(padded)
    p_start = p_end - padded
    dest = p_start[e_sorted] + jnp.arange(A, dtype=jnp.int32) - start[e_sorted]
    n_blocks = -(-(A + N_EXPERTS * (MOE_BLOCK - 1)) // MOE_BLOCK)
    P = n_blocks * MOE_BLOCK
    buf_t = jnp.zeros((P,), jnp.int32).at[dest].set(flat_t[order])
    buf_w = jnp.zeros((P,), h.dtype).at[dest].set(flat_w[order].astype(h.dtype))
    blk_e = jnp.clip(jnp.searchsorted(p_end, jnp.arange(n_blocks, dtype=jnp.int32) * MOE_BLOCK, side='right'),
                     0, N_EXPERTS - 1)

    def expert_block(args):
        tok, w, e = args
        xb = h[tok]
        y = clamped_swiglu(xb @ w_gu[e] + b_gu[e]) @ w_dn[e] + b_dn[e]
        return y * w[:, None]

    y = lax.map(expert_block, (buf_t.reshape(n_blocks, MOE_BLOCK), buf_w.reshape(n_blocks, MOE_BLOCK), blk_e))
    return jnp.zeros_like(h).at[buf_t].add(y.reshape(P, -1))


def setup_inputs(seed: int = 0) -> dict:
    key = jax.random.key(seed)
    ks = jax.random.split(key, 24)

    def nrm(k, shape, scale):
        return jax.random.normal(k, shape, jnp.float32) * scale

    return {
        'x': nrm(ks[0], (BATCH, SEQ, D_MODEL), 1.0),
        'c': nrm(ks[1], (BATCH, D_MODEL), 1.0),
        'ctx': nrm(ks[2], (BATCH, CTX_LEN, D_MODEL), 1.0),
        'c_ctx': nrm(ks[3], (D_MODEL,), 1.0),
        'w_ada': nrm(ks[4], (DEPTH, D_MODEL, N_MOD * D_MODEL), 0.5 * D_MODEL ** -0.5),
        'b_ada': nrm(ks[5], (DEPTH, N_MOD * D_MODEL), 0.02),
        'g_mix': 1.0 + nrm(ks[6], (DEPTH, D_MODEL), 0.02),
        'g_ffn': 1.0 + nrm(ks[7], (DEPTH, D_MODEL), 0.02),
        'w_in': nrm(ks[8], (DEPTH, D_MODEL, PROJ_DIM), D_MODEL ** -0.5),
        'w_dw': nrm(ks[9], (DEPTH, CONV_K, CONV_CH), CONV_K ** -0.5),
        'b_dw': nrm(ks[10], (DEPTH, CONV_CH), 0.02),
        'ln_g': 1.0 + nrm(ks[11], (DEPTH, CONV_CH), 0.02),
        'ln_b': nrm(ks[12], (DEPTH, CONV_CH), 0.02),
        'g_q': 1.0 + nrm(ks[13], (DEPTH, HEAD_DIM), 0.02),
        'g_k': 1.0 + nrm(ks[14], (DEPTH, HEAD_DIM), 0.02),
        'rpb': nrm(ks[15], (DEPTH, NA_HEADS, 2 * NA_KR - 1, 2 * NA_KC - 1), 0.1),
        'w_out': nrm(ks[16], (DEPTH, MIX_DIM, D_MODEL), MIX_DIM ** -0.5),
        'w_router': nrm(ks[17], (DEPTH, D_MODEL, N_EXPERTS), D_MODEL ** -0.5),
        'b_router': nrm(ks[18], (DEPTH, N_EXPERTS), 0.01),
        'w_gate_up': nrm(ks[19], (DEPTH, N_EXPERTS, D_MODEL, 2 * D_EXPERT), D_MODEL ** -0.5),
        'b_gate_up': nrm(ks[20], (DEPTH, N_EXPERTS, 2 * D_EXPERT), 0.01),
        'w_down': nrm(ks[21], (DEPTH, N_EXPERTS, D_EXPERT, D_MODEL), D_EXPERT ** -0.5),
        'b_down': nrm(ks[22], (DEPTH, N_EXPERTS, D_MODEL), 0.01),
    }


def reference(x, c, ctx, c_ctx, w_ada, b_ada, g_mix, g_ffn, w_in, w_dw, b_dw, ln_g, ln_b, g_q, g_k, rpb,
              w_out, w_router, b_router, w_gate_up, b_gate_up, w_down, b_down):
    L = x.shape[1]
    cos, sin = rope_2d(L)
    s_c = jax.nn.silu(c)
    s_cc = jax.nn.silu(c_ctx)
    for l in range(DEPTH):
        last = l == DEPTH - 1
        sh_m, sc_m, gt_m, sh_f, sc_f, gt_f = jnp.split((s_c @ w_ada[l] + b_ada[l])[:, None, :], N_MOD, axis=-1)
        csh_m, csc_m, cgt_m, csh_f, csc_f, cgt_f = jnp.split(s_cc @ w_ada[l] + b_ada[l], N_MOD, axis=-1)

        h = modulate(x, g_mix[l], sh_m, sc_m)
        hc = modulate(ctx, g_mix[l], csh_m, csc_m)
        p = h @ w_in[l]
        q = apply_rope_2d(rms_norm(to_heads(p[..., Q_OFF:K_OFF]), g_q[l]), cos, sin)
        k = apply_rope_2d(rms_norm(to_heads(p[..., K_OFF:V_OFF]), g_k[l]), cos, sin)
        v = to_heads(p[..., V_OFF:])
        if last:
            pc = hc @ w_in[l][:, K_OFF:]
            kc = rms_norm(to_heads(pc[..., :NA_DIM]), g_k[l])
            vc = to_heads(pc[..., NA_DIM:])
        else:
            pc = hc @ w_in[l]
            qc = rms_norm(to_heads(pc[..., Q_OFF:K_OFF]), g_q[l])
            kc = rms_norm(to_heads(pc[..., K_OFF:V_OFF]), g_k[l])
            vc = to_heads(pc[..., V_OFF:])
        o_lat = jnp.concatenate([conv_module(p[..., :Q_OFF], w_dw[l], b_dw[l], ln_g[l], ln_b[l]),
                                 from_heads(neighbourhood_attention(q, k, v, kc, vc, rpb[l]))], axis=-1)
        if not last:
            o_ctx = jnp.concatenate([conv_module(pc[..., :Q_OFF], w_dw[l], b_dw[l], ln_g[l], ln_b[l]),
                                     from_heads(dense_attention(qc, kc, vc))], axis=-1)
            ctx = ctx + cgt_m * (o_ctx @ w_out[l])
        x = x + gt_m * (o_lat @ w_out[l])

        h = modulate(x, g_ffn[l], sh_f, sc_f).reshape(-1, D_MODEL)
        if last:
            y = moe(h, w_router[l], b_router[l], w_gate_up[l], b_gate_up[l], w_down[l], b_down[l])
            x = x + gt_f * y.reshape(x.shape)
        else:
            hc = modulate(ctx, g_ffn[l], csh_f, csc_f).reshape(-1, D_MODEL)
            n_lat = h.shape[0]
            y = moe(jnp.concatenate([h, hc], axis=0), w_router[l], b_router[l], w_gate_up[l], b_gate_up[l],
                    w_down[l], b_down[l])
            x = x + gt_f * y[:n_lat].reshape(x.shape)
            ctx = ctx + cgt_f * y[n_lat:].reshape(ctx.shape)
    return x
```

```python
import ml_dtypes
from concourse.bass_utils import run_bass_kernel_spmd
import numpy as np
from contextlib import ExitStack
import concourse.bass as bass
import concourse.mybir as mybir

F32 = mybir.dt.float32
BF16 = mybir.dt.bfloat16
I32 = mybir.dt.int32
ALU = mybir.AluOpType
AF = mybir.ActivationFunctionType
AX = mybir.AxisListType


class KB:
    SEM_ROLL = 20000
    NDMA = 8

    def __init__(self, nc):
        self.nc = nc
        self.es = ExitStack()
        self.eng = {"pe": nc.tensor, "dve": nc.vector, "act": nc.scalar, "pool": nc.gpsimd, "sp": nc.sync}
        self.cur = {}
        self.cnt = {}
        self.nsem = 0
        for e in ("pe", "dve", "act", "pool"):
            self._roll(e)
        self.dsem = {}
        self.dcnt = {}
        self.dnext = {}
        for q in ("sp", "pool", "act"):
            self.dsem[q] = [self._newsem() for _ in range(self.NDMA)]
            self.dcnt[q] = [0] * self.NDMA
            self.dnext[q] = 0
        self.seen = {e: {} for e in self.eng}
        self.state = {}
        self.nt = 0
        self.out_deps = []

    def _newsem(self):
        self.nsem += 1
        return self.es.enter_context(self.nc.semaphore("s%d" % self.nsem))

    def _roll(self, e):
        self.cur[e] = self._newsem()
        self.cnt[e] = 0

    def sb(self, shape, dt, name=None):
        self.nt += 1
        return self.es.enter_context(self.nc.sbuf_tensor(name or ("t%d" % self.nt), list(shape), dt))

    def ps(self, shape, dt, name=None):
        self.nt += 1
        return self.es.enter_context(self.nc.psum_tensor(name or ("p%d" % self.nt), list(shape), dt))

    def dram(self, name, shape, dt, kind="Internal"):
        return self.nc.dram_tensor(name, list(shape), dt, kind=kind).ap()

    def _wait(self, e, deps):
        best = {}
        for d in deps:
            if d is None:
                continue
            sem, val = d
            k = id(sem)
            if k not in best or best[k][1] < val:
                best[k] = (sem, val)
        for k, (sem, val) in best.items():
            if self.seen[e].get(k, 0) >= val:
                continue
            self.eng[e].wait_ge(sem, val)
            self.seen[e][k] = val

    def _deps(self, reads, writes):
        deps = []
        for r in reads:
            st = self.state.get(r)
            if st:
                deps.append(st[0])
        for w in writes:
            st = self.state.get(w)
            if st:
                deps.append(st[0])
                deps.extend(st[1])
        return deps

    def _commit(self, my, reads, writes):
        for r in reads:
            st = self.state.setdefault(r, [None, []])
            st[1].append(my)
            if len(st[1]) > 64:
                st[1] = st[1][-64:]
        for w in writes:
            self.state[w] = [my, []]

    def op(self, e, fn, reads=(), writes=()):
        self._wait(e, self._deps(reads, writes))
        ins = fn(self.eng[e])
        if self.cnt[e] >= self.SEM_ROLL:
            self._roll(e)
        self.cnt[e] += 1
        ins.then_inc(self.cur[e], 1)
        my = (self.cur[e], self.cnt[e])
        self._commit(my, reads, writes)
        return my

    def dma(self, q, out, in_, reads=(), writes=(), is_output=False, **kw):
        i = self.dnext[q]
        self.dnext[q] = (i + 1) % self.NDMA
        sem = self.dsem[q][i]
        deps = self._deps(reads, writes)
        if self.dcnt[q][i] > 0:
            deps.append((sem, 16 * self.dcnt[q][i]))
        self._wait(q, deps)
        ins = self.eng[q].dma_start(out=out, in_=in_, **kw)
        self.dcnt[q][i] += 1
        ins.then_inc(sem, 16)
        my = (sem, 16 * self.dcnt[q][i])
        self._commit(my, reads, writes)
        if is_output:
            self.out_deps.append(my)
        return my

    def finish(self):
        self.seen["sp"] = {}
        self._wait("sp", self.out_deps)
        last = [(self.cur[e], self.cnt[e]) for e in ("pe", "dve", "act", "pool") if self.cnt[e] > 0]
        self._wait("sp", last)

    def close(self):
        self.es.close()


def _kb_barrier(self):
    deps = [(self.cur[e], self.cnt[e]) for e in ("pe", "dve", "act", "pool") if self.cnt[e] > 0]
    for q in self.dsem:
        for i, sem in enumerate(self.dsem[q]):
            if self.dcnt[q][i] > 0:
                deps.append((sem, 16 * self.dcnt[q][i]))
    for e in self.eng:
        self._wait(e, deps)
    self.state.clear()


KB.barrier = _kb_barrier


class Phase:
    def __init__(self, k):
        self.k = k
        self.es = ExitStack()

    def sb(self, shape, dt):
        self.k.nt += 1
        return self.es.enter_context(self.k.nc.sbuf_tensor("t%d" % self.k.nt, list(shape), dt))

    def ps(self, shape, dt):
        self.k.nt += 1
        return self.es.enter_context(self.k.nc.psum_tensor("p%d" % self.k.nt, list(shape), dt))

    def end(self):
        self.k.barrier()
        self.es.close()


NCOL = 1536

def build_A():
    nc = bass.Bass("TRN2", target_bir_lowering=False)
    sT = nc.dram_tensor("sT", [D, 3], F32, kind="ExternalInput").ap()
    wa = nc.dram_tensor("wa", [2, D, NCOL], F32, kind="ExternalInput").ap()
    ba = nc.dram_tensor("ba", [2, NCOL], F32, kind="ExternalInput").ap()
    mod = nc.dram_tensor("mod", [2, 3, NCOL], F32, kind="ExternalOutput").ap()
    k = KB(nc)
    s_raw = k.sb([128, 16, 3], F32)
    s_act = k.sb([128, 16, 3], F32)
    wt = [k.sb([128, 16, 512], F32) for _ in range(2)]
    bt = [k.sb([3, 512], F32) for _ in range(2)]
    ot = [k.sb([3, 512], F32) for _ in range(2)]
    pt = [k.ps([3, 512], F32) for _ in range(2)]
    k.dma("sp", s_raw[:, :, :], sT.rearrange("(kc p) b -> p kc b", p=128), writes=[("s_raw", 0)])
    k.op("act", lambda e: e.activation(out=s_act[:, :, :], in_=s_raw[:, :, :], func=AF.Silu),
         reads=[("s_raw", 0)], writes=[("s_act", 0)])
    g = 0
    for l in range(2):
        for cb in range(3):
            b = g % 2
            k.dma("sp", wt[b][:, :, :], wa[l, :, cb * 512:(cb + 1) * 512].rearrange("(kc p) c -> p kc c", p=128),
                  writes=[("wt", b)])
            k.dma("sp", bt[b][:, :], ba[l:l + 1, cb * 512:(cb + 1) * 512].broadcast_to([3, 512]), writes=[("bt", b)])
            def mm(e, b=b):
                for kc in range(16):
                    ins = e.matmul(pt[b][:, :], s_act[:, kc, :], wt[b][:, kc, :], start=(kc == 0), stop=(kc == 15))
                return ins
            k.op("pe", mm, reads=[("s_act", 0), ("wt", b)], writes=[("pt", b)])
            k.op("dve", lambda e, b=b: e.tensor_tensor(out=ot[b][:, :], in0=pt[b][:, :], in1=bt[b][:, :], op=ALU.add),
                 reads=[("pt", b), ("bt", b)], writes=[("ot", b)])
            k.dma("sp", mod[l, :, cb * 512:(cb + 1) * 512], ot[b][:, :], reads=[("ot", b)], is_output=True)
            g += 1
    k.finish()
    k.close()
    return nc


D = 2048
NE = 2560
NO = 2048
OWN0 = 256
NCX = 256
NT = NE + NCX
NQ = NO + NCX
EPS = 1e-6
SCALE = 128 ** -0.5
NEGB = -30000.0
CT0 = NE + 15
VGW = NE + 15 + NCX + 15


def build_B(last):
    nc = bass.Bass("TRN2", target_bir_lowering=False)

    def din(name, shape, dt=F32):
        return nc.dram_tensor(name, list(shape), dt, kind="ExternalInput").ap()

    def dout(name, shape, dt=F32):
        return nc.dram_tensor(name, list(shape), dt, kind="ExternalOutput").ap()

    xe = din("xe", [NE, D])
    cx = din("cx", [NCX, D])
    modv = din("modv", [2, 6 * D])
    gmf = din("gmf", [2, D])
    win = din("win", [40, 128, 16 * 128])
    wout = din("wout", [128, 16 * D])
    chp = din("chp", [128, 8 * 34])
    gqk = din("gqk", [128, 2])
    cs = din("cs", [2, 128, NE])
    rmt = din("rmt", [128, 128])
    idn = din("idn", [128, 128])
    vmask = din("vmask", [1, NE])
    btab = din("btab", [8, 128, 5 * 896])
    wr = din("wr", [128, 16 * 32])
    br = din("br", [1, 32])
    x_mid = dout("x_mid", [NO, D])
    G_o = dout("G_o", [NO, 32])
    h2_o = dout("h2_o", [NO, D], BF16)
    if not last:
        c_mid = dout("c_mid", [NCX, D])
        Gc_o = dout("Gc_o", [NCX, 32])
        h2c_o = dout("h2c_o", [NCX, D], BF16)

    k = KB(nc)
    conv_d = k.dram("conv_d", [8, 128, NQ], F32)
    oT_d = k.dram("oT_d", [16, 128, NQ], BF16)

    ident_f = k.sb([128, 128], F32)
    ident_b = k.sb([128, 128], BF16)
    ones_f = k.sb([128, 128], F32)
    ones_b = k.sb([128, 128], BF16)
    rmt_s = k.sb([128, 128], F32)
    chp_s = k.sb([128, 8, 34], F32)
    gqk_s = k.sb([128, 2], F32)
    big = k.sb([128, 16 * NT], BF16)
    hT = big[:, :].rearrange("p (kc t) -> p kc t", kc=16)
    k.dma("sp", ident_f[:, :], idn, writes=[("c", 0)])
    k.dma("sp", rmt_s[:, :], rmt, writes=[("c", 1)])
    k.dma("sp", chp_s[:, :, :], chp.rearrange("p (c j) -> p c j", j=34), writes=[("c", 2)])
    k.dma("sp", gqk_s[:, :], gqk, writes=[("c", 3)])
    k.op("dve", lambda e: e.tensor_copy(out=ident_b[:, :], in_=ident_f[:, :]), reads=[("c", 0)], writes=[("c", 4)])
    k.op("dve", lambda e: e.memset(ones_f[:, :], 1.0), writes=[("c", 5)])
    k.op("dve", lambda e: e.memset(ones_b[:, :], 1.0), writes=[("c", 6)])
    eps_s = k.sb([128, 1], F32)
    k.op("dve", lambda e: e.memset(eps_s[:, :], EPS), writes=[("c", 7)])
    CONST = [("c", i) for i in range(8)]

    def rstd_ops(e_sum_ap, out_ap, n, deps_r, deps_w):
        k.op("act", lambda e: e.activation(out=out_ap, in_=e_sum_ap, func=AF.Sqrt, bias=eps_s[:, 0:1], scale=1.0 / n),
             reads=list(deps_r) + CONST, writes=deps_w)
        k.op("dve", lambda e: e.reciprocal(out=out_ap, in_=out_ap), reads=deps_w, writes=deps_w)

    ph = Phase(k)
    rows = [ph.sb([128, D], F32) for _ in range(5)]
    xt = [ph.sb([128, D], F32) for _ in range(2)]
    sq = ph.sb([128, D], F32)
    tmpf = ph.sb([128, D], F32)
    hb = [ph.sb([128, D], BF16) for _ in range(2)]
    ss = [ph.sb([128, 2], F32) for _ in range(2)]
    ptr = [ph.ps([128, 1024], F32) for _ in range(2)]

    def bc(ap_row):
        return ap_row.broadcast_to([128, D])

    k.dma("sp", rows[4][:, :], bc(gmf[0:1, :]), writes=[("row", 4)])
    for j, r in enumerate((0, 1)):
        k.dma("sp", rows[2 * j][:, :], bc(modv[r:r + 1, D:2 * D]), writes=[("row", 2 * j)])
        k.dma("sp", rows[2 * j + 1][:, :], bc(modv[r:r + 1, 0:D]), writes=[("row", 2 * j + 1)])
        k.op("dve", lambda e, j=j: e.scalar_tensor_tensor(out=rows[2 * j][:, :], in0=rows[2 * j][:, :], scalar=1.0,
                                                          in1=rows[4][:, :], op0=ALU.add, op1=ALU.mult),
             reads=[("row", 4)], writes=[("row", 2 * j)])

    tiles0 = [(xe, t, t * 128, 0) for t in range(NE // 128)] + [(cx, t, NE + t * 128, 1) for t in range(NCX // 128)]

    def load_x(i):
        src, t, col, which = tiles0[i]
        k.dma("sp", xt[i % 2][:, :], src[t * 128:(t + 1) * 128, :], writes=[("xt", i % 2)])

    load_x(0)
    for i, (src, t, col, which) in enumerate(tiles0):
        b = i % 2
        if i + 1 < len(tiles0):
            load_x(i + 1)
        k.op("act", lambda e, b=b: e.activation(out=sq[:, :], in_=xt[b][:, :], func=AF.Square),
             reads=[("xt", b)], writes=[("sq", 0)])
        k.op("dve", lambda e, b=b: e.tensor_reduce(out=ss[b][:, 0:1], in_=sq[:, :], axis=AX.X, op=ALU.add),
             reads=[("sq", 0)], writes=[("ss", b)])
        rstd_ops(ss[b][:, 0:1], ss[b][:, 1:2], D, [("ss", b)], [("ss", b)])
        A, Bv = rows[2 * which], rows[2 * which + 1]
        k.op("dve", lambda e, b=b, A=A: e.scalar_tensor_tensor(out=tmpf[:, :], in0=xt[b][:, :], scalar=ss[b][:, 1:2],
                                                               in1=A[:, :], op0=ALU.mult, op1=ALU.mult),
             reads=[("xt", b), ("ss", b), ("row", 2 * which)], writes=[("tmpf", 0)])
        k.op("pool", lambda e, b=b, Bv=Bv: e.tensor_tensor(out=hb[b][:, :], in0=tmpf[:, :], in1=Bv[:, :], op=ALU.add),
             reads=[("tmpf", 0), ("row", 2 * which + 1)], writes=[("hb", b)])
        pv = ptr[b][:, :].bitcast(BF16)

        def tr(e, b=b, pv=pv):
            for kc in range(16):
                ins = e.transpose(pv[:, kc * 128:(kc + 1) * 128], hb[b][:, kc * 128:(kc + 1) * 128], ident_b[:, :])
            return ins
        k.op("pe", tr, reads=[("hb", b)] + CONST, writes=[("ptr", b)])
        k.op("act", lambda e, pv=pv, col=col: e.copy(out=hT[:, :, col:col + 128],
                                                     in_=pv.rearrange("p (kc t) -> p kc t", kc=16)),
             reads=[("ptr", b)], writes=[("hT", col // 128)])
    ph.end()
    HT_ALL = [("hT", i) for i in range(NT // 128)]

    def load_w(wb, chunk, slot):
        k.dma("pool", wb[slot][:, :, :], win[chunk].rearrange("p (kc n) -> p kc n", kc=16), writes=[("wb", slot)])

    def mm_fm(ps_ap, wb, slot, c0, n, ps_key):
        def f(e):
            for kc in range(16):
                ins = e.matmul(ps_ap, wb[slot][:, kc, :], hT[:, kc, c0:c0 + n], start=(kc == 0), stop=(kc == 15))
            return ins
        k.op("pe", f, reads=[("wb", slot)] + [("hT", i) for i in range(c0 // 128, (c0 + n) // 128)], writes=[ps_key])

    EXT_BLOCKS = [(i * 512, 512) for i in range(5)]
    OWN_BLOCKS = [(OWN0 + i * 512, 512) for i in range(4)]
    CTX_BLOCK = [(NE, NCX)]

    ph = Phase(k)
    wb = [ph.sb([128, 16, 128], BF16) for _ in range(4)]
    vg = [ph.sb([128, VGW], F32) for _ in range(2)]
    acc = [ph.sb([128, NQ], F32) for _ in range(2)]
    sig = [ph.sb([128, 512], F32) for _ in range(2)]
    vm = ph.sb([128, NE], F32)
    pa = [ph.ps([128, 512], F32) for _ in range(2)]
    pg = [ph.ps([128, 512], F32) for _ in range(2)]
    k.dma("sp", vm[:, :], vmask.broadcast_to([128, NE]), writes=[("vm", 0)])
    for b in range(2):
        k.op("pool", lambda e, b=b: e.memset(vg[b][:, NE:VGW], 0.0), writes=[("vg", b)])
    blocks = EXT_BLOCKS + ([] if last else CTX_BLOCK)
    load_w(wb, 0, 0)
    load_w(wb, 8, 1)
    it = 0
    for cc in range(8):
        sa, sg = (2 * cc) % 4, (2 * cc + 1) % 4
        if cc + 1 < 8:
            load_w(wb, cc + 1, (2 * cc + 2) % 4)
            load_w(wb, 8 + cc + 1, (2 * cc + 3) % 4)
        vb = cc % 2
        for (c0, n) in blocks:
            pb = it % 2
            it += 1
            mm_fm(pa[pb][:, :n], wb, sa, c0, n, ("pa", pb))
            mm_fm(pg[pb][:, :n], wb, sg, c0, n, ("pg", pb))
            k.op("act", lambda e, pb=pb, n=n: e.activation(out=sig[pb][:, :n], in_=pg[pb][:, :n], func=AF.Sigmoid),
                 reads=[("pg", pb)], writes=[("sig", pb)])
            if c0 < NE:
                k.op("pool", lambda e, pb=pb, n=n, c0=c0: e.tensor_tensor(out=sig[pb][:, :n], in0=sig[pb][:, :n],
                                                                          in1=vm[:, c0:c0 + n], op=ALU.mult),
                     reads=[("vm", 0)], writes=[("sig", pb)])
                dst = vg[vb][:, c0:c0 + n]
            else:
                dst = vg[vb][:, CT0:CT0 + n]
            k.op("dve", lambda e, pb=pb, n=n, dst=dst: e.tensor_tensor(out=dst, in0=pa[pb][:, :n], in1=sig[pb][:, :n],
                                                                       op=ALU.mult),
                 reads=[("pa", pb), ("sig", pb)], writes=[("vg", vb)])
        ce = "dve"
        segs = [(0, NO, OWN0)] + ([] if last else [(NO, NCX, CT0)])
        for j in range(31):
            for (o0, n, v0) in segs:
                src = vg[vb][:, v0 + j - 15:v0 + j - 15 + n]
                if j == 0:
                    k.op(ce, lambda e, src=src, o0=o0, n=n, cc=cc, vb=vb: e.tensor_scalar(
                        out=acc[vb][:, o0:o0 + n], in0=src, scalar1=chp_s[:, cc, 0:1], scalar2=chp_s[:, cc, 31:32],
                        op0=ALU.mult, op1=ALU.add), reads=[("vg", vb)] + CONST, writes=[("acc", vb, o0)])
                else:
                    k.op(ce, lambda e, src=src, o0=o0, n=n, cc=cc, vb=vb, j=j: e.scalar_tensor_tensor(
                        out=acc[vb][:, o0:o0 + n], in0=src, scalar=chp_s[:, cc, j:j + 1], in1=acc[vb][:, o0:o0 + n],
                        op0=ALU.mult, op1=ALU.add), reads=[("vg", vb)], writes=[("acc", vb, o0)])
        nq = NO if last else NQ
        k.dma("sp", conv_d[cc, :, 0:nq], acc[vb][:, 0:nq], reads=[("acc", vb, 0), ("acc", vb, NO)],
              writes=[("conv_d", cc)])
    ph.end()

    ph = Phase(k)
    wb = [ph.sb([128, 16, 128], BF16) for _ in range(3)]
    cs_s = ph.sb([128, 2, NE], F32)
    qT = ph.sb([128, NQ], BF16)
    kT = ph.sb([128, NT], BF16)
    Vh = ph.sb([128, NT // 128, 128], BF16)
    bt = ph.sb([128, 5, 896], F32)
    sqe = ph.sb([128, 512], F32)
    rs = ph.sb([128, 512], F32)
    xn = ph.sb([128, 512], F32)
    t1 = ph.sb([128, 512], F32)
    t2 = ph.sb([128, 512], F32)
    stmp = ph.sb([128, 896], F32)
    PT = [ph.sb([128, 9 * 128], BF16) for _ in range(2)]
    rec = ph.sb([128, 128], F32)
    oTh = ph.sb([128, NQ], BF16)
    pj = [ph.ps([128, 512], F32) for _ in range(2)]
    psr = ph.ps([128, 512], F32)
    pst = ph.ps([128, 1536], F32)
    po = ph.ps([128, 512], F32)
    k.dma("sp", cs_s[:, 0, :], cs[0], writes=[("cs", 0)])
    k.dma("sp", cs_s[:, 1, :], cs[1], writes=[("cs", 0)])
    pji = [0]

    def qk_block(wslot, c0, n, gcol, dst_ap, dst_key, rope_c0):
        pb = pji[0] % 2
        pji[0] += 1
        X = pj[pb][:, :n]
        mm_fm(X, wb, wslot, c0, n, ("pj", pb))
        k.op("act", lambda e: e.activation(out=sqe[:, :n], in_=X, func=AF.Square), reads=[("pj", pb)], writes=[("sqe", 0)])
        k.op("pe", lambda e: e.matmul(psr[:, :n], ones_f[:, :], sqe[:, :n], start=True, stop=True),
             reads=[("sqe", 0)] + CONST, writes=[("psr", 0)])
        rstd_ops(psr[:, :n], rs[:, :n], 128, [("psr", 0)], [("rs", 0)])
        k.op("dve", lambda e: e.scalar_tensor_tensor(out=xn[:, :n], in0=X, scalar=gqk_s[:, gcol:gcol + 1], in1=rs[:, :n],
                                                     op0=ALU.mult, op1=ALU.mult),
             reads=[("pj", pb), ("rs", 0)] + CONST, writes=[("xn", 0)])
        if rope_c0 is None:
            k.op("act", lambda e: e.copy(out=dst_ap, in_=xn[:, :n]), reads=[("xn", 0)], writes=[dst_key])
            return
        k.op("pe", lambda e: e.matmul(psr[:, :n], rmt_s[:, :], xn[:, :n], start=True, stop=True),
             reads=[("xn", 0), ("rs", 0)] + CONST, writes=[("psr", 0)])
        k.op("pool", lambda e: e.tensor_tensor(out=t1[:, :n], in0=xn[:, :n], in1=cs_s[:, 0, rope_c0:rope_c0 + n], op=ALU.mult),
             reads=[("xn", 0), ("cs", 0)], writes=[("t1", 0)])
        k.op("dve", lambda e: e.tensor_tensor(out=t2[:, :n], in0=psr[:, :n], in1=cs_s[:, 1, rope_c0:rope_c0 + n], op=ALU.mult),
             reads=[("psr", 0), ("cs", 0)], writes=[("t2", 0)])
        k.op("dve", lambda e: e.tensor_tensor(out=dst_ap, in0=t1[:, :n], in1=t2[:, :n], op=ALU.add),
             reads=[("t1", 0), ("t2", 0)], writes=[dst_key])

    def cls(qt):
        return {2: 0, 3: 1, 16: 3, 17: 4}.get(qt, 2)

    def attend(qcol, slots, bias_cls, o_lo, o_hi, pti):
        P = PT[pti]

        def sc(e):
            for (s, kt) in slots:
                ins = e.matmul(pst[:, s * 128:(s + 1) * 128], kT[:, kt * 128:(kt + 1) * 128], qT[:, qcol:qcol + 128],
                               start=True, stop=True)
            return ins
        k.op("pe", sc, reads=[("kT", 0), ("qT", 0)], writes=[("pst", 0)])
        if bias_cls is not None:
            a, b_ = o_lo * 128, o_hi * 128
            k.op("dve", lambda e: e.scalar_tensor_tensor(out=stmp[:, a:b_], in0=pst[:, a:b_], scalar=SCALE,
                                                         in1=bt[:, bias_cls, a:b_], op0=ALU.mult, op1=ALU.add),
                 reads=[("pst", 0), ("bt", 0)], writes=[("stmp", 0)])
            k.op("act", lambda e: e.activation(out=P[:, a:b_], in_=stmp[:, a:b_], func=AF.Exp),
                 reads=[("stmp", 0)], writes=[("PT", pti)])
        k.op("act", lambda e: e.activation(out=P[:, 896:1152], in_=pst[:, 896:1152], func=AF.Exp, scale=SCALE),
             reads=[("pst", 0)], writes=[("PT", pti)])

        def pv(e):
            for i, (s, kt) in enumerate(slots):
                e.matmul(po[:, 0:128], Vh[:, kt, :], P[:, s * 128:(s + 1) * 128], start=(i == 0), stop=(i == len(slots) - 1))
            for i, (s, kt) in enumerate(slots):
                ins = e.matmul(po[:, 128:256], ones_b[:, :], P[:, s * 128:(s + 1) * 128], start=(i == 0),
                               stop=(i == len(slots) - 1))
            return ins
        k.op("pe", pv, reads=[("PT", pti), ("Vh", 0)] + CONST, writes=[("po", 0)])
        k.op("dve", lambda e: e.reciprocal(out=rec[:, :], in_=po[:, 128:256]), reads=[("po", 0)], writes=[("rec", 0)])
        k.op("dve", lambda e: e.tensor_tensor(out=oTh[:, qcol:qcol + 128], in0=po[:, 0:128], in1=rec[:, :], op=ALU.mult),
             reads=[("po", 0), ("rec", 0)], writes=[("oTh", 0)])

    load_w(wb, 16, 0)
    wi = 0
    for h in range(8):
        chunks = [16 + h, 24 + h, 32 + h]
        nxt = chunks[1:] + ([16 + h + 1] if h < 7 else [])
        k.dma("sp", bt[:, :, :], btab[h].rearrange("p (c n) -> p c n", c=5), writes=[("bt", 0)])
        sq_, wi = wi % 3, wi + 1
        load_w(wb, nxt[0], wi % 3)
        for i, (c0, n) in enumerate(OWN_BLOCKS):
            qk_block(sq_, c0, n, 0, qT[:, i * 512:i * 512 + n], ("qT", 0), c0)
        if not last:
            qk_block(sq_, NE, NCX, 0, qT[:, NO:NQ], ("qT", 0), None)
        sk_, wi = wi % 3, wi + 1
        load_w(wb, nxt[1], wi % 3)
        for (c0, n) in EXT_BLOCKS:
            qk_block(sk_, c0, n, 1, kT[:, c0:c0 + n], ("kT", 0), c0)
        qk_block(sk_, NE, NCX, 1, kT[:, NE:NT], ("kT", 0), None)
        sv_, wi = wi % 3, wi + 1
        if len(nxt) > 2:
            load_w(wb, nxt[2], wi % 3)
        for g0 in range(0, NT // 128, 4):
            gn = min(4, NT // 128 - g0)
            pb = pji[0] % 2
            pji[0] += 1

            def vmm(e, g0=g0, gn=gn, pb=pb):
                for t in range(gn):
                    col = (g0 + t) * 128
                    for kc in range(16):
                        ins = e.matmul(pj[pb][:, t * 128:(t + 1) * 128], hT[:, kc, col:col + 128], wb[sv_][:, kc, :],
                                       start=(kc == 0), stop=(kc == 15))
                return ins
            k.op("pe", vmm, reads=[("wb", sv_)] + [("hT", g0 + t) for t in range(gn)], writes=[("pj", pb)])
            k.op("act", lambda e, g0=g0, gn=gn, pb=pb: e.copy(out=Vh[:, g0:g0 + gn, :],
                                                             in_=pj[pb][:, :gn * 128].rearrange("p (t d) -> p t d", d=128)),
                 reads=[("pj", pb)], writes=[("Vh", 0)])
        ai = 0
        for qt in range(2, 18):
            slots = [(o, qt - 3 + o) for o in range(7) if 0 <= qt - 3 + o <= 19]
            o_lo, o_hi = slots[0][0], slots[-1][0] + 1
            attend((qt - 2) * 128, slots + [(7, 20), (8, 21)], cls(qt), o_lo, o_hi, ai % 2)
            ai += 1
        if not last:
            for t in range(2):
                attend(NO + t * 128, [(7, 20), (8, 21)], None, 0, 0, ai % 2)
                ai += 1
        nq = NO if last else NQ
        k.dma("sp", oT_d[8 + h, :, 0:nq], oTh[:, 0:nq], reads=[("oTh", 0)], writes=[("oT_d", 8 + h)])
    ph.end()

    ph = Phase(k)
    cv = [ph.sb([128, 8, 512], F32) for _ in range(2)]
    cq = ph.sb([128, 8, 512], F32)
    mean = ph.sb([128, 512], F32)
    var = ph.sb([128, 512], F32)
    d1 = [ph.sb([128, 512], F32) for _ in range(2)]
    ob = [ph.sb([128, 8, 512], BF16) for _ in range(2)]
    p1 = ph.ps([128, 512], F32)
    p2 = ph.ps([128, 512], F32)
    lnblocks = [(i * 512, 512) for i in range(4)] + ([] if last else [(NO, NCX)])

    def load_cv(i):
        c0, n = lnblocks[i]
        k.dma("sp", cv[i % 2][:, :, :n], conv_d[:, :, c0:c0 + n].rearrange("c p t -> p c t"),
              reads=[("conv_d", c) for c in range(8)], writes=[("cv", i % 2)])
    load_cv(0)
    for i, (c0, n) in enumerate(lnblocks):
        b = i % 2
        if i + 1 < len(lnblocks):
            load_cv(i + 1)
        k.op("act", lambda e, b=b, n=n: e.activation(out=cq[:, :, :n], in_=cv[b][:, :, :n], func=AF.Square),
             reads=[("cv", b)], writes=[("cq", 0)])

        def s1(e, b=b, n=n):
            for c in range(8):
                ins = e.matmul(p1[:, :n], ones_f[:, :], cv[b][:, c, :n], start=(c == 0), stop=(c == 7))
            return ins

        def s2(e, n=n):
            for c in range(8):
                ins = e.matmul(p2[:, :n], ones_f[:, :], cq[:, c, :n], start=(c == 0), stop=(c == 7))
            return ins
        k.op("pe", s1, reads=[("cv", b)] + CONST, writes=[("p1", 0)])
        k.op("pe", s2, reads=[("cq", 0)] + CONST, writes=[("p2", 0)])
        k.op("dve", lambda e, n=n: e.tensor_scalar(out=mean[:, :n], in0=p1[:, :n], scalar1=1.0 / 1024, scalar2=None,
                                                   op0=ALU.mult), reads=[("p1", 0)], writes=[("mean", 0)])
        k.op("dve", lambda e, n=n: e.tensor_tensor(out=var[:, :n], in0=mean[:, :n], in1=mean[:, :n], op=ALU.mult),
             reads=[("mean", 0)], writes=[("var", 0)])
        k.op("dve", lambda e, n=n: e.scalar_tensor_tensor(out=var[:, :n], in0=p2[:, :n], scalar=1.0 / 1024, in1=var[:, :n],
                                                          op0=ALU.mult, op1=ALU.subtract),
             reads=[("p2", 0), ("var", 0)], writes=[("var", 0)])
        k.op("act", lambda e, n=n: e.activation(out=var[:, :n], in_=var[:, :n], func=AF.Sqrt, bias=eps_s[:, 0:1], scale=1.0),
             reads=[("var", 0)] + CONST, writes=[("var", 0)])
        k.op("dve", lambda e, n=n: e.reciprocal(out=var[:, :n], in_=var[:, :n]), reads=[("var", 0)], writes=[("var", 0)])
        for c in range(8):
            db = c % 2
            eng = "dve" if c % 2 == 0 else "pool"
            k.op(eng, lambda e, b=b, c=c, n=n, db=db: e.tensor_tensor(out=d1[db][:, :n], in0=cv[b][:, c, :n], in1=mean[:, :n],
                                                                     op=ALU.subtract),
                 reads=[("cv", b), ("mean", 0)], writes=[("d1", db)])
            k.op(eng, lambda e, n=n, db=db: e.tensor_tensor(out=d1[db][:, :n], in0=d1[db][:, :n], in1=var[:, :n], op=ALU.mult),
                 reads=[("var", 0), ("d1", db)], writes=[("d1", db)])
            k.op("act", lambda e, b=b, c=c, n=n, db=db: e.activation(out=ob[b][:, c, :n], in_=d1[db][:, :n], func=AF.Silu,
                                                                    bias=chp_s[:, c, 33:34], scale=chp_s[:, c, 32:33]),
                 reads=[("d1", db)] + CONST, writes=[("ob", b)])
        k.dma("sp", oT_d[0:8, :, c0:c0 + n].rearrange("c p t -> p c t"), ob[b][:, :, :n], reads=[("ob", b)],
              writes=[("oT_d", c) for c in range(8)])
    ph.end()

    ph = Phase(k)
    wo = big[:, 0:16 * D].rearrange("p (kc n) -> p kc n", kc=16)
    rows = [ph.sb([128, D], F32) for _ in range(4)]
    xt = [ph.sb([128, D], F32) for _ in range(2)]
    oTt = [ph.sb([128, 16, 128], BF16) for _ in range(2)]
    xm = ph.sb([128, D], F32)
    sq = ph.sb([128, D], F32)
    h2f = ph.sb([128, D], F32)
    h2b = ph.sb([128, D], BF16)
    h2T = ph.sb([128, 16, 128], F32)
    ss = ph.sb([128, 2], F32)
    wr_s = ph.sb([128, 16, 32], F32)
    br_s = ph.sb([128, 32], F32)
    lg = ph.sb([128, 32], F32)
    m8 = ph.sb([128, 8], F32)
    negm = ph.sb([128, 1], F32)
    msk = ph.sb([128, 32], F32)
    ex = ph.sb([128, 32], F32)
    gs = ph.sb([128, 2], F32)
    Gt = ph.sb([128, 32], F32)
    pw = ph.ps([128, D], F32)
    pt2 = ph.ps([128, 1024], F32)
    pl = ph.ps([128, 32], F32)
    k.dma("pool", wo, wout.rearrange("p (kc n) -> p kc n", kc=16), writes=[("wo", 0)])
    k.dma("sp", wr_s[:, :, :], wr.rearrange("p (kc n) -> p kc n", kc=16), writes=[("wr", 0)])
    k.dma("sp", br_s[:, :], br.broadcast_to([128, 32]), writes=[("wr", 1)])

    def load_rows(r):
        k.dma("sp", rows[0][:, :], bc(modv[r:r + 1, 2 * D:3 * D]), writes=[("row", 0)])
        k.dma("sp", rows[3][:, :], bc(gmf[1:2, :]), writes=[("row", 3)])
        k.dma("sp", rows[1][:, :], bc(modv[r:r + 1, 4 * D:5 * D]), writes=[("row", 1)])
        k.dma("sp", rows[2][:, :], bc(modv[r:r + 1, 3 * D:4 * D]), writes=[("row", 2)])
        k.op("dve", lambda e: e.scalar_tensor_tensor(out=rows[1][:, :], in0=rows[1][:, :], scalar=1.0, in1=rows[3][:, :],
                                                     op0=ALU.add, op1=ALU.mult), reads=[("row", 3)], writes=[("row", 1)])

    tiles4 = [(xe, OWN0 // 128 + t, t * 128, 0, x_mid, G_o, h2_o, t) for t in range(NO // 128)]
    if not last:
        tiles4 += [(cx, t, NO + t * 128, 1, c_mid, Gc_o, h2c_o, t) for t in range(NCX // 128)]

    def load4(i):
        src, st, col, which, _, _, _, _ = tiles4[i]
        k.dma("sp", xt[i % 2][:, :], src[st * 128:(st + 1) * 128, :], writes=[("xt", i % 2)])
        k.dma("sp", oTt[i % 2][:, :, :], oT_d[:, :, col:col + 128].rearrange("c p t -> p c t"),
              reads=[("oT_d", c) for c in range(16)], writes=[("oTt", i % 2)])

    load_rows(0)
    load4(0)
    for i, (src, st, col, which, xo, Go, ho, ot) in enumerate(tiles4):
        b = i % 2
        if which == 1 and tiles4[i - 1][3] == 0:
            load_rows(1)
        if i + 1 < len(tiles4):
            load4(i + 1)

        def mo(e, b=b):
            for nb in range(4):
                for kc in range(16):
                    ins = e.matmul(pw[:, nb * 512:(nb + 1) * 512], oTt[b][:, kc, :], wo[:, kc, nb * 512:(nb + 1) * 512],
                                   start=(kc == 0), stop=(kc == 15))
            return ins
        k.op("pe", mo, reads=[("oTt", b), ("wo", 0)], writes=[("pw", 0)])
        k.op("dve", lambda e: e.tensor_tensor(out=xm[:, :], in0=pw[:, :], in1=rows[0][:, :], op=ALU.mult),
             reads=[("pw", 0), ("row", 0)], writes=[("xm", 0)])
        k.op("pool", lambda e, b=b: e.tensor_tensor(out=xm[:, :], in0=xm[:, :], in1=xt[b][:, :], op=ALU.add),
             reads=[("xt", b), ("xm", 0)], writes=[("xm", 0)])
        k.dma("sp", xo[ot * 128:(ot + 1) * 128, :], xm[:, :], reads=[("xm", 0)], is_output=True)
        k.op("act", lambda e: e.activation(out=sq[:, :], in_=xm[:, :], func=AF.Square), reads=[("xm", 0)], writes=[("sq", 0)])
        k.op("dve", lambda e: e.tensor_reduce(out=ss[:, 0:1], in_=sq[:, :], axis=AX.X, op=ALU.add),
             reads=[("sq", 0)], writes=[("ss", 0)])
        rstd_ops(ss[:, 0:1], ss[:, 1:2], D, [("ss", 0)], [("ss", 0)])
        k.op("dve", lambda e: e.scalar_tensor_tensor(out=h2f[:, :], in0=xm[:, :], scalar=ss[:, 1:2], in1=rows[1][:, :],
                                                     op0=ALU.mult, op1=ALU.mult),
             reads=[("xm", 0), ("ss", 0), ("row", 1)], writes=[("h2f", 0)])
        k.op("pool", lambda e: e.tensor_tensor(out=h2f[:, :], in0=h2f[:, :], in1=rows[2][:, :], op=ALU.add),
             reads=[("h2f", 0), ("row", 2)], writes=[("h2f", 0)])
        k.op("act", lambda e: e.copy(out=h2b[:, :], in_=h2f[:, :]), reads=[("h2f", 0)], writes=[("h2b", 0)])
        k.dma("sp", ho[ot * 128:(ot + 1) * 128, :], h2b[:, :], reads=[("h2b", 0)], is_output=True)
        for half in range(2):
            def trf(e, half=half):
                for j in range(8):
                    kc = half * 8 + j
                    ins = e.transpose(pt2[:, j * 128:(j + 1) * 128], h2f[:, kc * 128:(kc + 1) * 128], ident_f[:, :])
                return ins
            k.op("pe", trf, reads=[("h2f", 0)] + CONST, writes=[("pt2", 0)])
            k.op("act", lambda e, half=half: e.copy(out=h2T[:, half * 8:(half + 1) * 8, :],
                                                   in_=pt2[:, :].rearrange("p (j t) -> p j t", j=8)),
                 reads=[("pt2", 0)], writes=[("h2T", half)])

        def rl(e):
            for kc in range(16):
                ins = e.matmul(pl[:, :], h2T[:, kc, :], wr_s[:, kc, :], start=(kc == 0), stop=(kc == 15))
            return ins
        k.op("pe", rl, reads=[("h2T", 0), ("h2T", 1), ("wr", 0)], writes=[("pl", 0)])
        k.op("dve", lambda e: e.tensor_tensor(out=lg[:, :], in0=pl[:, :], in1=br_s[:, :], op=ALU.add),
             reads=[("pl", 0), ("wr", 1)], writes=[("lg", 0)])
        k.op("dve", lambda e: e.max(out=m8[:, :], in_=lg[:, :]), reads=[("lg", 0)], writes=[("m8", 0)])
        k.op("dve", lambda e: e.tensor_scalar(out=negm[:, :], in0=m8[:, 0:1], scalar1=-1.0, scalar2=None, op0=ALU.mult),
             reads=[("m8", 0)], writes=[("negm", 0)])
        k.op("dve", lambda e: e.tensor_scalar(out=msk[:, :], in0=lg[:, :], scalar1=m8[:, 3:4], scalar2=None, op0=ALU.is_ge),
             reads=[("lg", 0), ("m8", 0)], writes=[("msk", 0)])
        k.op("act", lambda e: e.activation(out=ex[:, :], in_=lg[:, :], func=AF.Exp, bias=negm[:, 0:1], scale=1.0),
             reads=[("lg", 0), ("negm", 0)], writes=[("ex", 0)])
        k.op("dve", lambda e: e.tensor_tensor(out=ex[:, :], in0=ex[:, :], in1=msk[:, :], op=ALU.mult),
             reads=[("ex", 0), ("msk", 0)], writes=[("ex", 0)])
        k.op("dve", lambda e: e.tensor_reduce(out=gs[:, 0:1], in_=ex[:, :], axis=AX.X, op=ALU.add),
             reads=[("ex", 0)], writes=[("gs", 0)])
        k.op("dve", lambda e: e.reciprocal(out=gs[:, 1:2], in_=gs[:, 0:1]), reads=[("gs", 0)], writes=[("gs", 0)])
        k.op("dve", lambda e: e.tensor_scalar(out=Gt[:, :], in0=ex[:, :], scalar1=gs[:, 1:2], scalar2=None, op0=ALU.mult),
             reads=[("ex", 0), ("gs", 0)], writes=[("Gt", 0)])
        k.dma("sp", Go[ot * 128:(ot + 1) * 128, :], Gt[:, :], reads=[("Gt", 0)], is_output=True)
    k.finish()
    ph.es.close()
    k.close()
    return nc


ALPHA = 1.702
LIM = 7.0


def split_tiles(ntl, mx):
    out, t = [], 0
    while t < ntl:
        n = min(mx, ntl - t)
        out.append((t, n))
        t += n
    return out


def build_C(caps):
    nc = bass.Bass("TRN2", target_bir_lowering=False)

    def din(name, shape, dt=F32):
        return nc.dram_tensor(name, list(shape), dt, kind="ExternalInput").ap()

    wgu = din("wgu", [4, 16, 128, 16 * 256])
    wdn = din("wdn", [4, 128, 16 * D])
    bgu = din("bgu", [4, 128, 32])
    bdn = din("bdn", [4, 1, D])
    XT = [din("XT%d" % e, [128, 16 * caps[e]], BF16) for e in range(4)]
    gw = [din("gw%d" % e, [128, caps[e] // 128]) for e in range(4)]
    Y = [nc.dram_tensor("Y%d" % e, [caps[e], D], F32, kind="ExternalOutput").ap() for e in range(4)]

    k = KB(nc)
    SBT = 9
    sbs_e = []
    for e in range(4):
        ntl = caps[e] // 128
        nsb = -(-ntl // SBT)
        base, rem = ntl // nsb, ntl % nsb
        lst, t = [], 0
        for i in range(nsb):
            n = base + (1 if i < rem else 0)
            lst.append((t, n))
            t += n
        sbs_e.append(lst)
    mxs = max(n for lst in sbs_e for _, n in lst) * 128
    mxt = max(caps) // 128
    xts = k.sb([128, 16, mxs], BF16)
    act = k.sb([128, 16, mxs], BF16)
    wg = [k.sb([128, 16, 256], BF16) for _ in range(3)]
    wd = [k.sb([128, 16, 512], BF16) for _ in range(3)]
    gt_ = [k.sb([128, 512], F32) for _ in range(3)]
    st_ = [k.sb([128, 512], F32) for _ in range(3)]
    lt_ = [k.sb([128, 512], F32) for _ in range(3)]
    tt_ = [k.sb([128, 512], F32) for _ in range(3)]
    b1_s = [k.sb([128, 16], F32) for _ in range(2)]
    ys = [k.sb([128, 512], F32) for _ in range(3)]
    bg_s = [k.sb([128, 32], F32) for _ in range(2)]
    bd_s = [k.sb([128, D], F32) for _ in range(2)]
    gw_s = [k.sb([128, mxt], F32) for _ in range(2)]
    pA = [k.ps([128, 512], F32) for _ in range(3)]
    pB = [k.ps([128, 512], F32) for _ in range(3)]
    pD = [k.ps([128, 512], F32) for _ in range(2)]

    gu_jobs = [(e, si, j) for e in range(4) for si in range(len(sbs_e[e])) for j in range(16)]
    wgi = {job: i for i, job in enumerate(gu_jobs)}
    dn_jobs = [(e, si, nb) for e in range(4) for si in range(len(sbs_e[e])) for nb in range(4)]
    wdi = {job: i for i, job in enumerate(dn_jobs)}

    def load_wg(i):
        e, si, j = gu_jobs[i]
        k.dma("pool", wg[i % 3][:, :, :], wgu[e, j].rearrange("p (kc n) -> p kc n", kc=16), writes=[("wg", i % 3)])

    def load_wd(i):
        e, si, nb = dn_jobs[i]
        k.dma("pool", wd[i % 3][:, :, :],
              wdn[e].rearrange("p (kc n) -> p kc n", kc=16)[:, :, nb * 512:(nb + 1) * 512], writes=[("wd", i % 3)])

    load_wg(0)
    load_wg(1)
    load_wd(0)
    it = 0
    yi = 0
    for e in range(4):
        eb = e % 2
        ntl = caps[e] // 128
        k.dma("sp", bg_s[eb][:, :], bgu[e], writes=[("bg", eb)])
        k.dma("sp", bd_s[eb][:, :], bdn[e].broadcast_to([128, D]), writes=[("bd", eb)])
        k.dma("sp", gw_s[eb][:, :ntl], gw[e], writes=[("gw", eb)])
        k.op("dve", lambda e_: e_.tensor_scalar(out=b1_s[eb][:, :], in0=bg_s[eb][:, 16:32], scalar1=1.0, scalar2=None, op0=ALU.add),
             reads=[("bg", eb)], writes=[("b1", eb)])
        for si, (t0, nt) in enumerate(sbs_e[e]):
            ns = nt * 128
            k.dma("sp", xts[:, :, :ns], XT[e].rearrange("p (kc t) -> p kc t", kc=16)[:, :, t0 * 128:t0 * 128 + ns],
                  writes=[("xts", 0)])
            nblocks = [(a * 128, n * 128) for a, n in split_tiles(nt, 4)]
            for j in range(16):
                i = wgi[(e, si, j)]
                if i + 2 < len(gu_jobs):
                    load_wg(i + 2)
                ws = wg[i % 3]
                for (c0, n) in nblocks:
                    pb = it % 3
                    it += 1

                    def mm(e_, ps, off):
                        for kc in range(16):
                            ins = e_.matmul(ps[:, :n], ws[:, kc, off:off + 128], xts[:, kc, c0:c0 + n], start=(kc == 0), stop=(kc == 15))
                        return ins
                    k.op("pe", lambda e_: mm(e_, pA[pb], 0), reads=[("wg", i % 3), ("xts", 0)], writes=[("pA", pb)])
                    k.op("pe", lambda e_: mm(e_, pB[pb], 128), reads=[("wg", i % 3), ("xts", 0)], writes=[("pB", pb)])
                    k.op("dve", lambda e_: e_.tensor_scalar(out=gt_[pb][:, :n], in0=pA[pb][:, :n], scalar1=bg_s[eb][:, j:j + 1],
                                                            scalar2=LIM, op0=ALU.add, op1=ALU.min),
                         reads=[("pA", pb), ("bg", eb)], writes=[("g", pb)])
                    k.op("act", lambda e_: e_.activation(out=st_[pb][:, :n], in_=gt_[pb][:, :n], func=AF.Sigmoid, scale=ALPHA),
                         reads=[("g", pb)], writes=[("s", pb)])
                    k.op("dve", lambda e_: e_.tensor_scalar(out=lt_[pb][:, :n], in0=pB[pb][:, :n], scalar1=b1_s[eb][:, j:j + 1],
                                                            scalar2=1.0 - LIM, op0=ALU.add, op1=ALU.max),
                         reads=[("pB", pb), ("b1", eb)], writes=[("l", pb)])
                    k.op("pool", lambda e_: e_.tensor_tensor(out=tt_[pb][:, :n], in0=gt_[pb][:, :n], in1=st_[pb][:, :n], op=ALU.mult),
                         reads=[("g", pb), ("s", pb)], writes=[("t", pb)])
                    k.op("dve", lambda e_: e_.scalar_tensor_tensor(out=act[:, j, c0:c0 + n], in0=lt_[pb][:, :n], scalar=1.0 + LIM,
                                                                   in1=tt_[pb][:, :n], op0=ALU.min, op1=ALU.mult),
                         reads=[("t", pb), ("l", pb)], writes=[("act", j)])
            for nb in range(4):
                i = wdi[(e, si, nb)]
                if i + 1 < len(dn_jobs):
                    load_wd(i + 1)
                wds = wd[i % 3]
                for t in range(nt):
                    pb = it % 2
                    it += 1
                    yb = yi % 3
                    yi += 1

                    def mmd(e_):
                        for kc in range(16):
                            ins = e_.matmul(pD[pb][:, :], act[:, kc, t * 128:(t + 1) * 128], wds[:, kc, :], start=(kc == 0), stop=(kc == 15))
                        return ins
                    k.op("pe", mmd, reads=[("wd", i % 3)] + [("act", j) for j in range(16)], writes=[("pD", pb)])
                    k.op("dve", lambda e_: e_.tensor_tensor(out=ys[yb][:, :], in0=pD[pb][:, :], in1=bd_s[eb][:, nb * 512:(nb + 1) * 512],
                                                            op=ALU.add), reads=[("pD", pb), ("bd", eb)], writes=[("ys", yb)])
                    k.op("act", lambda e_: e_.mul(out=ys[yb][:, :], in_=ys[yb][:, :], mul=gw_s[eb][:, t0 + t:t0 + t + 1]),
                         reads=[("ys", yb), ("gw", eb)], writes=[("ys", yb)])
                    k.dma("sp", Y[e][(t0 + t) * 128:(t0 + t + 1) * 128, nb * 512:(nb + 1) * 512], ys[yb][:, :],
                          reads=[("ys", yb)], is_output=True)
    k.finish()
    k.close()
    return nc


def build_D(ntiles, nlat, ns=4):
    nc = bass.Bass("TRN2", target_bir_lowering=False)
    R = ntiles * 128
    xm = nc.dram_tensor("xm", [R, D], F32, kind="ExternalInput").ap()
    y4 = nc.dram_tensor("y4", [ns, R, D], F32, kind="ExternalInput").ap()
    gtv = nc.dram_tensor("gtv", [2, D], F32, kind="ExternalInput").ap()
    xo = nc.dram_tensor("xo", [R, D], F32, kind="ExternalOutput").ap()
    k = KB(nc)
    gts = [k.sb([128, D], F32) for _ in range(2)]
    xb = [k.sb([128, D], F32) for _ in range(2)]
    yb = [[k.sb([128, D], F32) for _ in range(ns)] for _ in range(2)]
    for r in range(2):
        k.dma("sp", gts[r][:, :], gtv[r:r + 1, :].broadcast_to([128, D]), writes=[("gt", r)])

    def load(i):
        b = i % 2
        k.dma("sp", xb[b][:, :], xm[i * 128:(i + 1) * 128, :], writes=[("xb", b)])
        for s in range(ns):
            k.dma("sp", yb[b][s][:, :], y4[s, i * 128:(i + 1) * 128, :], writes=[("yb", b, s)])
    load(0)
    for i in range(ntiles):
        b = i % 2
        if i + 1 < ntiles:
            load(i + 1)
        r = 0 if i < nlat else 1
        for s in range(1, ns):
            eng = "dve" if s % 2 == 1 else "pool"
            k.op(eng, lambda e: e.tensor_tensor(out=yb[b][0][:, :], in0=yb[b][0][:, :], in1=yb[b][s][:, :], op=ALU.add),
                 reads=[("yb", b, s)], writes=[("yb", b, 0)])
        k.op("pool", lambda e: e.tensor_tensor(out=yb[b][0][:, :], in0=yb[b][0][:, :], in1=gts[r][:, :], op=ALU.mult),
             reads=[("gt", r)], writes=[("yb", b, 0)])
        k.op("dve", lambda e: e.tensor_tensor(out=xb[b][:, :], in0=xb[b][:, :], in1=yb[b][0][:, :], op=ALU.add),
             reads=[("yb", b, 0)], writes=[("xb", b)])
        k.dma("sp", xo[i * 128:(i + 1) * 128, :], xb[b][:, :], reads=[("xb", b)], is_output=True)
    k.finish()
    k.close()
    return nc


def consts_B():
    idn = np.eye(128, dtype=np.float32)
    Rm = np.zeros((128, 128), np.float32)
    for base in (0, 64):
        for j in range(32):
            Rm[base + j, base + 32 + j] = -1.0
            Rm[base + 32 + j, base + j] = 1.0
    return idn, np.ascontiguousarray(Rm.T)

def rope_tables(R0):
    t = np.arange(NE)
    row = (R0 - 4 + t // 64).astype(np.float32)
    col = (t % 64).astype(np.float32)
    inv = (np.float32(10000.0) ** (-(np.arange(32, dtype=np.float32)) / np.float32(32))).astype(np.float32)
    d = np.arange(128)
    j = d % 32
    pos = np.where((d < 64)[:, None], row[None, :], col[None, :]).astype(np.float32)
    ang = (pos * inv[j][:, None]).astype(np.float32)
    return np.stack([np.cos(ang), np.sin(ang)], 0).astype(np.float32)

def valid_mask(R0):
    t = np.arange(NE)
    row = R0 - 4 + t // 64
    return ((row >= 0) & (row < 128)).astype(np.float32)[None, :]

def bias_tables(rpb_l, R0):
    out = np.empty((8, 128, 5, 7, 128), np.float32)
    kk = np.arange(128)[:, None]
    qq = np.arange(128)[None, :]
    for ci, qt in enumerate((2, 3, 8, 16, 17)):
        r0 = R0 - 4 + 2 * qt
        r = r0 + qq // 64
        qc = qq % 64
        rs = np.clip(r - 4, 0, 120)
        ws = np.clip(qc - 8, 0, 48)
        for o in range(7):
            kt = qt - 3 + o
            kr = R0 - 4 + 2 * kt + kk // 64
            kc = kk % 64
            valid = (kr >= 0) & (kr < 128) & (kr >= rs) & (kr < rs + 8) & (kc >= ws) & (kc < ws + 16)
            dr = np.clip(kr - r + 7, 0, 14)
            dc = np.clip(kc - qc, -15, 15) + 15
            b = rpb_l[:, dr, dc]
            out[:, :, ci, o, :] = np.where(valid[None], b, NEGB)
    return out.reshape(8, 128, 5 * 896)

def layer_consts(inp, l):
    w_in = inp["w_in"][l]
    win = np.ascontiguousarray(w_in.reshape(16, 128, 40, 128).transpose(2, 1, 0, 3)).reshape(40, 128, 2048)
    wout = np.ascontiguousarray(inp["w_out"][l].reshape(16, 128, 2048).transpose(1, 0, 2)).reshape(128, 16 * 2048)
    chp = np.empty((128, 8, 34), np.float32)
    chp[:, :, :31] = inp["w_dw"][l].T.reshape(8, 128, 31).transpose(1, 0, 2)
    chp[:, :, 31] = inp["b_dw"][l].reshape(8, 128).T
    chp[:, :, 32] = inp["ln_g"][l].reshape(8, 128).T
    chp[:, :, 33] = inp["ln_b"][l].reshape(8, 128).T
    gqk = np.ascontiguousarray(np.stack([inp["g_q"][l], inp["g_k"][l]], 1))
    gmf = np.ascontiguousarray(np.stack([inp["g_mix"][l], inp["g_ffn"][l]], 0))
    wr = np.ascontiguousarray(inp["w_router"][l].reshape(16, 128, 32).transpose(1, 0, 2)).reshape(128, 512)
    br = np.ascontiguousarray(inp["b_router"][l][None, :])
    return dict(win=win, wout=wout, chp=chp.reshape(128, 8 * 34), gqk=gqk, gmf=gmf, wr=wr, br=br)

def core_inputs_B(x, ctx, mod_l, rpb_l, lc, ci):
    b, j = ci // 4, ci % 4
    R0 = 32 * j
    lo, hi = (R0 - 4) * 64, (R0 + 36) * 64
    xe = np.zeros((NE, D), np.float32)
    a, e = max(lo, 0), min(hi, 8192)
    xe[a - lo:e - lo] = x[b, a:e]
    idn, rmt = consts_B()
    m = dict(lc)
    m.update(xe=xe, cx=np.ascontiguousarray(ctx[b]), modv=np.ascontiguousarray(mod_l[[b, 2]]), cs=rope_tables(R0),
             rmt=rmt, idn=idn, vmask=valid_mask(R0), btab=bias_tables(rpb_l, R0))
    return m


def prep_expert_weights(wgu, wdn, bgu, bdn):
    n = wgu.shape[0]
    a = wgu.reshape(n, 16, 128, 2, 16, 128)
    a = np.ascontiguousarray(a.transpose(0, 4, 2, 1, 3, 5)).reshape(n, 16, 128, 16 * 256)
    b = np.ascontiguousarray(wdn.reshape(n, 16, 128, 2048).transpose(0, 2, 1, 3)).reshape(n, 128, 16 * 2048)
    c = np.ascontiguousarray(bgu.reshape(n, 2, 16, 128).transpose(0, 3, 1, 2)).reshape(n, 128, 32)
    return a, b, c, np.ascontiguousarray(bdn[:, None, :])


def _run(nc, in_maps):
    res = run_bass_kernel_spmd(nc, in_maps, core_ids=list(range(8)))
    return res.results


def kernel(x, c, ctx, c_ctx, w_ada, b_ada, g_mix, g_ffn, w_in, w_dw, b_dw, ln_g, ln_b, g_q, g_k, rpb,
           w_out, w_router, b_router, w_gate_up, b_gate_up, w_down, b_down):
    f = lambda a: np.asarray(a, dtype=np.float32)
    inp = dict(x=f(x), c=f(c), ctx=f(ctx), c_ctx=f(c_ctx), w_ada=f(w_ada), b_ada=f(b_ada), g_mix=f(g_mix), g_ffn=f(g_ffn),
               w_in=f(w_in), w_dw=f(w_dw), b_dw=f(b_dw), ln_g=f(ln_g), ln_b=f(ln_b), g_q=f(g_q), g_k=f(g_k), rpb=f(rpb),
               w_out=f(w_out), w_router=f(w_router), b_router=f(b_router), w_gate_up=f(w_gate_up), b_gate_up=f(b_gate_up),
               w_down=f(w_down), b_down=f(b_down))
    sT = np.ascontiguousarray(np.stack([inp["c"][0], inp["c"][1], inp["c_ctx"]], 0).T)
    resA = _run(build_A(), [{"sT": sT, "wa": np.ascontiguousarray(inp["w_ada"][:, :, i * NCOL:(i + 1) * NCOL]),
                             "ba": np.ascontiguousarray(inp["b_ada"][:, i * NCOL:(i + 1) * NCOL])} for i in range(8)])
    mod = np.concatenate([r["mod"] for r in resA], axis=2)
    x_cur, ctx_cur = inp["x"], inp["ctx"]
    for l in range(2):
        last = l == 1
        lcst = layer_consts(inp, l)
        resB = _run(build_B(last), [core_inputs_B(x_cur, ctx_cur, mod[l], inp["rpb"][l], lcst, ci) for ci in range(8)])
        x_mid = np.concatenate([r["x_mid"] for r in resB], 0)
        Gall = np.concatenate([r["G_o"] for r in resB], 0)
        Hall = np.concatenate([np.asarray(r["h2_o"]).view(np.uint16) for r in resB], 0)
        if not last:
            c_mid = np.concatenate([resB[0]["c_mid"], resB[4]["c_mid"]], 0)
            Gall = np.concatenate([Gall, resB[0]["Gc_o"], resB[4]["Gc_o"]], 0)
            Hall = np.concatenate([Hall, np.asarray(resB[0]["h2c_o"]).view(np.uint16),
                                   np.asarray(resB[4]["h2c_o"]).view(np.uint16)], 0)
        T = Gall.shape[0]
        sel = Gall > 0
        idx = [np.nonzero(sel[:, E])[0] for E in range(32)]
        counts = np.array([len(i) for i in idx])
        order = np.argsort(-counts, kind="stable")
        caps = [max(128, int(-(-counts[order[8 * s:8 * s + 8]].max() // 128) * 128)) for s in range(4)]
        ns = int(sel.sum(1).max())
        in_maps = []
        for ci in range(8):
            es = [int(order[8 * s + ci]) for s in range(4)]
            a, b, cc_, d_ = prep_expert_weights(inp["w_gate_up"][l, es], inp["w_down"][l, es],
                                                inp["b_gate_up"][l, es], inp["b_down"][l, es])
            m = dict(wgu=a, wdn=b, bgu=cc_, bdn=d_)
            for s in range(4):
                ii = idx[es[s]]
                XT = np.zeros((128, 16, caps[s]), np.uint16)
                XT[:, :, :len(ii)] = Hall[ii].reshape(len(ii), 16, 128).transpose(2, 1, 0)
                gw = np.zeros((caps[s],), np.float32)
                gw[:len(ii)] = Gall[ii, es[s]]
                m["XT%d" % s] = XT.reshape(128, 16 * caps[s]).view(ml_dtypes.bfloat16)
                m["gw%d" % s] = np.ascontiguousarray(gw.reshape(caps[s] // 128, 128).T)
            in_maps.append(m)
        resC = _run(build_C(caps), in_maps)
        del in_maps
        slot = np.cumsum(sel, axis=1) - 1
        y4 = np.zeros((ns, T, D), np.float32)
        for pos in range(32):
            E = int(order[pos])
            ii = idx[E]
            y4[slot[ii, E], ii] = resC[pos % 8]["Y%d" % (pos // 8)][:len(ii)]
        del resC
        ntl = 16 if last else 17
        in_maps = []
        for ci in range(8):
            b = ci // 4
            xm = np.zeros((ntl * 128, D), np.float32)
            yy = np.zeros((ns, ntl * 128, D), np.float32)
            xm[:2048] = x_mid[ci * 2048:(ci + 1) * 2048]
            yy[:, :2048] = y4[:, ci * 2048:(ci + 1) * 2048]
            if not last and ci < 4:
                xm[2048:] = c_mid[ci * 128:(ci + 1) * 128]
                yy[:, 2048:] = y4[:, 16384 + ci * 128:16384 + (ci + 1) * 128]
            gtv = np.ascontiguousarray(np.stack([mod[l, b, 5 * D:6 * D], mod[l, 2, 5 * D:6 * D]], 0))
            in_maps.append(dict(xm=xm, y4=yy, gtv=gtv))
        resD = _run(build_D(ntl, 16, ns), in_maps)
        del in_maps, y4
        x_cur = np.concatenate([r["xo"][:2048] for r in resD], 0).reshape(2, 8192, D)
        if not last:
            ctx_cur = np.concatenate([resD[ci]["xo"][2048:] for ci in range(4)], 0).reshape(2, 256, D)
    return np.ascontiguousarray(x_cur, dtype=np.float32)
```

```python
import ml_dtypes
from concourse.bass_utils import run_bass_kernel_spmd
import numpy as np
from contextlib import ExitStack
import concourse.bass as bass
import concourse.mybir as mybir

F32 = mybir.dt.float32
BF16 = mybir.dt.bfloat16
I32 = mybir.dt.int32
ALU = mybir.AluOpType
AF = mybir.ActivationFunctionType
AX = mybir.AxisListType


class KB:
    SEM_ROLL = 20000
    NDMA = 8

    def __init__(self, nc):
        self.nc = nc
        self.es = ExitStack()
        self.eng = {"pe": nc.tensor, "dve": nc.vector, "act": nc.scalar, "pool": nc.gpsimd, "sp": nc.sync}
        self.cur = {}
        self.cnt = {}
        self.nsem = 0
        for e in ("pe", "dve", "act", "pool"):
            self._roll(e)
        self.dsem = {}
        self.dcnt = {}
        self.dnext = {}
        for q in ("sp", "pool", "act"):
            self.dsem[q] = [self._newsem() for _ in range(self.NDMA)]
            self.dcnt[q] = [0] * self.NDMA
            self.dnext[q] = 0
        self.seen = {e: {} for e in self.eng}
        self.state = {}
        self.nt = 0
        self.out_deps = []

    def _newsem(self):
        self.nsem += 1
        return self.es.enter_context(self.nc.semaphore("s%d" % self.nsem))

    def _roll(self, e):
        self.cur[e] = self._newsem()
        self.cnt[e] = 0

    def sb(self, shape, dt, name=None):
        self.nt += 1
        return self.es.enter_context(self.nc.sbuf_tensor(name or ("t%d" % self.nt), list(shape), dt))

    def ps(self, shape, dt, name=None):
        self.nt += 1
        return self.es.enter_context(self.nc.psum_tensor(name or ("p%d" % self.nt), list(shape), dt))

    def dram(self, name, shape, dt, kind="Internal"):
        return self.nc.dram_tensor(name, list(shape), dt, kind=kind).ap()

    def _wait(self, e, deps):
        best = {}
        for d in deps:
            if d is None:
                continue
            sem, val = d
            k = id(sem)
            if k not in best or best[k][1] < val:
                best[k] = (sem, val)
        for k, (sem, val) in best.items():
            if self.seen[e].get(k, 0) >= val:
                continue
            self.eng[e].wait_ge(sem, val)
            self.seen[e][k] = val

    def _deps(self, reads, writes):
        deps = []
        for r in reads:
            st = self.state.get(r)
            if st:
                deps.append(st[0])
        for w in writes:
            st = self.state.get(w)
            if st:
                deps.append(st[0])
                deps.extend(st[1])
        return deps

    def _commit(self, my, reads, writes):
        for r in reads:
            st = self.state.setdefault(r, [None, []])
            st[1].append(my)
            if len(st[1]) > 64:
                st[1] = st[1][-64:]
        for w in writes:
            self.state[w] = [my, []]

    def op(self, e, fn, reads=(), writes=()):
        self._wait(e, self._deps(reads, writes))
        ins = fn(self.eng[e])
        if self.cnt[e] >= self.SEM_ROLL:
            self._roll(e)
        self.cnt[e] += 1
        ins.then_inc(self.cur[e], 1)
        my = (self.cur[e], self.cnt[e])
        self._commit(my, reads, writes)
        return my

    def dma(self, q, out, in_, reads=(), writes=(), is_output=False, **kw):
        i = self.dnext[q]
        self.dnext[q] = (i + 1) % self.NDMA
        sem = self.dsem[q][i]
        deps = self._deps(reads, writes)
        if self.dcnt[q][i] > 0:
            deps.append((sem, 16 * self.dcnt[q][i]))
        self._wait(q, deps)
        ins = self.eng[q].dma_start(out=out, in_=in_, **kw)
        self.dcnt[q][i] += 1
        ins.then_inc(sem, 16)
        my = (sem, 16 * self.dcnt[q][i])
        self._commit(my, reads, writes)
        if is_output:
            self.out_deps.append(my)
        return my

    def finish(self):
        self.seen["sp"] = {}
        self._wait("sp", self.out_deps)
        last = [(self.cur[e], self.cnt[e]) for e in ("pe", "dve", "act", "pool") if self.cnt[e] > 0]
        self._wait("sp", last)

    def close(self):
        self.es.close()


def _kb_barrier(self):
    deps = [(self.cur[e], self.cnt[e]) for e in ("pe", "dve", "act", "pool") if self.cnt[e] > 0]
    for q in self.dsem:
        for i, sem in enumerate(self.dsem[q]):
            if self.dcnt[q][i] > 0:
                deps.append((sem, 16 * self.dcnt[q][i]))
    for e in self.eng:
        self._wait(e, deps)
    self.state.clear()


KB.barrier = _kb_barrier


class Phase:
    def __init__(self, k):
        self.k = k
        self.es = ExitStack()

    def sb(self, shape, dt):
        self.k.nt += 1
        return self.es.enter_context(self.k.nc.sbuf_tensor("t%d" % self.k.nt, list(shape), dt))

    def ps(self, shape, dt):
        self.k.nt += 1
        return self.es.enter_context(self.k.nc.psum_tensor("p%d" % self.k.nt, list(shape), dt))

    def end(self):
        self.k.barrier()
        self.es.close()


NCOL = 1536

def build_A():
    nc = bass.Bass("TRN2", target_bir_lowering=False)
    sT = nc.dram_tensor("sT", [D, 3], F32, kind="ExternalInput").ap()
    wa = nc.dram_tensor("wa", [2, D, NCOL], F32, kind="ExternalInput").ap()
    ba = nc.dram_tensor("ba", [2, NCOL], F32, kind="ExternalInput").ap()
    mod = nc.dram_tensor("mod", [2, 3, NCOL], F32, kind="ExternalOutput").ap()
    k = KB(nc)
    s_raw = k.sb([128, 16, 3], F32)
    s_act = k.sb([128, 16, 3], F32)
    wt = [k.sb([128, 16, 512], F32) for _ in range(2)]
    bt = [k.sb([3, 512], F32) for _ in range(2)]
    ot = [k.sb([3, 512], F32) for _ in range(2)]
    pt = [k.ps([3, 512], F32) for _ in range(2)]
    k.dma("sp", s_raw[:, :, :], sT.rearrange("(kc p) b -> p kc b", p=128), writes=[("s_raw", 0)])
    k.op("act", lambda e: e.activation(out=s_act[:, :, :], in_=s_raw[:, :, :], func=AF.Silu),
         reads=[("s_raw", 0)], writes=[("s_act", 0)])
    g = 0
    for l in range(2):
        for cb in range(3):
            b = g % 2
            k.dma("sp", wt[b][:, :, :], wa[l, :, cb * 512:(cb + 1) * 512].rearrange("(kc p) c -> p kc c", p=128),
                  writes=[("wt", b)])
            k.dma("sp", bt[b][:, :], ba[l:l + 1, cb * 512:(cb + 1) * 512].broadcast_to([3, 512]), writes=[("bt", b)])
            def mm(e, b=b):
                for kc in range(16):
                    ins = e.matmul(pt[b][:, :], s_act[:, kc, :], wt[b][:, kc, :], start=(kc == 0), stop=(kc == 15))
                return ins
            k.op("pe", mm, reads=[("s_act", 0), ("wt", b)], writes=[("pt", b)])
            k.op("dve", lambda e, b=b: e.tensor_tensor(out=ot[b][:, :], in0=pt[b][:, :], in1=bt[b][:, :], op=ALU.add),
                 reads=[("pt", b), ("bt", b)], writes=[("ot", b)])
            k.dma("sp", mod[l, :, cb * 512:(cb + 1) * 512], ot[b][:, :], reads=[("ot", b)], is_output=True)
            g += 1
    k.finish()
    k.close()
    return nc


D = 2048
NE = 2560
NO = 2048
OWN0 = 256
NCX = 256
NT = NE + NCX
NQ = NO + NCX
EPS = 1e-6
SCALE = 128 ** -0.5
NEGB = -30000.0
CT0 = NE + 15
VGW = NE + 15 + NCX + 15


def build_B(last):
    nc = bass.Bass("TRN2", target_bir_lowering=False)

    def din(name, shape, dt=F32):
        return nc.dram_tensor(name, list(shape), dt, kind="ExternalInput").ap()

    def dout(name, shape, dt=F32):
        return nc.dram_tensor(name, list(shape), dt, kind="ExternalOutput").ap()

    xe = din("xe", [NE, D])
    cx = din("cx", [NCX, D])
    modv = din("modv", [2, 6 * D])
    gmf = din("gmf", [2, D])
    win = din("win", [40, 128, 16 * 128])
    wout = din("wout", [128, 16 * D])
    chp = din("chp", [128, 8 * 34])
    gqk = din("gqk", [128, 2])
    cs = din("cs", [2, 128, NE])
    rmt = din("rmt", [128, 128])
    idn = din("idn", [128, 128])
    vmask = din("vmask", [1, NE])
    btab = din("btab", [8, 128, 5 * 896])
    wr = din("wr", [128, 16 * 32])
    br = din("br", [1, 32])
    x_mid = dout("x_mid", [NO, D])
    G_o = dout("G_o", [NO, 32])
    h2_o = dout("h2_o", [NO, D], BF16)
    if not last:
        c_mid = dout("c_mid", [NCX, D])
        Gc_o = dout("Gc_o", [NCX, 32])
        h2c_o = dout("h2c_o", [NCX, D], BF16)

    k = KB(nc)
    conv_d = k.dram("conv_d", [8, 128, NQ], F32)
    oT_d = k.dram("oT_d", [16, 128, NQ], BF16)

    ident_f = k.sb([128, 128], F32)
    ident_b = k.sb([128, 128], BF16)
    ones_f = k.sb([128, 128], F32)
    ones_b = k.sb([128, 128], BF16)
    rmt_s = k.sb([128, 128], F32)
    chp_s = k.sb([128, 8, 34], F32)
    gqk_s = k.sb([128, 2], F32)
    big = k.sb([128, 16 * NT], BF16)
    hT = big[:, :].rearrange("p (kc t) -> p kc t", kc=16)
    k.dma("sp", ident_f[:, :], idn, writes=[("c", 0)])
    k.dma("sp", rmt_s[:, :], rmt, writes=[("c", 1)])
    k.dma("sp", chp_s[:, :, :], chp.rearrange("p (c j) -> p c j", j=34), writes=[("c", 2)])
    k.dma("sp", gqk_s[:, :], gqk, writes=[("c", 3)])
    k.op("dve", lambda e: e.tensor_copy(out=ident_b[:, :], in_=ident_f[:, :]), reads=[("c", 0)], writes=[("c", 4)])
    k.op("dve", lambda e: e.memset(ones_f[:, :], 1.0), writes=[("c", 5)])
    k.op("dve", lambda e: e.memset(ones_b[:, :], 1.0), writes=[("c", 6)])
    eps_s = k.sb([128, 1], F32)
    k.op("dve", lambda e: e.memset(eps_s[:, :], EPS), writes=[("c", 7)])
    CONST = [("c", i) for i in range(8)]

    def rstd_ops(e_sum_ap, out_ap, n, deps_r, deps_w):
        k.op("act", lambda e: e.activation(out=out_ap, in_=e_sum_ap, func=AF.Sqrt, bias=eps_s[:, 0:1], scale=1.0 / n),
             reads=list(deps_r) + CONST, writes=deps_w)
        k.op("dve", lambda e: e.reciprocal(out=out_ap, in_=out_ap), reads=deps_w, writes=deps_w)

    ph = Phase(k)
    rows = [ph.sb([128, D], F32) for _ in range(5)]
    xt = [ph.sb([128, D], F32) for _ in range(2)]
    sqs = [ph.sb([128, D], F32) for _ in range(2)]
    tmpfs = [ph.sb([128, D], F32) for _ in range(2)]
    hb = [ph.sb([128, D], BF16) for _ in range(2)]
    ss = [ph.sb([128, 2], F32) for _ in range(2)]
    ptr = [ph.ps([128, 1024], F32) for _ in range(2)]

    def bc(ap_row):
        return ap_row.broadcast_to([128, D])

    k.dma("sp", rows[4][:, :], bc(gmf[0:1, :]), writes=[("row", 4)])
    for j, r in enumerate((0, 1)):
        k.dma("sp", rows[2 * j][:, :], bc(modv[r:r + 1, D:2 * D]), writes=[("row", 2 * j)])
        k.dma("sp", rows[2 * j + 1][:, :], bc(modv[r:r + 1, 0:D]), writes=[("row", 2 * j + 1)])
        k.op("dve", lambda e, j=j: e.scalar_tensor_tensor(out=rows[2 * j][:, :], in0=rows[2 * j][:, :], scalar=1.0,
                                                          in1=rows[4][:, :], op0=ALU.add, op1=ALU.mult),
             reads=[("row", 4)], writes=[("row", 2 * j)])

    tiles0 = [(xe, t, t * 128, 0) for t in range(NE // 128)] + [(cx, t, NE + t * 128, 1) for t in range(NCX // 128)]

    def load_x(i):
        src, t, col, which = tiles0[i]
        k.dma("sp", xt[i % 2][:, :], src[t * 128:(t + 1) * 128, :], writes=[("xt", i % 2)])

    load_x(0)
    for i, (src, t, col, which) in enumerate(tiles0):
        b = i % 2
        sq, tmpf = sqs[b], tmpfs[b]
        if i + 1 < len(tiles0):
            load_x(i + 1)
        k.op("act", lambda e, b=b: e.activation(out=sq[:, :], in_=xt[b][:, :], func=AF.Square),
             reads=[("xt", b)], writes=[("sq", b)])
        k.op("dve", lambda e, b=b: e.tensor_reduce(out=ss[b][:, 0:1], in_=sq[:, :], axis=AX.X, op=ALU.add),
             reads=[("sq", b)], writes=[("ss", b)])
        rstd_ops(ss[b][:, 0:1], ss[b][:, 1:2], D, [("ss", b)], [("ss", b)])
        A, Bv = rows[2 * which], rows[2 * which + 1]
        k.op("dve", lambda e, b=b, A=A: e.scalar_tensor_tensor(out=tmpf[:, :], in0=xt[b][:, :], scalar=ss[b][:, 1:2],
                                                               in1=A[:, :], op0=ALU.mult, op1=ALU.mult),
             reads=[("xt", b), ("ss", b), ("row", 2 * which)], writes=[("tmpf", b)])
        k.op("pool", lambda e, b=b, Bv=Bv: e.tensor_tensor(out=hb[b][:, :], in0=tmpf[:, :], in1=Bv[:, :], op=ALU.add),
             reads=[("tmpf", b), ("row", 2 * which + 1)], writes=[("hb", b)])
        pv = ptr[b][:, :].bitcast(BF16)

        def tr(e, b=b, pv=pv):
            for kc in range(16):
                ins = e.transpose(pv[:, kc * 128:(kc + 1) * 128], hb[b][:, kc * 128:(kc + 1) * 128], ident_b[:, :])
            return ins
        k.op("pe", tr, reads=[("hb", b)] + CONST, writes=[("ptr", b)])
        k.op("act", lambda e, pv=pv, col=col: e.copy(out=hT[:, :, col:col + 128],
                                                     in_=pv.rearrange("p (kc t) -> p kc t", kc=16)),
             reads=[("ptr", b)], writes=[("hT", col // 128)])
    ph.end()
    HT_ALL = [("hT", i) for i in range(NT // 128)]

    def load_w(wb, chunk, slot):
        k.dma("pool", wb[slot][:, :, :], win[chunk].rearrange("p (kc n) -> p kc n", kc=16), writes=[("wb", slot)])

    def mm_fm(ps_ap, wb, slot, c0, n, ps_key):
        def f(e):
            for kc in range(16):
                ins = e.matmul(ps_ap, wb[slot][:, kc, :], hT[:, kc, c0:c0 + n], start=(kc == 0), stop=(kc == 15))
            return ins
        k.op("pe", f, reads=[("wb", slot)] + [("hT", i) for i in range(c0 // 128, (c0 + n) // 128)], writes=[ps_key])

    EXT_BLOCKS = [(i * 512, 512) for i in range(5)]
    OWN_BLOCKS = [(OWN0 + i * 512, 512) for i in range(4)]
    CTX_BLOCK = [(NE, NCX)]

    ph = Phase(k)
    wb = [ph.sb([128, 16, 128], BF16) for _ in range(4)]
    vg = [ph.sb([128, VGW], BF16) for _ in range(2)]
    dg = [ph.sb([128, 31, 128], BF16) for _ in range(2)]
    acc = [ph.sb([128, NQ], F32) for _ in range(2)]
    sig = [ph.sb([128, 512], F32) for _ in range(2)]
    vm = ph.sb([128, NE], F32)
    pa = [ph.ps([128, 512], F32) for _ in range(2)]
    pg = [ph.ps([128, 512], F32) for _ in range(2)]
    pc = [ph.ps([128, 512], F32) for _ in range(2)]
    k.dma("sp", vm[:, :], vmask.broadcast_to([128, NE]), writes=[("vm", 0)])
    for b in range(2):
        k.op("pool", lambda e, b=b: e.memset(vg[b][:, NE:VGW], 0.0), writes=[("vg", b)])
    blocks = EXT_BLOCKS + ([] if last else CTX_BLOCK)
    nq = NO if last else NQ
    oblocks = [(i * 512, 512, OWN0 + i * 512) for i in range(NO // 512)] + ([] if last else [(NO, NCX, CT0)])
    ci_ = [0]

    def conv_cc(cc):
        vb = cc % 2
        for (o0, n, v0) in oblocks:
            pb = ci_[0] % 2
            ci_[0] += 1

            def cm(e):
                for j in range(31):
                    ins = e.matmul(pc[pb][:, :n], dg[vb][:, j, :], vg[vb][:, v0 + j - 15:v0 + j - 15 + n],
                                   start=(j == 0), stop=(j == 30))
                return ins
            k.op("pe", cm, reads=[("vg", vb), ("dg", vb)], writes=[("pc", pb)])
            k.op("act", lambda e: e.activation(out=acc[vb][:, o0:o0 + n], in_=pc[pb][:, :n], func=AF.Identity,
                                               bias=chp_s[:, cc, 31:32], scale=1.0),
                 reads=[("pc", pb)] + CONST, writes=[("acc", vb, o0)])
        k.dma("sp", conv_d[cc, :, 0:nq], acc[vb][:, 0:nq], reads=[("acc", vb, o0) for (o0, _, _) in oblocks],
              writes=[("conv_d", cc)])

    load_w(wb, 0, 0)
    load_w(wb, 8, 1)
    it = 0
    for cc in range(8):
        sa, sg = (2 * cc) % 4, (2 * cc + 1) % 4
        if cc + 1 < 8:
            load_w(wb, cc + 1, (2 * cc + 2) % 4)
            load_w(wb, 8 + cc + 1, (2 * cc + 3) % 4)
        vb = cc % 2
        k.op("dve", lambda e: e.tensor_tensor(out=dg[vb][:, :, :], in0=ident_b[:, :].unsqueeze(1).broadcast_to([128, 31, 128]),
                                              in1=chp_s[:, cc, 0:31].unsqueeze(2).broadcast_to([128, 31, 128]), op=ALU.mult),
             reads=CONST, writes=[("dg", vb)])
        for (c0, n) in blocks:
            pb = it % 2
            it += 1
            mm_fm(pa[pb][:, :n], wb, sa, c0, n, ("pa", pb))
            mm_fm(pg[pb][:, :n], wb, sg, c0, n, ("pg", pb))
            k.op("act", lambda e: e.activation(out=sig[pb][:, :n], in_=pg[pb][:, :n], func=AF.Sigmoid),
                 reads=[("pg", pb)], writes=[("sig", pb)])
            if c0 < NE:
                k.op("pool", lambda e: e.tensor_tensor(out=sig[pb][:, :n], in0=sig[pb][:, :n], in1=vm[:, c0:c0 + n], op=ALU.mult),
                     reads=[("vm", 0)], writes=[("sig", pb)])
                dst = vg[vb][:, c0:c0 + n]
            else:
                dst = vg[vb][:, CT0:CT0 + n]
            k.op("dve", lambda e: e.tensor_tensor(out=dst, in0=pa[pb][:, :n], in1=sig[pb][:, :n], op=ALU.mult),
                 reads=[("pa", pb), ("sig", pb)], writes=[("vg", vb)])
        if cc >= 1:
            conv_cc(cc - 1)
    conv_cc(7)
    ph.end()

    ph = Phase(k)
    wb = [ph.sb([128, 16, 128], BF16) for _ in range(3)]
    cs_s = ph.sb([128, 2, NE], F32)
    qT = ph.sb([128, NQ], BF16)
    kT = ph.sb([128, NT], BF16)
    Vh = ph.sb([128, NT // 128, 128], BF16)
    bt = ph.sb([128, 5, 896], F32)
    sqe2 = [ph.sb([128, 512], F32) for _ in range(2)]
    rs2 = [ph.sb([128, 512], F32) for _ in range(2)]
    xn2 = [ph.sb([128, 512], F32) for _ in range(2)]
    t12 = [ph.sb([128, 512], F32) for _ in range(2)]
    t22 = [ph.sb([128, 512], F32) for _ in range(2)]
    stmps = [ph.sb([128, 768], F32) for _ in range(2)]
    PT = [ph.sb([128, 8 * 128], BF16) for _ in range(2)]
    rec = ph.sb([128, 128], F32)
    oTh = ph.sb([128, NQ], BF16)
    arena = ph.ps([128, 4096], F32)

    def bank(i, n=1):
        return arena[:, i * 512:(i + n) * 512]
    pj = [bank(0), bank(1), bank(2)]
    psr2 = [bank(3), bank(4)]
    po = bank(5)
    pstv = [bank(6, 2), bank(0, 2)]
    pstk = [[("bk", 6), ("bk", 7)], [("bk", 0), ("bk", 1)]]
    k.dma("sp", cs_s[:, 0, :], cs[0], writes=[("cs", 0)])
    k.dma("sp", cs_s[:, 1, :], cs[1], writes=[("cs", 0)])
    pji = [0]

    def stageA(i, blk):
        wslot, c0, n, gcol, dst_ap, dst_key, rope_c0 = blk
        pb = i % 3
        mm_fm(pj[pb][:, :n], wb, wslot, c0, n, ("bk", pb))

    def stageB(i, blk):
        wslot, c0, n, gcol, dst_ap, dst_key, rope_c0 = blk
        pb, tb = i % 3, i % 2
        X = pj[pb][:, :n]
        sqe, rs, xn, psr = sqe2[tb], rs2[tb], xn2[tb], psr2[tb]
        k.op("act", lambda e: e.activation(out=sqe[:, :n], in_=X, func=AF.Square), reads=[("bk", pb)], writes=[("sqe", tb)])
        k.op("pe", lambda e: e.matmul(psr[:, :n], ones_f[:, :], sqe[:, :n], start=True, stop=True),
             reads=[("sqe", tb)] + CONST, writes=[("bk", 3 + tb)])
        rstd_ops(psr[:, :n], rs[:, :n], 128, [("bk", 3 + tb)], [("rs", tb)])
        k.op("dve", lambda e: e.scalar_tensor_tensor(out=xn[:, :n], in0=X, scalar=gqk_s[:, gcol:gcol + 1], in1=rs[:, :n],
                                                     op0=ALU.mult, op1=ALU.mult),
             reads=[("bk", pb), ("rs", tb)] + CONST, writes=[("xn", tb)])

    def stageC(i, blk):
        wslot, c0, n, gcol, dst_ap, dst_key, rope_c0 = blk
        tb = i % 2
        rs, xn, t1, t2, psr = rs2[tb], xn2[tb], t12[tb], t22[tb], psr2[tb]
        if rope_c0 is None:
            k.op("act", lambda e: e.copy(out=dst_ap, in_=xn[:, :n]), reads=[("xn", tb)], writes=[dst_key])
            return
        k.op("pe", lambda e: e.matmul(psr[:, :n], rmt_s[:, :], xn[:, :n], start=True, stop=True),
             reads=[("xn", tb), ("rs", tb)] + CONST, writes=[("bk", 3 + tb)])
        k.op("pool", lambda e: e.tensor_tensor(out=t1[:, :n], in0=xn[:, :n], in1=cs_s[:, 0, rope_c0:rope_c0 + n], op=ALU.mult),
             reads=[("xn", tb), ("cs", 0)], writes=[("t1", tb)])
        k.op("dve", lambda e: e.tensor_tensor(out=t2[:, :n], in0=psr[:, :n], in1=cs_s[:, 1, rope_c0:rope_c0 + n], op=ALU.mult),
             reads=[("bk", 3 + tb), ("cs", 0)], writes=[("t2", tb)])
        k.op("dve", lambda e: e.tensor_tensor(out=dst_ap, in0=t1[:, :n], in1=t2[:, :n], op=ALU.add),
             reads=[("t1", tb), ("t2", tb)], writes=[dst_key])

    def qk_pipeline(blocks):
        base = pji[0]
        N = len(blocks)
        for s_ in range(N + 2):
            if s_ < N:
                stageA(base + s_, blocks[s_])
            if 0 <= s_ - 1 < N:
                stageB(base + s_ - 1, blocks[s_ - 1])
            if 0 <= s_ - 2 < N:
                stageC(base + s_ - 2, blocks[s_ - 2])
        pji[0] = base + N

    def cls(qt):
        return {2: 0, 3: 1, 16: 3, 17: 4}.get(qt, 2)

    def att_sc(job, pti):
        qcol, slots, bias_cls, o_lo, o_hi = job
        pst = pstv[pti]

        def sc(e):
            for (s, kt) in slots:
                ins = e.matmul(pst[:, s * 128:(s + 1) * 128], kT[:, kt * 128:(kt + 1) * 128], qT[:, qcol:qcol + 128],
                               start=True, stop=True)
            return ins
        k.op("pe", sc, reads=[("kT", 0), ("qT", 0)], writes=pstk[pti])

    def att_rest(job, pti):
        qcol, slots, bias_cls, o_lo, o_hi = job
        pst, P, stmp = pstv[pti], PT[pti], stmps[pti]
        if bias_cls is not None:
            w_ = (o_hi - o_lo) * 128
            k.op("dve", lambda e: e.scalar_tensor_tensor(out=stmp[:, 0:w_], in0=pst[:, 0:w_], scalar=SCALE,
                                                         in1=bt[:, bias_cls, o_lo * 128:o_hi * 128], op0=ALU.mult, op1=ALU.add),
                 reads=pstk[pti] + [("bt", 0)], writes=[("stmp", pti)])
            k.op("act", lambda e: e.activation(out=P[:, 0:w_], in_=stmp[:, 0:w_], func=AF.Exp),
                 reads=[("stmp", pti)], writes=[("PT", pti)])
        k.op("act", lambda e: e.activation(out=P[:, 768:1024], in_=pst[:, 768:1024], func=AF.Exp, scale=SCALE),
             reads=pstk[pti], writes=[("PT", pti)])

        def pv(e):
            for i, (s, kt) in enumerate(slots):
                e.matmul(po[:, 0:128], Vh[:, kt, :], P[:, s * 128:(s + 1) * 128], start=(i == 0), stop=(i == len(slots) - 1))
            for i, (s, kt) in enumerate(slots):
                ins = e.matmul(po[:, 128:256], ones_b[:, :], P[:, s * 128:(s + 1) * 128], start=(i == 0),
                               stop=(i == len(slots) - 1))
            return ins
        k.op("pe", pv, reads=[("PT", pti), ("Vh", 0)] + CONST, writes=[("bk", 5)])
        k.op("dve", lambda e: e.reciprocal(out=rec[:, :], in_=po[:, 128:256]), reads=[("bk", 5)], writes=[("rec", 0)])
        k.op("dve", lambda e: e.tensor_tensor(out=oTh[:, qcol:qcol + 128], in0=po[:, 0:128], in1=rec[:, :], op=ALU.mult),
             reads=[("bk", 5), ("rec", 0)], writes=[("oTh", 0)])

    load_w(wb, 16, 0)
    wi = 0
    for h in range(8):
        chunks = [16 + h, 24 + h, 32 + h]
        nxt = chunks[1:] + ([16 + h + 1] if h < 7 else [])
        k.dma("sp", bt[:, :, :], btab[h].rearrange("p (c n) -> p c n", c=5), writes=[("bt", 0)])
        sq_, wi = wi % 3, wi + 1
        load_w(wb, nxt[0], wi % 3)
        sk_, wi = wi % 3, wi + 1
        load_w(wb, nxt[1], wi % 3)
        blocks = [(sq_, c0, n, 0, qT[:, i * 512:i * 512 + n], ("qT", 0), c0) for i, (c0, n) in enumerate(OWN_BLOCKS)]
        if not last:
            blocks.append((sq_, NE, NCX, 0, qT[:, NO:NQ], ("qT", 0), None))
        blocks += [(sk_, c0, n, 1, kT[:, c0:c0 + n], ("kT", 0), c0) for (c0, n) in EXT_BLOCKS]
        blocks.append((sk_, NE, NCX, 1, kT[:, NE:NT], ("kT", 0), None))
        qk_pipeline(blocks)
        sv_, wi = wi % 3, wi + 1
        if len(nxt) > 2:
            load_w(wb, nxt[2], wi % 3)
        for g0 in range(0, NT // 128, 4):
            gn = min(4, NT // 128 - g0)
            pb = pji[0] % 3
            pji[0] += 1

            def vmm(e, g0=g0, gn=gn, pb=pb):
                for t in range(gn):
                    col = (g0 + t) * 128
                    for kc in range(16):
                        ins = e.matmul(pj[pb][:, t * 128:(t + 1) * 128], hT[:, kc, col:col + 128], wb[sv_][:, kc, :],
                                       start=(kc == 0), stop=(kc == 15))
                return ins
            k.op("pe", vmm, reads=[("wb", sv_)] + [("hT", g0 + t) for t in range(gn)], writes=[("bk", pb)])
            k.op("act", lambda e, g0=g0, gn=gn, pb=pb: e.copy(out=Vh[:, g0:g0 + gn, :],
                                                             in_=pj[pb][:, :gn * 128].rearrange("p (t d) -> p t d", d=128)),
                 reads=[("bk", pb)], writes=[("Vh", 0)])
        jobs = []
        for qt in range(2, 18):
            o_lo = 0 if qt == 17 else 1
            o_hi = 7 if qt == 2 else 6
            slots = [(o - o_lo, qt - 3 + o) for o in range(o_lo, o_hi)]
            jobs.append(((qt - 2) * 128, slots + [(6, 20), (7, 21)], cls(qt), o_lo, o_hi))
        if not last:
            for t in range(2):
                jobs.append((NO + t * 128, [(6, 20), (7, 21)], None, 0, 0))
        att_sc(jobs[0], 0)
        for n_, job in enumerate(jobs):
            if n_ + 1 < len(jobs):
                att_sc(jobs[n_ + 1], (n_ + 1) % 2)
            att_rest(job, n_ % 2)
        nq = NO if last else NQ
        k.dma("sp", oT_d[8 + h, :, 0:nq], oTh[:, 0:nq], reads=[("oTh", 0)], writes=[("oT_d", 8 + h)])
    ph.end()

    ph = Phase(k)
    cv = [ph.sb([128, 8, 512], F32) for _ in range(2)]
    cq = ph.sb([128, 8, 512], F32)
    mean = ph.sb([128, 512], F32)
    var = ph.sb([128, 512], F32)
    d1 = [ph.sb([128, 512], F32) for _ in range(2)]
    ob = [ph.sb([128, 8, 512], BF16) for _ in range(2)]
    p1 = ph.ps([128, 512], F32)
    p2 = ph.ps([128, 512], F32)
    lnblocks = [(i * 512, 512) for i in range(4)] + ([] if last else [(NO, NCX)])

    def load_cv(i):
        c0, n = lnblocks[i]
        k.dma("sp", cv[i % 2][:, :, :n], conv_d[:, :, c0:c0 + n].rearrange("c p t -> p c t"),
              reads=[("conv_d", c) for c in range(8)], writes=[("cv", i % 2)])
    load_cv(0)
    for i, (c0, n) in enumerate(lnblocks):
        b = i % 2
        if i + 1 < len(lnblocks):
            load_cv(i + 1)
        k.op("act", lambda e, b=b, n=n: e.activation(out=cq[:, :, :n], in_=cv[b][:, :, :n], func=AF.Square),
             reads=[("cv", b)], writes=[("cq", 0)])

        def s1(e, b=b, n=n):
            for c in range(8):
                ins = e.matmul(p1[:, :n], ones_f[:, :], cv[b][:, c, :n], start=(c == 0), stop=(c == 7))
            return ins

        def s2(e, n=n):
            for c in range(8):
                ins = e.matmul(p2[:, :n], ones_f[:, :], cq[:, c, :n], start=(c == 0), stop=(c == 7))
            return ins
        k.op("pe", s1, reads=[("cv", b)] + CONST, writes=[("p1", 0)])
        k.op("pe", s2, reads=[("cq", 0)] + CONST, writes=[("p2", 0)])
        k.op("dve", lambda e, n=n: e.tensor_scalar(out=mean[:, :n], in0=p1[:, :n], scalar1=1.0 / 1024, scalar2=None,
                                                   op0=ALU.mult), reads=[("p1", 0)], writes=[("mean", 0)])
        k.op("dve", lambda e, n=n: e.tensor_tensor(out=var[:, :n], in0=mean[:, :n], in1=mean[:, :n], op=ALU.mult),
             reads=[("mean", 0)], writes=[("var", 0)])
        k.op("dve", lambda e, n=n: e.scalar_tensor_tensor(out=var[:, :n], in0=p2[:, :n], scalar=1.0 / 1024, in1=var[:, :n],
                                                          op0=ALU.mult, op1=ALU.subtract),
             reads=[("p2", 0), ("var", 0)], writes=[("var", 0)])
        k.op("act", lambda e, n=n: e.activation(out=var[:, :n], in_=var[:, :n], func=AF.Sqrt, bias=eps_s[:, 0:1], scale=1.0),
             reads=[("var", 0)] + CONST, writes=[("var", 0)])
        k.op("dve", lambda e, n=n: e.reciprocal(out=var[:, :n], in_=var[:, :n]), reads=[("var", 0)], writes=[("var", 0)])
        for c in range(8):
            db = c % 2
            eng = "dve" if c % 2 == 0 else "pool"
            k.op(eng, lambda e, b=b, c=c, n=n, db=db: e.tensor_tensor(out=d1[db][:, :n], in0=cv[b][:, c, :n], in1=mean[:, :n],
                                                                     op=ALU.subtract),
                 reads=[("cv", b), ("mean", 0)], writes=[("d1", db)])
            k.op(eng, lambda e, n=n, db=db: e.tensor_tensor(out=d1[db][:, :n], in0=d1[db][:, :n], in1=var[:, :n], op=ALU.mult),
                 reads=[("var", 0), ("d1", db)], writes=[("d1", db)])
            k.op("act", lambda e, b=b, c=c, n=n, db=db: e.activation(out=ob[b][:, c, :n], in_=d1[db][:, :n], func=AF.Silu,
                                                                    bias=chp_s[:, c, 33:34], scale=chp_s[:, c, 32:33]),
                 reads=[("d1", db)] + CONST, writes=[("ob", b)])
        k.dma("sp", oT_d[0:8, :, c0:c0 + n].rearrange("c p t -> p c t"), ob[b][:, :, :n], reads=[("ob", b)],
              writes=[("oT_d", c) for c in range(8)])
    ph.end()

    ph = Phase(k)
    wo = big[:, 0:16 * D].rearrange("p (kc n) -> p kc n", kc=16)
    rows = [ph.sb([128, D], F32) for _ in range(4)]
    xt = [ph.sb([128, D], F32) for _ in range(2)]
    oTt = [ph.sb([128, 16, 128], BF16) for _ in range(2)]
    xm2 = [ph.sb([128, D], F32) for _ in range(2)]
    h2f2 = [ph.sb([128, D], F32) for _ in range(2)]
    h2b2 = [ph.sb([128, D], BF16) for _ in range(2)]
    h2T2 = [ph.sb([128, 16, 128], F32) for _ in range(2)]
    ss = ph.sb([128, 2], F32)
    wr_s = ph.sb([128, 16, 32], F32)
    br_s = ph.sb([128, 32], F32)
    lg = ph.sb([128, 32], F32)
    m8 = ph.sb([128, 8], F32)
    negm = ph.sb([128, 1], F32)
    msk = ph.sb([128, 32], F32)
    ex = ph.sb([128, 32], F32)
    gs = ph.sb([128, 2], F32)
    Gt = ph.sb([128, 32], F32)
    pw = ph.ps([128, D], F32)
    pt2 = ph.ps([128, 1024], F32)
    pl = ph.ps([128, 32], F32)
    k.dma("pool", wo, wout.rearrange("p (kc n) -> p kc n", kc=16), writes=[("wo", 0)])
    k.dma("sp", wr_s[:, :, :], wr.rearrange("p (kc n) -> p kc n", kc=16), writes=[("wr", 0)])
    k.dma("sp", br_s[:, :], br.broadcast_to([128, 32]), writes=[("wr", 1)])

    def load_rows(r):
        k.dma("sp", rows[0][:, :], bc(modv[r:r + 1, 2 * D:3 * D]), writes=[("row", 0)])
        k.dma("sp", rows[3][:, :], bc(gmf[1:2, :]), writes=[("row", 3)])
        k.dma("sp", rows[1][:, :], bc(modv[r:r + 1, 4 * D:5 * D]), writes=[("row", 1)])
        k.dma("sp", rows[2][:, :], bc(modv[r:r + 1, 3 * D:4 * D]), writes=[("row", 2)])
        k.op("dve", lambda e: e.scalar_tensor_tensor(out=rows[1][:, :], in0=rows[1][:, :], scalar=1.0, in1=rows[3][:, :],
                                                     op0=ALU.add, op1=ALU.mult), reads=[("row", 3)], writes=[("row", 1)])

    tiles4 = [(xe, OWN0 // 128 + t, t * 128, 0, x_mid, G_o, h2_o, t) for t in range(NO // 128)]
    if not last:
        tiles4 += [(cx, t, NO + t * 128, 1, c_mid, Gc_o, h2c_o, t) for t in range(NCX // 128)]

    def load4(i):
        src, st, col, which, _, _, _, _ = tiles4[i]
        k.dma("sp", xt[i % 2][:, :], src[st * 128:(st + 1) * 128, :], writes=[("xt", i % 2)])
        k.dma("sp", oTt[i % 2][:, :, :], oT_d[:, :, col:col + 128].rearrange("c p t -> p c t"),
              reads=[("oT_d", c) for c in range(16)], writes=[("oTt", i % 2)])

    def tile_vars(i):
        b = i % 2
        return b, xm2[b], h2f2[b], h2b2[b], h2T2[b]

    def emit_mo(i):
        b = i % 2
        def mo(e, b=b):
            for nb in range(4):
                for kc in range(16):
                    ins = e.matmul(pw[:, nb * 512:(nb + 1) * 512], oTt[b][:, kc, :], wo[:, kc, nb * 512:(nb + 1) * 512],
                                   start=(kc == 0), stop=(kc == 15))
            return ins
        k.op("pe", mo, reads=[("oTt", b), ("wo", 0)], writes=[("pw", 0)])

    def part1(i):
        src, st, col, which, xo, Go, ho, ot = tiles4[i]
        b, xm, h2f, h2b, h2T = tile_vars(i)
        sq = h2f
        k.op("dve", lambda e: e.tensor_tensor(out=xm[:, :], in0=pw[:, :], in1=rows[0][:, :], op=ALU.mult),
             reads=[("pw", 0), ("row", 0)], writes=[("xm", b)])
        k.op("pool", lambda e, b=b: e.tensor_tensor(out=xm[:, :], in0=xm[:, :], in1=xt[b][:, :], op=ALU.add),
             reads=[("xt", b), ("xm", b)], writes=[("xm", b)])
        k.dma("sp", xo[ot * 128:(ot + 1) * 128, :], xm[:, :], reads=[("xm", b)], is_output=True)
        k.op("act", lambda e: e.activation(out=sq[:, :], in_=xm[:, :], func=AF.Square), reads=[("xm", b)], writes=[("h2f", b)])
        k.op("dve", lambda e: e.tensor_reduce(out=ss[:, 0:1], in_=sq[:, :], axis=AX.X, op=ALU.add),
             reads=[("h2f", b)], writes=[("ss", 0)])
        rstd_ops(ss[:, 0:1], ss[:, 1:2], D, [("ss", 0)], [("ss", 0)])
        k.op("dve", lambda e: e.scalar_tensor_tensor(out=h2f[:, :], in0=xm[:, :], scalar=ss[:, 1:2], in1=rows[1][:, :],
                                                     op0=ALU.mult, op1=ALU.mult),
             reads=[("xm", b), ("ss", 0), ("row", 1)], writes=[("h2f", b)])
        k.op("pool", lambda e: e.tensor_tensor(out=h2f[:, :], in0=h2f[:, :], in1=rows[2][:, :], op=ALU.add),
             reads=[("h2f", b), ("row", 2)], writes=[("h2f", b)])
        k.op("act", lambda e: e.copy(out=h2b[:, :], in_=h2f[:, :]), reads=[("h2f", b)], writes=[("h2b", b)])
        k.dma("sp", ho[ot * 128:(ot + 1) * 128, :], h2b[:, :], reads=[("h2b", b)], is_output=True)

    def part2(i):
        src, st, col, which, xo, Go, ho, ot = tiles4[i]
        b, xm, h2f, h2b, h2T = tile_vars(i)
        for half in range(2):
            def trf(e, half=half):
                for j in range(8):
                    kc = half * 8 + j
                    ins = e.transpose(pt2[:, j * 128:(j + 1) * 128], h2f[:, kc * 128:(kc + 1) * 128], ident_f[:, :])
                return ins
            k.op("pe", trf, reads=[("h2f", b)] + CONST, writes=[("pt2", 0)])
            k.op("act", lambda e, half=half: e.copy(out=h2T[:, half * 8:(half + 1) * 8, :],
                                                   in_=pt2[:, :].rearrange("p (j t) -> p j t", j=8)),
                 reads=[("pt2", 0)], writes=[("h2T", b, half)])

        def rl(e):
            for kc in range(16):
                ins = e.matmul(pl[:, :], h2T[:, kc, :], wr_s[:, kc, :], start=(kc == 0), stop=(kc == 15))
            return ins
        k.op("pe", rl, reads=[("h2T", b, 0), ("h2T", b, 1), ("wr", 0)], writes=[("pl", 0)])
        k.op("dve", lambda e: e.tensor_tensor(out=lg[:, :], in0=pl[:, :], in1=br_s[:, :], op=ALU.add),
             reads=[("pl", 0), ("wr", 1)], writes=[("lg", 0)])
        k.op("dve", lambda e: e.max(out=m8[:, :], in_=lg[:, :]), reads=[("lg", 0)], writes=[("m8", 0)])
        k.op("dve", lambda e: e.tensor_scalar(out=negm[:, :], in0=m8[:, 0:1], scalar1=-1.0, scalar2=None, op0=ALU.mult),
             reads=[("m8", 0)], writes=[("negm", 0)])
        k.op("dve", lambda e: e.tensor_scalar(out=msk[:, :], in0=lg[:, :], scalar1=m8[:, 3:4], scalar2=None, op0=ALU.is_ge),
             reads=[("lg", 0), ("m8", 0)], writes=[("msk", 0)])
        k.op("act", lambda e: e.activation(out=ex[:, :], in_=lg[:, :], func=AF.Exp, bias=negm[:, 0:1], scale=1.0),
             reads=[("lg", 0), ("negm", 0)], writes=[("ex", 0)])
        k.op("dve", lambda e: e.tensor_tensor(out=ex[:, :], in0=ex[:, :], in1=msk[:, :], op=ALU.mult),
             reads=[("ex", 0), ("msk", 0)], writes=[("ex", 0)])
        k.op("dve", lambda e: e.tensor_reduce(out=gs[:, 0:1], in_=ex[:, :], axis=AX.X, op=ALU.add),
             reads=[("ex", 0)], writes=[("gs", 0)])
        k.op("dve", lambda e: e.reciprocal(out=gs[:, 1:2], in_=gs[:, 0:1]), reads=[("gs", 0)], writes=[("gs", 0)])
        k.op("dve", lambda e: e.tensor_scalar(out=Gt[:, :], in0=ex[:, :], scalar1=gs[:, 1:2], scalar2=None, op0=ALU.mult),
             reads=[("ex", 0), ("gs", 0)], writes=[("Gt", 0)])
        k.dma("sp", Go[ot * 128:(ot + 1) * 128, :], Gt[:, :], reads=[("Gt", 0)], is_output=True)

    load_rows(0)
    load4(0)
    emit_mo(0)
    for i in range(len(tiles4)):
        if tiles4[i][3] == 1 and tiles4[i - 1][3] == 0:
            load_rows(1)
        if i + 1 < len(tiles4):
            load4(i + 1)
        part1(i)
        if i + 1 < len(tiles4):
            emit_mo(i + 1)
        part2(i)
    k.finish()
    ph.es.close()
    k.close()
    return nc


ALPHA = 1.702
LIM = 7.0


def split_tiles(ntl, mx):
    out, t = [], 0
    while t < ntl:
        n = min(mx, ntl - t)
        out.append((t, n))
        t += n
    return out


def build_C(caps):
    nc = bass.Bass("TRN2", target_bir_lowering=False)

    def din(name, shape, dt=F32):
        return nc.dram_tensor(name, list(shape), dt, kind="ExternalInput").ap()

    wgu = din("wgu", [4, 16, 128, 16 * 256])
    wdn = din("wdn", [4, 128, 16 * D])
    bgu = din("bgu", [4, 128, 32])
    bdn = din("bdn", [4, 1, D])
    XT = [din("XT%d" % e, [128, 16 * caps[e]], BF16) for e in range(4)]
    gw = [din("gw%d" % e, [128, caps[e] // 128]) for e in range(4)]
    Y = [nc.dram_tensor("Y%d" % e, [caps[e], D], F32, kind="ExternalOutput").ap() for e in range(4)]

    k = KB(nc)
    SBT = 9
    sbs_e = []
    for e in range(4):
        ntl = caps[e] // 128
        nsb = -(-ntl // SBT)
        base, rem = ntl // nsb, ntl % nsb
        lst, t = [], 0
        for i in range(nsb):
            n = base + (1 if i < rem else 0)
            lst.append((t, n))
            t += n
        sbs_e.append(lst)
    mxs = max(n for lst in sbs_e for _, n in lst) * 128
    mxt = max(caps) // 128
    xts = k.sb([128, 16, mxs], BF16)
    act = k.sb([128, 16, mxs], BF16)
    wg = [k.sb([128, 16, 256], BF16) for _ in range(3)]
    wd = [k.sb([128, 16, 512], BF16) for _ in range(3)]
    gt_ = [k.sb([128, 512], F32) for _ in range(3)]
    st_ = [k.sb([128, 512], F32) for _ in range(3)]
    lt_ = [k.sb([128, 512], F32) for _ in range(3)]
    tt_ = [k.sb([128, 512], F32) for _ in range(3)]
    b1_s = [k.sb([128, 16], F32) for _ in range(2)]
    ys = [k.sb([128, 512], F32) for _ in range(3)]
    bg_s = [k.sb([128, 32], F32) for _ in range(2)]
    bd_s = [k.sb([128, D], F32) for _ in range(2)]
    gw_s = [k.sb([128, mxt], F32) for _ in range(2)]
    pA = [k.ps([128, 512], F32) for _ in range(3)]
    pB = [k.ps([128, 512], F32) for _ in range(3)]
    pD = [k.ps([128, 512], F32) for _ in range(2)]

    gu_jobs = [(e, si, j) for e in range(4) for si in range(len(sbs_e[e])) for j in range(16)]
    wgi = {job: i for i, job in enumerate(gu_jobs)}
    dn_jobs = [(e, si, nb) for e in range(4) for si in range(len(sbs_e[e])) for nb in range(4)]
    wdi = {job: i for i, job in enumerate(dn_jobs)}

    def load_wg(i):
        e, si, j = gu_jobs[i]
        k.dma("pool", wg[i % 3][:, :, :], wgu[e, j].rearrange("p (kc n) -> p kc n", kc=16), writes=[("wg", i % 3)])

    def load_wd(i):
        e, si, nb = dn_jobs[i]
        k.dma("pool", wd[i % 3][:, :, :],
              wdn[e].rearrange("p (kc n) -> p kc n", kc=16)[:, :, nb * 512:(nb + 1) * 512], writes=[("wd", i % 3)])

    load_wg(0)
    load_wg(1)
    load_wd(0)
    it = 0
    yi = 0
    for e in range(4):
        eb = e % 2
        ntl = caps[e] // 128
        k.dma("sp", bg_s[eb][:, :], bgu[e], writes=[("bg", eb)])
        k.dma("sp", bd_s[eb][:, :], bdn[e].broadcast_to([128, D]), writes=[("bd", eb)])
        k.dma("sp", gw_s[eb][:, :ntl], gw[e], writes=[("gw", eb)])
        k.op("dve", lambda e_: e_.tensor_scalar(out=b1_s[eb][:, :], in0=bg_s[eb][:, 16:32], scalar1=1.0, scalar2=None, op0=ALU.add),
             reads=[("bg", eb)], writes=[("b1", eb)])
        for si, (t0, nt) in enumerate(sbs_e[e]):
            ns = nt * 128
            k.dma("sp", xts[:, :, :ns], XT[e].rearrange("p (kc t) -> p kc t", kc=16)[:, :, t0 * 128:t0 * 128 + ns],
                  writes=[("xts", 0)])
            nblocks = [(a * 128, n * 128) for a, n in split_tiles(nt, 4)]
            for j in range(16):
                i = wgi[(e, si, j)]
                if i + 2 < len(gu_jobs):
                    load_wg(i + 2)
                ws = wg[i % 3]
                for (c0, n) in nblocks:
                    pb = it % 3
                    it += 1

                    def mm(e_, ps, off):
                        for kc in range(16):
                            ins = e_.matmul(ps[:, :n], ws[:, kc, off:off + 128], xts[:, kc, c0:c0 + n], start=(kc == 0), stop=(kc == 15))
                        return ins
                    k.op("pe", lambda e_: mm(e_, pA[pb], 0), reads=[("wg", i % 3), ("xts", 0)], writes=[("pA", pb)])
                    k.op("pe", lambda e_: mm(e_, pB[pb], 128), reads=[("wg", i % 3), ("xts", 0)], writes=[("pB", pb)])
                    k.op("dve", lambda e_: e_.tensor_scalar(out=gt_[pb][:, :n], in0=pA[pb][:, :n], scalar1=bg_s[eb][:, j:j + 1],
                                                            scalar2=LIM, op0=ALU.add, op1=ALU.min),
                         reads=[("pA", pb), ("bg", eb)], writes=[("g", pb)])
                    k.op("act", lambda e_: e_.activation(out=st_[pb][:, :n], in_=gt_[pb][:, :n], func=AF.Sigmoid, scale=ALPHA),
                         reads=[("g", pb)], writes=[("s", pb)])
                    k.op("dve", lambda e_: e_.tensor_scalar(out=lt_[pb][:, :n], in0=pB[pb][:, :n], scalar1=b1_s[eb][:, j:j + 1],
                                                            scalar2=1.0 - LIM, op0=ALU.add, op1=ALU.max),
                         reads=[("pB", pb), ("b1", eb)], writes=[("l", pb)])
                    k.op("pool", lambda e_: e_.tensor_tensor(out=tt_[pb][:, :n], in0=gt_[pb][:, :n], in1=st_[pb][:, :n], op=ALU.mult),
                         reads=[("g", pb), ("s", pb)], writes=[("t", pb)])
                    k.op("dve", lambda e_: e_.scalar_tensor_tensor(out=act[:, j, c0:c0 + n], in0=lt_[pb][:, :n], scalar=1.0 + LIM,
                                                                   in1=tt_[pb][:, :n], op0=ALU.min, op1=ALU.mult),
                         reads=[("t", pb), ("l", pb)], writes=[("act", j)])
            for nb in range(4):
                i = wdi[(e, si, nb)]
                if i + 1 < len(dn_jobs):
                    load_wd(i + 1)
                wds = wd[i % 3]
                for t in range(nt):
                    pb = it % 2
                    it += 1
                    yb = yi % 3
                    yi += 1

                    def mmd(e_):
                        for kc in range(16):
                            ins = e_.matmul(pD[pb][:, :], act[:, kc, t * 128:(t + 1) * 128], wds[:, kc, :], start=(kc == 0), stop=(kc == 15))
                        return ins
                    k.op("pe", mmd, reads=[("wd", i % 3)] + [("act", j) for j in range(16)], writes=[("pD", pb)])
                    k.op("dve", lambda e_: e_.tensor_tensor(out=ys[yb][:, :], in0=pD[pb][:, :], in1=bd_s[eb][:, nb * 512:(nb + 1) * 512],
                                                            op=ALU.add), reads=[("pD", pb), ("bd", eb)], writes=[("ys", yb)])
                    k.op("act", lambda e_: e_.mul(out=ys[yb][:, :], in_=ys[yb][:, :], mul=gw_s[eb][:, t0 + t:t0 + t + 1]),
                         reads=[("ys", yb), ("gw", eb)], writes=[("ys", yb)])
                    k.dma("sp", Y[e][(t0 + t) * 128:(t0 + t + 1) * 128, nb * 512:(nb + 1) * 512], ys[yb][:, :],
                          reads=[("ys", yb)], is_output=True)
    k.finish()
    k.close()
    return nc


def build_D(ntiles, nlat, ns=4):
    nc = bass.Bass("TRN2", target_bir_lowering=False)
    R = ntiles * 128
    xm = nc.dram_tensor("xm", [R, D], F32, kind="ExternalInput").ap()
    y4 = nc.dram_tensor("y4", [ns, R, D], F32, kind="ExternalInput").ap()
    gtv = nc.dram_tensor("gtv", [2, D], F32, kind="ExternalInput").ap()
    xo = nc.dram_tensor("xo", [R, D], F32, kind="ExternalOutput").ap()
    k = KB(nc)
    gts = [k.sb([128, D], F32) for _ in range(2)]
    xb = [k.sb([128, D], F32) for _ in range(2)]
    yb = [[k.sb([128, D], F32) for _ in range(ns)] for _ in range(2)]
    for r in range(2):
        k.dma("sp", gts[r][:, :], gtv[r:r + 1, :].broadcast_to([128, D]), writes=[("gt", r)])

    def load(i):
        b = i % 2
        k.dma("sp", xb[b][:, :], xm[i * 128:(i + 1) * 128, :], writes=[("xb", b)])
        for s in range(ns):
            k.dma("sp", yb[b][s][:, :], y4[s, i * 128:(i + 1) * 128, :], writes=[("yb", b, s)])
    load(0)
    for i in range(ntiles):
        b = i % 2
        if i + 1 < ntiles:
            load(i + 1)
        r = 0 if i < nlat else 1
        for s in range(1, ns):
            eng = "dve" if s % 2 == 1 else "pool"
            k.op(eng, lambda e: e.tensor_tensor(out=yb[b][0][:, :], in0=yb[b][0][:, :], in1=yb[b][s][:, :], op=ALU.add),
                 reads=[("yb", b, s)], writes=[("yb", b, 0)])
        k.op("pool", lambda e: e.tensor_tensor(out=yb[b][0][:, :], in0=yb[b][0][:, :], in1=gts[r][:, :], op=ALU.mult),
             reads=[("gt", r)], writes=[("yb", b, 0)])
        k.op("dve", lambda e: e.tensor_tensor(out=xb[b][:, :], in0=xb[b][:, :], in1=yb[b][0][:, :], op=ALU.add),
             reads=[("yb", b, 0)], writes=[("xb", b)])
        k.dma("sp", xo[i * 128:(i + 1) * 128, :], xb[b][:, :], reads=[("xb", b)], is_output=True)
    k.finish()
    k.close()
    return nc


def consts_B():
    idn = np.eye(128, dtype=np.float32)
    Rm = np.zeros((128, 128), np.float32)
    for base in (0, 64):
        for j in range(32):
            Rm[base + j, base + 32 + j] = -1.0
            Rm[base + 32 + j, base + j] = 1.0
    return idn, np.ascontiguousarray(Rm.T)

def rope_tables(R0):
    t = np.arange(NE)
    row = (R0 - 4 + t // 64).astype(np.float32)
    col = (t % 64).astype(np.float32)
    inv = (np.float32(10000.0) ** (-(np.arange(32, dtype=np.float32)) / np.float32(32))).astype(np.float32)
    d = np.arange(128)
    j = d % 32
    pos = np.where((d < 64)[:, None], row[None, :], col[None, :]).astype(np.float32)
    ang = (pos * inv[j][:, None]).astype(np.float32)
    return np.stack([np.cos(ang), np.sin(ang)], 0).astype(np.float32)

def valid_mask(R0):
    t = np.arange(NE)
    row = R0 - 4 + t // 64
    return ((row >= 0) & (row < 128)).astype(np.float32)[None, :]

def bias_tables(rpb_l, R0):
    out = np.empty((8, 128, 5, 7, 128), np.float32)
    kk = np.arange(128)[:, None]
    qq = np.arange(128)[None, :]
    for ci, qt in enumerate((2, 3, 8, 16, 17)):
        r0 = R0 - 4 + 2 * qt
        r = r0 + qq // 64
        qc = qq % 64
        rs = np.clip(r - 4, 0, 120)
        ws = np.clip(qc - 8, 0, 48)
        for o in range(7):
            kt = qt - 3 + o
            kr = R0 - 4 + 2 * kt + kk // 64
            kc = kk % 64
            valid = (kr >= 0) & (kr < 128) & (kr >= rs) & (kr < rs + 8) & (kc >= ws) & (kc < ws + 16)
            dr = np.clip(kr - r + 7, 0, 14)
            dc = np.clip(kc - qc, -15, 15) + 15
            b = rpb_l[:, dr, dc]
            out[:, :, ci, o, :] = np.where(valid[None], b, NEGB)
    return out.reshape(8, 128, 5 * 896)

def layer_consts(inp, l):
    w_in = inp["w_in"][l]
    win = np.ascontiguousarray(w_in.reshape(16, 128, 40, 128).transpose(2, 1, 0, 3)).reshape(40, 128, 2048)
    wout = np.ascontiguousarray(inp["w_out"][l].reshape(16, 128, 2048).transpose(1, 0, 2)).reshape(128, 16 * 2048)
    chp = np.empty((128, 8, 34), np.float32)
    chp[:, :, :31] = inp["w_dw"][l].T.reshape(8, 128, 31).transpose(1, 0, 2)
    chp[:, :, 31] = inp["b_dw"][l].reshape(8, 128).T
    chp[:, :, 32] = inp["ln_g"][l].reshape(8, 128).T
    chp[:, :, 33] = inp["ln_b"][l].reshape(8, 128).T
    gqk = np.ascontiguousarray(np.stack([inp["g_q"][l], inp["g_k"][l]], 1))
    gmf = np.ascontiguousarray(np.stack([inp["g_mix"][l], inp["g_ffn"][l]], 0))
    wr = np.ascontiguousarray(inp["w_router"][l].reshape(16, 128, 32).transpose(1, 0, 2)).reshape(128, 512)
    br = np.ascontiguousarray(inp["b_router"][l][None, :])
    return dict(win=win, wout=wout, chp=chp.reshape(128, 8 * 34), gqk=gqk, gmf=gmf, wr=wr, br=br)

def core_inputs_B(x, ctx, mod_l, rpb_l, lc, ci):
    b, j = ci // 4, ci % 4
    R0 = 32 * j
    lo, hi = (R0 - 4) * 64, (R0 + 36) * 64
    xe = np.zeros((NE, D), np.float32)
    a, e = max(lo, 0), min(hi, 8192)
    xe[a - lo:e - lo] = x[b, a:e]
    idn, rmt = consts_B()
    m = dict(lc)
    m.update(xe=xe, cx=np.ascontiguousarray(ctx[b]), modv=np.ascontiguousarray(mod_l[[b, 2]]), cs=rope_tables(R0),
             rmt=rmt, idn=idn, vmask=valid_mask(R0), btab=bias_tables(rpb_l, R0))
    return m


def prep_expert_weights(wgu, wdn, bgu, bdn):
    n = wgu.shape[0]
    a = wgu.reshape(n, 16, 128, 2, 16, 128)
    a = np.ascontiguousarray(a.transpose(0, 4, 2, 1, 3, 5)).reshape(n, 16, 128, 16 * 256)
    b = np.ascontiguousarray(wdn.reshape(n, 16, 128, 2048).transpose(0, 2, 1, 3)).reshape(n, 128, 16 * 2048)
    c = np.ascontiguousarray(bgu.reshape(n, 2, 16, 128).transpose(0, 3, 1, 2)).reshape(n, 128, 32)
    return a, b, c, np.ascontiguousarray(bdn[:, None, :])


def _run(nc, in_maps):
    res = run_bass_kernel_spmd(nc, in_maps, core_ids=list(range(8)))
    return res.results


def kernel(x, c, ctx, c_ctx, w_ada, b_ada, g_mix, g_ffn, w_in, w_dw, b_dw, ln_g, ln_b, g_q, g_k, rpb,
           w_out, w_router, b_router, w_gate_up, b_gate_up, w_down, b_down):
    f = lambda a: np.asarray(a, dtype=np.float32)
    inp = dict(x=f(x), c=f(c), ctx=f(ctx), c_ctx=f(c_ctx), w_ada=f(w_ada), b_ada=f(b_ada), g_mix=f(g_mix), g_ffn=f(g_ffn),
               w_in=f(w_in), w_dw=f(w_dw), b_dw=f(b_dw), ln_g=f(ln_g), ln_b=f(ln_b), g_q=f(g_q), g_k=f(g_k), rpb=f(rpb),
               w_out=f(w_out), w_router=f(w_router), b_router=f(b_router), w_gate_up=f(w_gate_up), b_gate_up=f(b_gate_up),
               w_down=f(w_down), b_down=f(b_down))
    sT = np.ascontiguousarray(np.stack([inp["c"][0], inp["c"][1], inp["c_ctx"]], 0).T)
    resA = _run(build_A(), [{"sT": sT, "wa": np.ascontiguousarray(inp["w_ada"][:, :, i * NCOL:(i + 1) * NCOL]),
                             "ba": np.ascontiguousarray(inp["b_ada"][:, i * NCOL:(i + 1) * NCOL])} for i in range(8)])
    mod = np.concatenate([r["mod"] for r in resA], axis=2)
    x_cur, ctx_cur = inp["x"], inp["ctx"]
    for l in range(2):
        last = l == 1
        lcst = layer_consts(inp, l)
        resB = _run(build_B(last), [core_inputs_B(x_cur, ctx_cur, mod[l], inp["rpb"][l], lcst, ci) for ci in range(8)])
        x_mid = np.concatenate([r["x_mid"] for r in resB], 0)
        Gall = np.concatenate([r["G_o"] for r in resB], 0)
        Hall = np.concatenate([np.asarray(r["h2_o"]).view(np.uint16) for r in resB], 0)
        if not last:
            c_mid = np.concatenate([resB[0]["c_mid"], resB[4]["c_mid"]], 0)
            Gall = np.concatenate([Gall, resB[0]["Gc_o"], resB[4]["Gc_o"]], 0)
            Hall = np.concatenate([Hall, np.asarray(resB[0]["h2c_o"]).view(np.uint16),
                                   np.asarray(resB[4]["h2c_o"]).view(np.uint16)], 0)
        T = Gall.shape[0]
        sel = Gall > 0
        idx = [np.nonzero(sel[:, E])[0] for E in range(32)]
        counts = np.array([len(i) for i in idx])
        order = np.argsort(-counts, kind="stable")
        caps = [max(128, int(-(-counts[order[8 * s:8 * s + 8]].max() // 128) * 128)) for s in range(4)]
        ns = int(sel.sum(1).max())
        in_maps = []
        for ci in range(8):
            es = [int(order[8 * s + ci]) for s in range(4)]
            a, b, cc_, d_ = prep_expert_weights(inp["w_gate_up"][l, es], inp["w_down"][l, es],
                                                inp["b_gate_up"][l, es], inp["b_down"][l, es])
            m = dict(wgu=a, wdn=b, bgu=cc_, bdn=d_)
            for s in range(4):
                ii = idx[es[s]]
                XT = np.zeros((128, 16, caps[s]), np.uint16)
                XT[:, :, :len(ii)] = Hall[ii].reshape(len(ii), 16, 128).transpose(2, 1, 0)
                gw = np.zeros((caps[s],), np.float32)
                gw[:len(ii)] = Gall[ii, es[s]]
                m["XT%d" % s] = XT.reshape(128, 16 * caps[s]).view(ml_dtypes.bfloat16)
                m["gw%d" % s] = np.ascontiguousarray(gw.reshape(caps[s] // 128, 128).T)
            in_maps.append(m)
        resC = _run(build_C(caps), in_maps)
        del in_maps
        slot = np.cumsum(sel, axis=1) - 1
        y4 = np.zeros((ns, T, D), np.float32)
        for pos in range(32):
            E = int(order[pos])
            ii = idx[E]
            y4[slot[ii, E], ii] = resC[pos % 8]["Y%d" % (pos // 8)][:len(ii)]
        del resC
        ntl = 16 if last else 17
        in_maps = []
        for ci in range(8):
            b = ci // 4
            xm = np.zeros((ntl * 128, D), np.float32)
            yy = np.zeros((ns, ntl * 128, D), np.float32)
            xm[:2048] = x_mid[ci * 2048:(ci + 1) * 2048]
            yy[:, :2048] = y4[:, ci * 2048:(ci + 1) * 2048]
            if not last and ci < 4:
                xm[2048:] = c_mid[ci * 128:(ci + 1) * 128]
                yy[:, 2048:] = y4[:, 16384 + ci * 128:16384 + (ci + 1) * 128]
            gtv = np.ascontiguousarray(np.stack([mod[l, b, 5 * D:6 * D], mod[l, 2, 5 * D:6 * D]], 0))
            in_maps.append(dict(xm=xm, y4=yy, gtv=gtv))
        resD = _run(build_D(ntl, 16, ns), in_maps)
        del in_maps, y4
        x_cur = np.concatenate([r["xo"][:2048] for r in resD], 0).reshape(2, 8192, D)
        if not last:
            ctx_cur = np.concatenate([resD[ci]["xo"][2048:] for ci in range(4)], 0).reshape(2, 256, D)
    return np.ascontiguousarray(x_cur, dtype=np.float32)
```

```python
import ml_dtypes
from concourse.bass_utils import run_bass_kernel_spmd
import numpy as np
from contextlib import ExitStack
import concourse.bass as bass
import concourse.mybir as mybir

F32 = mybir.dt.float32
BF16 = mybir.dt.bfloat16
I32 = mybir.dt.int32
ALU = mybir.AluOpType
AF = mybir.ActivationFunctionType
AX = mybir.AxisListType


class KB:
    SEM_ROLL = 20000
    NDMA = 8

    def __init__(self, nc):
        self.nc = nc
        self.es = ExitStack()
        self.eng = {"pe": nc.tensor, "dve": nc.vector, "act": nc.scalar, "pool": nc.gpsimd, "sp": nc.sync}
        self.cur = {}
        self.cnt = {}
        self.nsem = 0
        for e in ("pe", "dve", "act", "pool"):
            self._roll(e)
        self.dsem = {}
        self.dcnt = {}
        self.dnext = {}
        for q in ("sp", "pool", "act"):
            self.dsem[q] = [self._newsem() for _ in range(self.NDMA)]
            self.dcnt[q] = [0] * self.NDMA
            self.dnext[q] = 0
        self.seen = {e: {} for e in self.eng}
        self.state = {}
        self.nt = 0
        self.out_deps = []

    def _newsem(self):
        self.nsem += 1
        return self.es.enter_context(self.nc.semaphore("s%d" % self.nsem))

    def _roll(self, e):
        self.cur[e] = self._newsem()
        self.cnt[e] = 0

    def sb(self, shape, dt, name=None):
        self.nt += 1
        return self.es.enter_context(self.nc.sbuf_tensor(name or ("t%d" % self.nt), list(shape), dt))

    def ps(self, shape, dt, name=None):
        self.nt += 1
        return self.es.enter_context(self.nc.psum_tensor(name or ("p%d" % self.nt), list(shape), dt))

    def dram(self, name, shape, dt, kind="Internal"):
        return self.nc.dram_tensor(name, list(shape), dt, kind=kind).ap()

    def _wait(self, e, deps):
        best = {}
        for d in deps:
            if d is None:
                continue
            sem, val = d
            k = id(sem)
            if k not in best or best[k][1] < val:
                best[k] = (sem, val)
        for k, (sem, val) in best.items():
            if self.seen[e].get(k, 0) >= val:
                continue
            self.eng[e].wait_ge(sem, val)
            self.seen[e][k] = val

    def _deps(self, reads, writes):
        deps = []
        for r in reads:
            st = self.state.get(r)
            if st:
                deps.append(st[0])
        for w in writes:
            st = self.state.get(w)
            if st:
                deps.append(st[0])
                deps.extend(st[1])
        return deps

    def _commit(self, my, reads, writes):
        for r in reads:
            st = self.state.setdefault(r, [None, []])
            st[1].append(my)
            if len(st[1]) > 64:
                st[1] = st[1][-64:]
        for w in writes:
            self.state[w] = [my, []]

    def op(self, e, fn, reads=(), writes=()):
        self._wait(e, self._deps(reads, writes))
        ins = fn(self.eng[e])
        if self.cnt[e] >= self.SEM_ROLL:
            self._roll(e)
        self.cnt[e] += 1
        ins.then_inc(self.cur[e], 1)
        my = (self.cur[e], self.cnt[e])
        self._commit(my, reads, writes)
        return my

    def dma(self, q, out, in_, reads=(), writes=(), is_output=False, **kw):
        i = self.dnext[q]
        self.dnext[q] = (i + 1) % self.NDMA
        sem = self.dsem[q][i]
        deps = self._deps(reads, writes)
        if self.dcnt[q][i] > 0:
            deps.append((sem, 16 * self.dcnt[q][i]))
        self._wait(q, deps)
        ins = self.eng[q].dma_start(out=out, in_=in_, **kw)
        self.dcnt[q][i] += 1
        ins.then_inc(sem, 16)
        my = (sem, 16 * self.dcnt[q][i])
        self._commit(my, reads, writes)
        if is_output:
            self.out_deps.append(my)
        return my

    def finish(self):
        self.seen["sp"] = {}
        self._wait("sp", self.out_deps)
        last = [(self.cur[e], self.cnt[e]) for e in ("pe", "dve", "act", "pool") if self.cnt[e] > 0]
        self._wait("sp", last)

    def close(self):
        self.es.close()


def _kb_barrier(self):
    deps = [(self.cur[e], self.cnt[e]) for e in ("pe", "dve", "act", "pool") if self.cnt[e] > 0]
    for q in self.dsem:
        for i, sem in enumerate(self.dsem[q]):
            if self.dcnt[q][i] > 0:
                deps.append((sem, 16 * self.dcnt[q][i]))
    for e in self.eng:
        self._wait(e, deps)
    self.state.clear()


KB.barrier = _kb_barrier


class Phase:
    def __init__(self, k):
        self.k = k
        self.es = ExitStack()

    def sb(self, shape, dt):
        self.k.nt += 1
        return self.es.enter_context(self.k.nc.sbuf_tensor("t%d" % self.k.nt, list(shape), dt))

    def ps(self, shape, dt):
        self.k.nt += 1
        return self.es.enter_context(self.k.nc.psum_tensor("p%d" % self.k.nt, list(shape), dt))

    def end(self):
        self.k.barrier()
        self.es.close()


NCOL = 1536

def build_A():
    nc = bass.Bass("TRN2", target_bir_lowering=False)
    sT = nc.dram_tensor("sT", [D, 3], F32, kind="ExternalInput").ap()
    wa = nc.dram_tensor("wa", [2, D, NCOL], F32, kind="ExternalInput").ap()
    ba = nc.dram_tensor("ba", [2, NCOL], F32, kind="ExternalInput").ap()
    mod = nc.dram_tensor("mod", [2, 3, NCOL], F32, kind="ExternalOutput").ap()
    k = KB(nc)
    s_raw = k.sb([128, 16, 3], F32)
    s_act = k.sb([128, 16, 3], F32)
    wt = [k.sb([128, 16, 512], F32) for _ in range(2)]
    bt = [k.sb([3, 512], F32) for _ in range(2)]
    ot = [k.sb([3, 512], F32) for _ in range(2)]
    pt = [k.ps([3, 512], F32) for _ in range(2)]
    k.dma("sp", s_raw[:, :, :], sT.rearrange("(kc p) b -> p kc b", p=128), writes=[("s_raw", 0)])
    k.op("act", lambda e: e.activation(out=s_act[:, :, :], in_=s_raw[:, :, :], func=AF.Silu),
         reads=[("s_raw", 0)], writes=[("s_act", 0)])
    g = 0
    for l in range(2):
        for cb in range(3):
            b = g % 2
            k.dma("sp", wt[b][:, :, :], wa[l, :, cb * 512:(cb + 1) * 512].rearrange("(kc p) c -> p kc c", p=128),
                  writes=[("wt", b)])
            k.dma("sp", bt[b][:, :], ba[l:l + 1, cb * 512:(cb + 1) * 512].broadcast_to([3, 512]), writes=[("bt", b)])
            def mm(e, b=b):
                for kc in range(16):
                    ins = e.matmul(pt[b][:, :], s_act[:, kc, :], wt[b][:, kc, :], start=(kc == 0), stop=(kc == 15))
                return ins
            k.op("pe", mm, reads=[("s_act", 0), ("wt", b)], writes=[("pt", b)])
            k.op("dve", lambda e, b=b: e.tensor_tensor(out=ot[b][:, :], in0=pt[b][:, :], in1=bt[b][:, :], op=ALU.add),
                 reads=[("pt", b), ("bt", b)], writes=[("ot", b)])
            k.dma("sp", mod[l, :, cb * 512:(cb + 1) * 512], ot[b][:, :], reads=[("ot", b)], is_output=True)
            g += 1
    k.finish()
    k.close()
    return nc


D = 2048
NE = 2560
NO = 2048
OWN0 = 256
NCX = 256
NT = NE + NCX
NQ = NO + NCX
EPS = 1e-6
SCALE = 128 ** -0.5
NEGB = -30000.0
CT0 = NE + 15
VGW = NE + 15 + NCX + 15


def build_B(last):
    nc = bass.Bass("TRN2", target_bir_lowering=False)

    def din(name, shape, dt=F32):
        return nc.dram_tensor(name, list(shape), dt, kind="ExternalInput").ap()

    def dout(name, shape, dt=F32):
        return nc.dram_tensor(name, list(shape), dt, kind="ExternalOutput").ap()

    xe = din("xe", [NE, D])
    cx = din("cx", [NCX, D])
    modv = din("modv", [2, 6 * D])
    gmf = din("gmf", [2, D])
    win = din("win", [40, 128, 16 * 128])
    wout = din("wout", [128, 16 * D])
    chp = din("chp", [128, 8 * 34])
    gqk = din("gqk", [128, 2])
    cs = din("cs", [2, 128, NE])
    rmt = din("rmt", [128, 128])
    idn = din("idn", [128, 128])
    vmask = din("vmask", [1, NE])
    btab = din("btab", [8, 128, 5 * 896])
    wr = din("wr", [128, 16 * 32])
    br = din("br", [1, 32])
    x_mid = dout("x_mid", [NO, D])
    G_o = dout("G_o", [NO, 32])
    h2_o = dout("h2_o", [NO, D], BF16)
    if not last:
        c_mid = dout("c_mid", [NCX, D])
        Gc_o = dout("Gc_o", [NCX, 32])
        h2c_o = dout("h2c_o", [NCX, D], BF16)

    k = KB(nc)
    conv_d = k.dram("conv_d", [8, 128, NQ], F32)
    oT_d = k.dram("oT_d", [16, 128, NQ], BF16)

    ident_f = k.sb([128, 128], F32)
    ident_b = k.sb([128, 128], BF16)
    ones_f = k.sb([128, 128], F32)
    ones_b = k.sb([128, 128], BF16)
    rmt_s = k.sb([128, 128], F32)
    chp_s = k.sb([128, 8, 34], F32)
    gqk_s = k.sb([128, 2], F32)
    big = k.sb([128, 16 * NT], BF16)
    hT = big[:, :].rearrange("p (kc t) -> p kc t", kc=16)
    k.dma("sp", ident_f[:, :], idn, writes=[("c", 0)])
    k.dma("sp", rmt_s[:, :], rmt, writes=[("c", 1)])
    k.dma("sp", chp_s[:, :, :], chp.rearrange("p (c j) -> p c j", j=34), writes=[("c", 2)])
    k.dma("sp", gqk_s[:, :], gqk, writes=[("c", 3)])
    k.op("dve", lambda e: e.tensor_copy(out=ident_b[:, :], in_=ident_f[:, :]), reads=[("c", 0)], writes=[("c", 4)])
    k.op("dve", lambda e: e.memset(ones_f[:, :], 1.0), writes=[("c", 5)])
    k.op("dve", lambda e: e.memset(ones_b[:, :], 1.0), writes=[("c", 6)])
    eps_s = k.sb([128, 1], F32)
    k.op("dve", lambda e: e.memset(eps_s[:, :], EPS), writes=[("c", 7)])
    CONST = [("c", i) for i in range(8)]

    def rstd_ops(e_sum_ap, out_ap, n, deps_r, deps_w):
        k.op("act", lambda e: e.activation(out=out_ap, in_=e_sum_ap, func=AF.Sqrt, bias=eps_s[:, 0:1], scale=1.0 / n),
             reads=list(deps_r) + CONST, writes=deps_w)
        k.op("dve", lambda e: e.reciprocal(out=out_ap, in_=out_ap), reads=deps_w, writes=deps_w)

    ph = Phase(k)
    rows = [ph.sb([128, D], F32) for _ in range(5)]
    xt = [ph.sb([128, D], F32) for _ in range(2)]
    sqs = [ph.sb([128, D], F32) for _ in range(2)]
    tmpfs = [ph.sb([128, D], F32) for _ in range(2)]
    hb = [ph.sb([128, D], BF16) for _ in range(2)]
    ss = [ph.sb([128, 2], F32) for _ in range(2)]
    ptr = [ph.ps([128, 1024], F32) for _ in range(2)]

    def bc(ap_row):
        return ap_row.broadcast_to([128, D])

    k.dma("sp", rows[4][:, :], bc(gmf[0:1, :]), writes=[("row", 4)])
    for j, r in enumerate((0, 1)):
        k.dma("sp", rows[2 * j][:, :], bc(modv[r:r + 1, D:2 * D]), writes=[("row", 2 * j)])
        k.dma("sp", rows[2 * j + 1][:, :], bc(modv[r:r + 1, 0:D]), writes=[("row", 2 * j + 1)])
        k.op("dve", lambda e, j=j: e.scalar_tensor_tensor(out=rows[2 * j][:, :], in0=rows[2 * j][:, :], scalar=1.0,
                                                          in1=rows[4][:, :], op0=ALU.add, op1=ALU.mult),
             reads=[("row", 4)], writes=[("row", 2 * j)])

    tiles0 = [(xe, t, t * 128, 0) for t in range(NE // 128)] + [(cx, t, NE + t * 128, 1) for t in range(NCX // 128)]

    def load_x(i):
        src, t, col, which = tiles0[i]
        k.dma("sp", xt[i % 2][:, :], src[t * 128:(t + 1) * 128, :], writes=[("xt", i % 2)])

    load_x(0)
    for i, (src, t, col, which) in enumerate(tiles0):
        b = i % 2
        sq, tmpf = sqs[b], tmpfs[b]
        if i + 1 < len(tiles0):
            load_x(i + 1)
        k.op("act", lambda e, b=b: e.activation(out=sq[:, :], in_=xt[b][:, :], func=AF.Square),
             reads=[("xt", b)], writes=[("sq", b)])
        k.op("dve", lambda e, b=b: e.tensor_reduce(out=ss[b][:, 0:1], in_=sq[:, :], axis=AX.X, op=ALU.add),
             reads=[("sq", b)], writes=[("ss", b)])
        rstd_ops(ss[b][:, 0:1], ss[b][:, 1:2], D, [("ss", b)], [("ss", b)])
        A, Bv = rows[2 * which], rows[2 * which + 1]
        k.op("dve", lambda e, b=b, A=A: e.scalar_tensor_tensor(out=tmpf[:, :], in0=xt[b][:, :], scalar=ss[b][:, 1:2],
                                                               in1=A[:, :], op0=ALU.mult, op1=ALU.mult),
             reads=[("xt", b), ("ss", b), ("row", 2 * which)], writes=[("tmpf", b)])
        k.op("pool", lambda e, b=b, Bv=Bv: e.tensor_tensor(out=hb[b][:, :], in0=tmpf[:, :], in1=Bv[:, :], op=ALU.add),
             reads=[("tmpf", b), ("row", 2 * which + 1)], writes=[("hb", b)])
        pv = ptr[b][:, :].bitcast(BF16)

        def tr(e, b=b, pv=pv):
            for kc in range(16):
                ins = e.transpose(pv[:, kc * 128:(kc + 1) * 128], hb[b][:, kc * 128:(kc + 1) * 128], ident_b[:, :])
            return ins
        k.op("pe", tr, reads=[("hb", b)] + CONST, writes=[("ptr", b)])
        k.op("act", lambda e, pv=pv, col=col: e.copy(out=hT[:, :, col:col + 128],
                                                     in_=pv.rearrange("p (kc t) -> p kc t", kc=16)),
             reads=[("ptr", b)], writes=[("hT", col // 128)])
    ph.end()
    HT_ALL = [("hT", i) for i in range(NT // 128)]

    def load_w(wb, chunk, slot):
        k.dma("pool", wb[slot][:, :, :], win[chunk].rearrange("p (kc n) -> p kc n", kc=16), writes=[("wb", slot)])

    def mm_fm(ps_ap, wb, slot, c0, n, ps_key):
        def f(e):
            for kc in range(16):
                ins = e.matmul(ps_ap, wb[slot][:, kc, :], hT[:, kc, c0:c0 + n], start=(kc == 0), stop=(kc == 15))
            return ins
        k.op("pe", f, reads=[("wb", slot)] + [("hT", i) for i in range(c0 // 128, (c0 + n) // 128)], writes=[ps_key])

    EXT_BLOCKS = [(i * 512, 512) for i in range(5)]
    OWN_BLOCKS = [(OWN0 + i * 512, 512) for i in range(4)]
    CTX_BLOCK = [(NE, NCX)]

    ph = Phase(k)
    wb = [ph.sb([128, 16, 128], BF16) for _ in range(4)]
    vg = [ph.sb([128, VGW], BF16) for _ in range(2)]
    dg = [ph.sb([128, 31, 128], BF16) for _ in range(2)]
    acc = [ph.sb([128, NQ], F32) for _ in range(2)]
    sig = [ph.sb([128, 512], F32) for _ in range(2)]
    vm = ph.sb([128, NE], F32)
    pa = [ph.ps([128, 512], F32) for _ in range(2)]
    pg = [ph.ps([128, 512], F32) for _ in range(2)]
    pc = [ph.ps([128, 512], F32) for _ in range(2)]
    k.dma("sp", vm[:, :], vmask.broadcast_to([128, NE]), writes=[("vm", 0)])
    for b in range(2):
        k.op("pool", lambda e, b=b: e.memset(vg[b][:, NE:VGW], 0.0), writes=[("vg", b)])
    blocks = EXT_BLOCKS + ([] if last else CTX_BLOCK)
    nq = NO if last else NQ
    oblocks = [(i * 512, 512, OWN0 + i * 512) for i in range(NO // 512)] + ([] if last else [(NO, NCX, CT0)])
    ci_ = [0]

    def conv_cc(cc):
        vb = cc % 2
        for (o0, n, v0) in oblocks:
            pb = ci_[0] % 2
            ci_[0] += 1

            def cm(e):
                for j in range(31):
                    ins = e.matmul(pc[pb][:, :n], dg[vb][:, j, :], vg[vb][:, v0 + j - 15:v0 + j - 15 + n],
                                   start=(j == 0), stop=(j == 30))
                return ins
            k.op("pe", cm, reads=[("vg", vb), ("dg", vb)], writes=[("pc", pb)])
            k.op("act", lambda e: e.activation(out=acc[vb][:, o0:o0 + n], in_=pc[pb][:, :n], func=AF.Identity,
                                               bias=chp_s[:, cc, 31:32], scale=1.0),
                 reads=[("pc", pb)] + CONST, writes=[("acc", vb, o0)])
        k.dma("sp", conv_d[cc, :, 0:nq], acc[vb][:, 0:nq], reads=[("acc", vb, o0) for (o0, _, _) in oblocks],
              writes=[("conv_d", cc)])

    load_w(wb, 0, 0)
    load_w(wb, 8, 1)
    it = 0
    for cc in range(8):
        sa, sg = (2 * cc) % 4, (2 * cc + 1) % 4
        if cc + 1 < 8:
            load_w(wb, cc + 1, (2 * cc + 2) % 4)
            load_w(wb, 8 + cc + 1, (2 * cc + 3) % 4)
        vb = cc % 2
        k.op("dve", lambda e: e.tensor_tensor(out=dg[vb][:, :, :], in0=ident_b[:, :].unsqueeze(1).broadcast_to([128, 31, 128]),
                                              in1=chp_s[:, cc, 0:31].unsqueeze(2).broadcast_to([128, 31, 128]), op=ALU.mult),
             reads=CONST, writes=[("dg", vb)])
        for (c0, n) in blocks:
            pb = it % 2
            it += 1
            mm_fm(pa[pb][:, :n], wb, sa, c0, n, ("pa", pb))
            mm_fm(pg[pb][:, :n], wb, sg, c0, n, ("pg", pb))
            k.op("act", lambda e: e.activation(out=sig[pb][:, :n], in_=pg[pb][:, :n], func=AF.Sigmoid),
                 reads=[("pg", pb)], writes=[("sig", pb)])
            if c0 < NE:
                k.op("pool", lambda e: e.tensor_tensor(out=sig[pb][:, :n], in0=sig[pb][:, :n], in1=vm[:, c0:c0 + n], op=ALU.mult),
                     reads=[("vm", 0)], writes=[("sig", pb)])
                dst = vg[vb][:, c0:c0 + n]
            else:
                dst = vg[vb][:, CT0:CT0 + n]
            k.op("dve", lambda e: e.tensor_tensor(out=dst, in0=pa[pb][:, :n], in1=sig[pb][:, :n], op=ALU.mult),
                 reads=[("pa", pb), ("sig", pb)], writes=[("vg", vb)])
        if cc >= 1:
            conv_cc(cc - 1)
    conv_cc(7)
    ph.end()

    ph = Phase(k)
    wb = [ph.sb([128, 16, 128], BF16) for _ in range(3)]
    cs_s = ph.sb([128, 2, NE], F32)
    qT = ph.sb([128, NQ], BF16)
    kT = ph.sb([128, NT], BF16)
    Vh = ph.sb([128, NT // 128, 128], BF16)
    bt = ph.sb([128, 5, 896], F32)
    sqe2 = [ph.sb([128, 512], F32) for _ in range(2)]
    rs2 = [ph.sb([128, 512], F32) for _ in range(2)]
    xn2 = [ph.sb([128, 512], F32) for _ in range(2)]
    t12 = [ph.sb([128, 512], F32) for _ in range(2)]
    t22 = [ph.sb([128, 512], F32) for _ in range(2)]
    stmps = [ph.sb([128, 768], F32) for _ in range(2)]
    PT = [ph.sb([128, 8 * 128], BF16) for _ in range(2)]
    rec = ph.sb([128, 128], F32)
    oTh = ph.sb([128, NQ], BF16)
    arena = ph.ps([128, 4096], F32)

    def bank(i, n=1):
        return arena[:, i * 512:(i + n) * 512]
    pj = [bank(0), bank(1), bank(2)]
    psr2 = [bank(3), bank(4)]
    po = bank(5)
    pstv = [bank(6, 2), bank(0, 2)]
    pstk = [[("bk", 6), ("bk", 7)], [("bk", 0), ("bk", 1)]]
    k.dma("sp", cs_s[:, 0, :], cs[0], writes=[("cs", 0)])
    k.dma("sp", cs_s[:, 1, :], cs[1], writes=[("cs", 0)])
    pji = [0]

    def stageA(i, blk):
        wslot, c0, n, gcol, dst_ap, dst_key, rope_c0 = blk
        pb = i % 3
        mm_fm(pj[pb][:, :n], wb, wslot, c0, n, ("bk", pb))

    def stageB(i, blk):
        wslot, c0, n, gcol, dst_ap, dst_key, rope_c0 = blk
        pb, tb = i % 3, i % 2
        X = pj[pb][:, :n]
        sqe, rs, xn, psr = sqe2[tb], rs2[tb], xn2[tb], psr2[tb]
        k.op("act", lambda e: e.activation(out=sqe[:, :n], in_=X, func=AF.Square), reads=[("bk", pb)], writes=[("sqe", tb)])
        k.op("pe", lambda e: e.matmul(psr[:, :n], ones_f[:, :], sqe[:, :n], start=True, stop=True),
             reads=[("sqe", tb)] + CONST, writes=[("bk", 3 + tb)])
        rstd_ops(psr[:, :n], rs[:, :n], 128, [("bk", 3 + tb)], [("rs", tb)])
        k.op("dve", lambda e: e.scalar_tensor_tensor(out=xn[:, :n], in0=X, scalar=gqk_s[:, gcol:gcol + 1], in1=rs[:, :n],
                                                     op0=ALU.mult, op1=ALU.mult),
             reads=[("bk", pb), ("rs", tb)] + CONST, writes=[("xn", tb)])

    def stageC(i, blk):
        wslot, c0, n, gcol, dst_ap, dst_key, rope_c0 = blk
        tb = i % 2
        rs, xn, t1, t2, psr = rs2[tb], xn2[tb], t12[tb], t22[tb], psr2[tb]
        if rope_c0 is None:
            k.op("act", lambda e: e.copy(out=dst_ap, in_=xn[:, :n]), reads=[("xn", tb)], writes=[dst_key])
            return
        k.op("pe", lambda e: e.matmul(psr[:, :n], rmt_s[:, :], xn[:, :n], start=True, stop=True),
             reads=[("xn", tb), ("rs", tb)] + CONST, writes=[("bk", 3 + tb)])
        k.op("pool", lambda e: e.tensor_tensor(out=t1[:, :n], in0=xn[:, :n], in1=cs_s[:, 0, rope_c0:rope_c0 + n], op=ALU.mult),
             reads=[("xn", tb), ("cs", 0)], writes=[("t1", tb)])
        k.op("dve", lambda e: e.tensor_tensor(out=t2[:, :n], in0=psr[:, :n], in1=cs_s[:, 1, rope_c0:rope_c0 + n], op=ALU.mult),
             reads=[("bk", 3 + tb), ("cs", 0)], writes=[("t2", tb)])
        k.op("dve", lambda e: e.tensor_tensor(out=dst_ap, in0=t1[:, :n], in1=t2[:, :n], op=ALU.add),
             reads=[("t1", tb), ("t2", tb)], writes=[dst_key])

    def qk_pipeline(blocks):
        base = pji[0]
        N = len(blocks)
        for s_ in range(N + 2):
            if s_ < N:
                stageA(base + s_, blocks[s_])
            if 0 <= s_ - 1 < N:
                stageB(base + s_ - 1, blocks[s_ - 1])
            if 0 <= s_ - 2 < N:
                stageC(base + s_ - 2, blocks[s_ - 2])
        pji[0] = base + N

    def cls(qt):
        return {2: 0, 3: 1, 16: 3, 17: 4}.get(qt, 2)

    def att_sc(job, pti):
        qcol, slots, bias_cls, o_lo, o_hi = job
        pst = pstv[pti]

        def sc(e):
            for (s, kt) in slots:
                ins = e.matmul(pst[:, s * 128:(s + 1) * 128], kT[:, kt * 128:(kt + 1) * 128], qT[:, qcol:qcol + 128],
                               start=True, stop=True)
            return ins
        k.op("pe", sc, reads=[("kT", 0), ("qT", 0)], writes=pstk[pti])

    def att_rest(job, pti):
        qcol, slots, bias_cls, o_lo, o_hi = job
        pst, P, stmp = pstv[pti], PT[pti], stmps[pti]
        if bias_cls is not None:
            w_ = (o_hi - o_lo) * 128
            k.op("dve", lambda e: e.scalar_tensor_tensor(out=stmp[:, 0:w_], in0=pst[:, 0:w_], scalar=SCALE,
                                                         in1=bt[:, bias_cls, o_lo * 128:o_hi * 128], op0=ALU.mult, op1=ALU.add),
                 reads=pstk[pti] + [("bt", 0)], writes=[("stmp", pti)])
            k.op("act", lambda e: e.activation(out=P[:, 0:w_], in_=stmp[:, 0:w_], func=AF.Exp),
                 reads=[("stmp", pti)], writes=[("PT", pti)])
        k.op("act", lambda e: e.activation(out=P[:, 768:1024], in_=pst[:, 768:1024], func=AF.Exp, scale=SCALE),
             reads=pstk[pti], writes=[("PT", pti)])

        def pv(e):
            for i, (s, kt) in enumerate(slots):
                e.matmul(po[:, 0:128], Vh[:, kt, :], P[:, s * 128:(s + 1) * 128], start=(i == 0), stop=(i == len(slots) - 1))
            for i, (s, kt) in enumerate(slots):
                ins = e.matmul(po[:, 128:256], ones_b[:, :], P[:, s * 128:(s + 1) * 128], start=(i == 0),
                               stop=(i == len(slots) - 1))
            return ins
        k.op("pe", pv, reads=[("PT", pti), ("Vh", 0)] + CONST, writes=[("bk", 5)])
        k.op("dve", lambda e: e.reciprocal(out=rec[:, :], in_=po[:, 128:256]), reads=[("bk", 5)], writes=[("rec", 0)])
        k.op("dve", lambda e: e.tensor_tensor(out=oTh[:, qcol:qcol + 128], in0=po[:, 0:128], in1=rec[:, :], op=ALU.mult),
             reads=[("bk", 5), ("rec", 0)], writes=[("oTh", 0)])

    load_w(wb, 16, 0)
    wi = 0
    for h in range(8):
        chunks = [16 + h, 24 + h, 32 + h]
        nxt = chunks[1:] + ([16 + h + 1] if h < 7 else [])
        k.dma("sp", bt[:, :, :], btab[h].rearrange("p (c n) -> p c n", c=5), writes=[("bt", 0)])
        sq_, wi = wi % 3, wi + 1
        load_w(wb, nxt[0], wi % 3)
        sk_, wi = wi % 3, wi + 1
        load_w(wb, nxt[1], wi % 3)
        blocks = [(sq_, c0, n, 0, qT[:, i * 512:i * 512 + n], ("qT", 0), c0) for i, (c0, n) in enumerate(OWN_BLOCKS)]
        if not last:
            blocks.append((sq_, NE, NCX, 0, qT[:, NO:NQ], ("qT", 0), None))
        blocks += [(sk_, c0, n, 1, kT[:, c0:c0 + n], ("kT", 0), c0) for (c0, n) in EXT_BLOCKS]
        blocks.append((sk_, NE, NCX, 1, kT[:, NE:NT], ("kT", 0), None))
        qk_pipeline(blocks)
        sv_, wi = wi % 3, wi + 1
        if len(nxt) > 2:
            load_w(wb, nxt[2], wi % 3)
        for g0 in range(0, NT // 128, 4):
            gn = min(4, NT // 128 - g0)
            pb = pji[0] % 3
            pji[0] += 1

            def vmm(e, g0=g0, gn=gn, pb=pb):
                for t in range(gn):
                    col = (g0 + t) * 128
                    for kc in range(16):
                        ins = e.matmul(pj[pb][:, t * 128:(t + 1) * 128], hT[:, kc, col:col + 128], wb[sv_][:, kc, :],
                                       start=(kc == 0), stop=(kc == 15))
                return ins
            k.op("pe", vmm, reads=[("wb", sv_)] + [("hT", g0 + t) for t in range(gn)], writes=[("bk", pb)])
            k.op("act", lambda e, g0=g0, gn=gn, pb=pb: e.copy(out=Vh[:, g0:g0 + gn, :],
                                                             in_=pj[pb][:, :gn * 128].rearrange("p (t d) -> p t d", d=128)),
                 reads=[("bk", pb)], writes=[("Vh", 0)])
        jobs = []
        for qt in range(2, 18):
            o_lo = 0 if qt == 17 else 1
            o_hi = 7 if qt == 2 else 6
            slots = [(o - o_lo, qt - 3 + o) for o in range(o_lo, o_hi)]
            jobs.append(((qt - 2) * 128, slots + [(6, 20), (7, 21)], cls(qt), o_lo, o_hi))
        if not last:
            for t in range(2):
                jobs.append((NO + t * 128, [(6, 20), (7, 21)], None, 0, 0))
        att_sc(jobs[0], 0)
        for n_, job in enumerate(jobs):
            if n_ + 1 < len(jobs):
                att_sc(jobs[n_ + 1], (n_ + 1) % 2)
            att_rest(job, n_ % 2)
        nq = NO if last else NQ
        k.dma("sp", oT_d[8 + h, :, 0:nq], oTh[:, 0:nq], reads=[("oTh", 0)], writes=[("oT_d", 8 + h)])
    ph.end()

    ph = Phase(k)
    cv = [ph.sb([128, 8, 512], F32) for _ in range(2)]
    cq = ph.sb([128, 8, 512], F32)
    mean = ph.sb([128, 512], F32)
    var = ph.sb([128, 512], F32)
    d1 = [ph.sb([128, 512], F32) for _ in range(2)]
    ob = [ph.sb([128, 8, 512], BF16) for _ in range(2)]
    p1 = ph.ps([128, 512], F32)
    p2 = ph.ps([128, 512], F32)
    lnblocks = [(i * 512, 512) for i in range(4)] + ([] if last else [(NO, NCX)])

    def load_cv(i):
        c0, n = lnblocks[i]
        k.dma("sp", cv[i % 2][:, :, :n], conv_d[:, :, c0:c0 + n].rearrange("c p t -> p c t"),
              reads=[("conv_d", c) for c in range(8)], writes=[("cv", i % 2)])
    load_cv(0)
    for i, (c0, n) in enumerate(lnblocks):
        b = i % 2
        if i + 1 < len(lnblocks):
            load_cv(i + 1)
        k.op("act", lambda e, b=b, n=n: e.activation(out=cq[:, :, :n], in_=cv[b][:, :, :n], func=AF.Square),
             reads=[("cv", b)], writes=[("cq", 0)])

        def s1(e, b=b, n=n):
            for c in range(8):
                ins = e.matmul(p1[:, :n], ones_f[:, :], cv[b][:, c, :n], start=(c == 0), stop=(c == 7))
            return ins

        def s2(e, n=n):
            for c in range(8):
                ins = e.matmul(p2[:, :n], ones_f[:, :], cq[:, c, :n], start=(c == 0), stop=(c == 7))
            return ins
        k.op("pe", s1, reads=[("cv", b)] + CONST, writes=[("p1", 0)])
        k.op("pe", s2, reads=[("cq", 0)] + CONST, writes=[("p2", 0)])
        k.op("dve", lambda e, n=n: e.tensor_scalar(out=mean[:, :n], in0=p1[:, :n], scalar1=1.0 / 1024, scalar2=None,
                                                   op0=ALU.mult), reads=[("p1", 0)], writes=[("mean", 0)])
        k.op("dve", lambda e, n=n: e.tensor_tensor(out=var[:, :n], in0=mean[:, :n], in1=mean[:, :n], op=ALU.mult),
             reads=[("mean", 0)], writes=[("var", 0)])
        k.op("dve", lambda e, n=n: e.scalar_tensor_tensor(out=var[:, :n], in0=p2[:, :n], scalar=1.0 / 1024, in1=var[:, :n],
                                                          op0=ALU.mult, op1=ALU.subtract),
             reads=[("p2", 0), ("var", 0)], writes=[("var", 0)])
        k.op("act", lambda e, n=n: e.activation(out=var[:, :n], in_=var[:, :n], func=AF.Sqrt, bias=eps_s[:, 0:1], scale=1.0),
             reads=[("var", 0)] + CONST, writes=[("var", 0)])
        k.op("dve", lambda e, n=n: e.reciprocal(out=var[:, :n], in_=var[:, :n]), reads=[("var", 0)], writes=[("var", 0)])
        for c in range(8):
            db = c % 2
            eng = "dve" if c % 2 == 0 else "pool"
            k.op(eng, lambda e, b=b, c=c, n=n, db=db: e.tensor_tensor(out=d1[db][:, :n], in0=cv[b][:, c, :n], in1=mean[:, :n],
                                                                     op=ALU.subtract),
                 reads=[("cv", b), ("mean", 0)], writes=[("d1", db)])
            k.op(eng, lambda e, n=n, db=db: e.tensor_tensor(out=d1[db][:, :n], in0=d1[db][:, :n], in1=var[:, :n], op=ALU.mult),
                 reads=[("var", 0), ("d1", db)], writes=[("d1", db)])
            k.op("act", lambda e, b=b, c=c, n=n, db=db: e.activation(out=ob[b][:, c, :n], in_=d1[db][:, :n], func=AF.Silu,
                                                                    bias=chp_s[:, c, 33:34], scale=chp_s[:, c, 32:33]),
                 reads=[("d1", db)] + CONST, writes=[("ob", b)])
        k.dma("sp", oT_d[0:8, :, c0:c0 + n].rearrange("c p t -> p c t"), ob[b][:, :, :n], reads=[("ob", b)],
              writes=[("oT_d", c) for c in range(8)])
    ph.end()

    ph = Phase(k)
    wo = big[:, 0:16 * D].rearrange("p (kc n) -> p kc n", kc=16)
    rows = [ph.sb([128, D], F32) for _ in range(4)]
    xt = [ph.sb([128, D], F32) for _ in range(2)]
    oTt = [ph.sb([128, 16, 128], BF16) for _ in range(2)]
    xm2 = [ph.sb([128, D], F32) for _ in range(2)]
    h2f2 = [ph.sb([128, D], F32) for _ in range(2)]
    h2b2 = [ph.sb([128, D], BF16) for _ in range(2)]
    h2T2 = [ph.sb([128, 16, 128], F32) for _ in range(2)]
    ss = ph.sb([128, 2], F32)
    wr_s = ph.sb([128, 16, 32], F32)
    br_s = ph.sb([128, 32], F32)
    lg = ph.sb([128, 32], F32)
    m8 = ph.sb([128, 8], F32)
    negm = ph.sb([128, 1], F32)
    msk = ph.sb([128, 32], F32)
    ex = ph.sb([128, 32], F32)
    gs = ph.sb([128, 2], F32)
    Gt = ph.sb([128, 32], F32)
    pw = ph.ps([128, D], F32)
    pt2 = ph.ps([128, 1024], F32)
    pl = ph.ps([128, 32], F32)
    k.dma("pool", wo, wout.rearrange("p (kc n) -> p kc n", kc=16), writes=[("wo", 0)])
    k.dma("sp", wr_s[:, :, :], wr.rearrange("p (kc n) -> p kc n", kc=16), writes=[("wr", 0)])
    k.dma("sp", br_s[:, :], br.broadcast_to([128, 32]), writes=[("wr", 1)])

    def load_rows(r):
        k.dma("sp", rows[0][:, :], bc(modv[r:r + 1, 2 * D:3 * D]), writes=[("row", 0)])
        k.dma("sp", rows[3][:, :], bc(gmf[1:2, :]), writes=[("row", 3)])
        k.dma("sp", rows[1][:, :], bc(modv[r:r + 1, 4 * D:5 * D]), writes=[("row", 1)])
        k.dma("sp", rows[2][:, :], bc(modv[r:r + 1, 3 * D:4 * D]), writes=[("row", 2)])
        k.op("dve", lambda e: e.scalar_tensor_tensor(out=rows[1][:, :], in0=rows[1][:, :], scalar=1.0, in1=rows[3][:, :],
                                                     op0=ALU.add, op1=ALU.mult), reads=[("row", 3)], writes=[("row", 1)])

    tiles4 = [(xe, OWN0 // 128 + t, t * 128, 0, x_mid, G_o, h2_o, t) for t in range(NO // 128)]
    if not last:
        tiles4 += [(cx, t, NO + t * 128, 1, c_mid, Gc_o, h2c_o, t) for t in range(NCX // 128)]

    def load4(i):
        src, st, col, which, _, _, _, _ = tiles4[i]
        k.dma("sp", xt[i % 2][:, :], src[st * 128:(st + 1) * 128, :], writes=[("xt", i % 2)])
        k.dma("sp", oTt[i % 2][:, :, :], oT_d[:, :, col:col + 128].rearrange("c p t -> p c t"),
              reads=[("oT_d", c) for c in range(16)], writes=[("oTt", i % 2)])

    def tile_vars(i):
        b = i % 2
        return b, xm2[b], h2f2[b], h2b2[b], h2T2[b]

    def emit_mo(i):
        b = i % 2
        def mo(e, b=b):
            for nb in range(4):
                for kc in range(16):
                    ins = e.matmul(pw[:, nb * 512:(nb + 1) * 512], oTt[b][:, kc, :], wo[:, kc, nb * 512:(nb + 1) * 512],
                                   start=(kc == 0), stop=(kc == 15))
            return ins
        k.op("pe", mo, reads=[("oTt", b), ("wo", 0)], writes=[("pw", 0)])

    def part1(i):
        src, st, col, which, xo, Go, ho, ot = tiles4[i]
        b, xm, h2f, h2b, h2T = tile_vars(i)
        sq = h2f
        k.op("dve", lambda e: e.tensor_tensor(out=xm[:, :], in0=pw[:, :], in1=rows[0][:, :], op=ALU.mult),
             reads=[("pw", 0), ("row", 0)], writes=[("xm", b)])
        k.op("pool", lambda e, b=b: e.tensor_tensor(out=xm[:, :], in0=xm[:, :], in1=xt[b][:, :], op=ALU.add),
             reads=[("xt", b), ("xm", b)], writes=[("xm", b)])
        k.dma("sp", xo[ot * 128:(ot + 1) * 128, :], xm[:, :], reads=[("xm", b)], is_output=True)
        k.op("act", lambda e: e.activation(out=sq[:, :], in_=xm[:, :], func=AF.Square), reads=[("xm", b)], writes=[("h2f", b)])
        k.op("dve", lambda e: e.tensor_reduce(out=ss[:, 0:1], in_=sq[:, :], axis=AX.X, op=ALU.add),
             reads=[("h2f", b)], writes=[("ss", 0)])
        rstd_ops(ss[:, 0:1], ss[:, 1:2], D, [("ss", 0)], [("ss", 0)])
        k.op("dve", lambda e: e.scalar_tensor_tensor(out=h2f[:, :], in0=xm[:, :], scalar=ss[:, 1:2], in1=rows[1][:, :],
                                                     op0=ALU.mult, op1=ALU.mult),
             reads=[("xm", b), ("ss", 0), ("row", 1)], writes=[("h2f", b)])
        k.op("pool", lambda e: e.tensor_tensor(out=h2f[:, :], in0=h2f[:, :], in1=rows[2][:, :], op=ALU.add),
             reads=[("h2f", b), ("row", 2)], writes=[("h2f", b)])
        k.op("act", lambda e: e.copy(out=h2b[:, :], in_=h2f[:, :]), reads=[("h2f", b)], writes=[("h2b", b)])
        k.dma("sp", ho[ot * 128:(ot + 1) * 128, :], h2b[:, :], reads=[("h2b", b)], is_output=True)

    def part2(i):
        src, st, col, which, xo, Go, ho, ot = tiles4[i]
        b, xm, h2f, h2b, h2T = tile_vars(i)
        for half in range(2):
            def trf(e, half=half):
                for j in range(8):
                    kc = half * 8 + j
                    ins = e.transpose(pt2[:, j * 128:(j + 1) * 128], h2f[:, kc * 128:(kc + 1) * 128], ident_f[:, :])
                return ins
            k.op("pe", trf, reads=[("h2f", b)] + CONST, writes=[("pt2", 0)])
            k.op("act", lambda e, half=half: e.copy(out=h2T[:, half * 8:(half + 1) * 8, :],
                                                   in_=pt2[:, :].rearrange("p (j t) -> p j t", j=8)),
                 reads=[("pt2", 0)], writes=[("h2T", b, half)])

        def rl(e):
            for kc in range(16):
                ins = e.matmul(pl[:, :], h2T[:, kc, :], wr_s[:, kc, :], start=(kc == 0), stop=(kc == 15))
            return ins
        k.op("pe", rl, reads=[("h2T", b, 0), ("h2T", b, 1), ("wr", 0)], writes=[("pl", 0)])
        k.op("dve", lambda e: e.tensor_tensor(out=lg[:, :], in0=pl[:, :], in1=br_s[:, :], op=ALU.add),
             reads=[("pl", 0), ("wr", 1)], writes=[("lg", 0)])
        k.op("dve", lambda e: e.max(out=m8[:, :], in_=lg[:, :]), reads=[("lg", 0)], writes=[("m8", 0)])
        k.op("dve", lambda e: e.tensor_scalar(out=negm[:, :], in0=m8[:, 0:1], scalar1=-1.0, scalar2=None, op0=ALU.mult),
             reads=[("m8", 0)], writes=[("negm", 0)])
        k.op("dve", lambda e: e.tensor_scalar(out=msk[:, :], in0=lg[:, :], scalar1=m8[:, 3:4], scalar2=None, op0=ALU.is_ge),
             reads=[("lg", 0), ("m8", 0)], writes=[("msk", 0)])
        k.op("act", lambda e: e.activation(out=ex[:, :], in_=lg[:, :], func=AF.Exp, bias=negm[:, 0:1], scale=1.0),
             reads=[("lg", 0), ("negm", 0)], writes=[("ex", 0)])
        k.op("dve", lambda e: e.tensor_tensor(out=ex[:, :], in0=ex[:, :], in1=msk[:, :], op=ALU.mult),
             reads=[("ex", 0), ("msk", 0)], writes=[("ex", 0)])
        k.op("dve", lambda e: e.tensor_reduce(out=gs[:, 0:1], in_=ex[:, :], axis=AX.X, op=ALU.add),
             reads=[("ex", 0)], writes=[("gs", 0)])
        k.op("dve", lambda e: e.reciprocal(out=gs[:, 1:2], in_=gs[:, 0:1]), reads=[("gs", 0)], writes=[("gs", 0)])
        k.op("dve", lambda e: e.tensor_scalar(out=Gt[:, :], in0=ex[:, :], scalar1=gs[:, 1:2], scalar2=None, op0=ALU.mult),
             reads=[("ex", 0), ("gs", 0)], writes=[("Gt", 0)])
        k.dma("sp", Go[ot * 128:(ot + 1) * 128, :], Gt[:, :], reads=[("Gt", 0)], is_output=True)

    load_rows(0)
    load4(0)
    emit_mo(0)
    for i in range(len(tiles4)):
        if tiles4[i][3] == 1 and tiles4[i - 1][3] == 0:
            load_rows(1)
        if i + 1 < len(tiles4):
            load4(i + 1)
        part1(i)
        if i + 1 < len(tiles4):
            emit_mo(i + 1)
        part2(i)
    k.finish()
    ph.es.close()
    k.close()
    return nc


ALPHA = 1.702
LIM = 7.0


def split_tiles(ntl, mx):
    out, t = [], 0
    while t < ntl:
        n = min(mx, ntl - t)
        out.append((t, n))
        t += n
    return out


def build_C(caps):
    nc = bass.Bass("TRN2", target_bir_lowering=False)
    NSL = len(caps)

    def din(name, shape, dt=F32):
        return nc.dram_tensor(name, list(shape), dt, kind="ExternalInput").ap()

    wgu = din("wgu", [NSL, 16, 128, 16 * 256])
    wdn = din("wdn", [NSL, 128, 16 * D])
    bgu = din("bgu", [NSL, 128, 32])
    bdn = din("bdn", [NSL, 1, D])
    XT = [din("XT%d" % e, [128, 16 * caps[e]], BF16) for e in range(NSL)]
    gw = [din("gw%d" % e, [128, caps[e] // 128]) for e in range(NSL)]
    Y = [nc.dram_tensor("Y%d" % e, [caps[e], D], F32, kind="ExternalOutput").ap() for e in range(NSL)]

    k = KB(nc)
    SBT = 9
    sbs_e = []
    for e in range(NSL):
        ntl = caps[e] // 128
        nsb = -(-ntl // SBT)
        base, rem = ntl // nsb, ntl % nsb
        lst, t = [], 0
        for i in range(nsb):
            n = base + (1 if i < rem else 0)
            lst.append((t, n))
            t += n
        sbs_e.append(lst)
    mxs = max(n for lst in sbs_e for _, n in lst) * 128
    mxt = max(caps) // 128
    xts = k.sb([128, 16, mxs], BF16)
    act = k.sb([128, 16, mxs], BF16)
    wg = [k.sb([128, 16, 256], BF16) for _ in range(3)]
    wd = [k.sb([128, 16, 512], BF16) for _ in range(3)]
    gt_ = [k.sb([128, 512], F32) for _ in range(3)]
    st_ = [k.sb([128, 512], F32) for _ in range(3)]
    lt_ = [k.sb([128, 512], F32) for _ in range(3)]
    tt_ = [k.sb([128, 512], F32) for _ in range(3)]
    b1_s = [k.sb([128, 16], F32) for _ in range(2)]
    ys = [k.sb([128, 512], F32) for _ in range(3)]
    bg_s = [k.sb([128, 32], F32) for _ in range(2)]
    bd_s = [k.sb([128, D], F32) for _ in range(2)]
    gw_s = [k.sb([128, mxt], F32) for _ in range(2)]
    pA = [k.ps([128, 512], F32) for _ in range(3)]
    pB = [k.ps([128, 512], F32) for _ in range(3)]
    pD = [k.ps([128, 512], F32) for _ in range(2)]

    gu_jobs = [(e, si, j) for e in range(NSL) for si in range(len(sbs_e[e])) for j in range(16)]
    wgi = {job: i for i, job in enumerate(gu_jobs)}
    dn_jobs = [(e, si, nb) for e in range(NSL) for si in range(len(sbs_e[e])) for nb in range(4)]
    wdi = {job: i for i, job in enumerate(dn_jobs)}

    def load_wg(i):
        e, si, j = gu_jobs[i]
        k.dma("pool", wg[i % 3][:, :, :], wgu[e, j].rearrange("p (kc n) -> p kc n", kc=16), writes=[("wg", i % 3)])

    def load_wd(i):
        e, si, nb = dn_jobs[i]
        k.dma("pool", wd[i % 3][:, :, :],
              wdn[e].rearrange("p (kc n) -> p kc n", kc=16)[:, :, nb * 512:(nb + 1) * 512], writes=[("wd", i % 3)])

    load_wg(0)
    load_wg(1)
    load_wd(0)
    it = 0
    yi = 0
    for e in range(NSL):
        eb = e % 2
        ntl = caps[e] // 128
        k.dma("sp", bg_s[eb][:, :], bgu[e], writes=[("bg", eb)])
        k.dma("sp", bd_s[eb][:, :], bdn[e].broadcast_to([128, D]), writes=[("bd", eb)])
        k.dma("sp", gw_s[eb][:, :ntl], gw[e], writes=[("gw", eb)])
        k.op("dve", lambda e_: e_.tensor_scalar(out=b1_s[eb][:, :], in0=bg_s[eb][:, 16:32], scalar1=1.0, scalar2=None, op0=ALU.add),
             reads=[("bg", eb)], writes=[("b1", eb)])
        for si, (t0, nt) in enumerate(sbs_e[e]):
            ns = nt * 128
            k.dma("sp", xts[:, :, :ns], XT[e].rearrange("p (kc t) -> p kc t", kc=16)[:, :, t0 * 128:t0 * 128 + ns],
                  writes=[("xts", 0)])
            nblocks = [(a * 128, n * 128) for a, n in split_tiles(nt, 4)]
            for j in range(16):
                i = wgi[(e, si, j)]
                if i + 2 < len(gu_jobs):
                    load_wg(i + 2)
                ws = wg[i % 3]
                for (c0, n) in nblocks:
                    pb = it % 3
                    it += 1

                    def mm(e_, ps, off):
                        for kc in range(16):
                            ins = e_.matmul(ps[:, :n], ws[:, kc, off:off + 128], xts[:, kc, c0:c0 + n], start=(kc == 0), stop=(kc == 15))
                        return ins
                    k.op("pe", lambda e_: mm(e_, pA[pb], 0), reads=[("wg", i % 3), ("xts", 0)], writes=[("pA", pb)])
                    k.op("pe", lambda e_: mm(e_, pB[pb], 128), reads=[("wg", i % 3), ("xts", 0)], writes=[("pB", pb)])
                    k.op("dve", lambda e_: e_.tensor_scalar(out=gt_[pb][:, :n], in0=pA[pb][:, :n], scalar1=bg_s[eb][:, j:j + 1],
                                                            scalar2=LIM, op0=ALU.add, op1=ALU.min),
                         reads=[("pA", pb), ("bg", eb)], writes=[("g", pb)])
                    k.op("act", lambda e_: e_.activation(out=st_[pb][:, :n], in_=gt_[pb][:, :n], func=AF.Sigmoid, scale=ALPHA),
                         reads=[("g", pb)], writes=[("s", pb)])
                    k.op("dve", lambda e_: e_.tensor_scalar(out=lt_[pb][:, :n], in0=pB[pb][:, :n], scalar1=b1_s[eb][:, j:j + 1],
                                                            scalar2=1.0 - LIM, op0=ALU.add, op1=ALU.max),
                         reads=[("pB", pb), ("b1", eb)], writes=[("l", pb)])
                    k.op("pool", lambda e_: e_.tensor_tensor(out=tt_[pb][:, :n], in0=gt_[pb][:, :n], in1=st_[pb][:, :n], op=ALU.mult),
                         reads=[("g", pb), ("s", pb)], writes=[("t", pb)])
                    k.op("dve", lambda e_: e_.scalar_tensor_tensor(out=act[:, j, c0:c0 + n], in0=lt_[pb][:, :n], scalar=1.0 + LIM,
                                                                   in1=tt_[pb][:, :n], op0=ALU.min, op1=ALU.mult),
                         reads=[("t", pb), ("l", pb)], writes=[("act", j)])
            for nb in range(4):
                i = wdi[(e, si, nb)]
                if i + 1 < len(dn_jobs):
                    load_wd(i + 1)
                wds = wd[i % 3]
                for t in range(nt):
                    pb = it % 2
                    it += 1
                    yb = yi % 3
                    yi += 1

                    def mmd(e_):
                        for kc in range(16):
                            ins = e_.matmul(pD[pb][:, :], act[:, kc, t * 128:(t + 1) * 128], wds[:, kc, :], start=(kc == 0), stop=(kc == 15))
                        return ins
                    k.op("pe", mmd, reads=[("wd", i % 3)] + [("act", j) for j in range(16)], writes=[("pD", pb)])
                    k.op("dve", lambda e_: e_.tensor_tensor(out=ys[yb][:, :], in0=pD[pb][:, :], in1=bd_s[eb][:, nb * 512:(nb + 1) * 512],
                                                            op=ALU.add), reads=[("pD", pb), ("bd", eb)], writes=[("ys", yb)])
                    k.op("act", lambda e_: e_.mul(out=ys[yb][:, :], in_=ys[yb][:, :], mul=gw_s[eb][:, t0 + t:t0 + t + 1]),
                         reads=[("ys", yb), ("gw", eb)], writes=[("ys", yb)])
                    k.dma("sp", Y[e][(t0 + t) * 128:(t0 + t + 1) * 128, nb * 512:(nb + 1) * 512], ys[yb][:, :],
                          reads=[("ys", yb)], is_output=True)
    k.finish()
    k.close()
    return nc


def build_D(ntiles, nlat, ns=4):
    nc = bass.Bass("TRN2", target_bir_lowering=False)
    R = ntiles * 128
    xm = nc.dram_tensor("xm", [R, D], F32, kind="ExternalInput").ap()
    y4 = nc.dram_tensor("y4", [ns, R, D], F32, kind="ExternalInput").ap()
    gtv = nc.dram_tensor("gtv", [2, D], F32, kind="ExternalInput").ap()
    xo = nc.dram_tensor("xo", [R, D], F32, kind="ExternalOutput").ap()
    k = KB(nc)
    gts = [k.sb([128, D], F32) for _ in range(2)]
    xb = [k.sb([128, D], F32) for _ in range(2)]
    yb = [[k.sb([128, D], F32) for _ in range(ns)] for _ in range(2)]
    for r in range(2):
        k.dma("sp", gts[r][:, :], gtv[r:r + 1, :].broadcast_to([128, D]), writes=[("gt", r)])

    def load(i):
        b = i % 2
        k.dma("sp", xb[b][:, :], xm[i * 128:(i + 1) * 128, :], writes=[("xb", b)])
        for s in range(ns):
            k.dma("sp", yb[b][s][:, :], y4[s, i * 128:(i + 1) * 128, :], writes=[("yb", b, s)])
    load(0)
    for i in range(ntiles):
        b = i % 2
        if i + 1 < ntiles:
            load(i + 1)
        r = 0 if i < nlat else 1
        for s in range(1, ns):
            eng = "dve" if s % 2 == 1 else "pool"
            k.op(eng, lambda e: e.tensor_tensor(out=yb[b][0][:, :], in0=yb[b][0][:, :], in1=yb[b][s][:, :], op=ALU.add),
                 reads=[("yb", b, s)], writes=[("yb", b, 0)])
        k.op("pool", lambda e: e.tensor_tensor(out=yb[b][0][:, :], in0=yb[b][0][:, :], in1=gts[r][:, :], op=ALU.mult),
             reads=[("gt", r)], writes=[("yb", b, 0)])
        k.op("dve", lambda e: e.tensor_tensor(out=xb[b][:, :], in0=xb[b][:, :], in1=yb[b][0][:, :], op=ALU.add),
             reads=[("yb", b, 0)], writes=[("xb", b)])
        k.dma("sp", xo[i * 128:(i + 1) * 128, :], xb[b][:, :], reads=[("xb", b)], is_output=True)
    k.finish()
    k.close()
    return nc


def consts_B():
    idn = np.eye(128, dtype=np.float32)
    Rm = np.zeros((128, 128), np.float32)
    for base in (0, 64):
        for j in range(32):
            Rm[base + j, base + 32 + j] = -1.0
            Rm[base + 32 + j, base + j] = 1.0
    return idn, np.ascontiguousarray(Rm.T)

def rope_tables(R0):
    t = np.arange(NE)
    row = (R0 - 4 + t // 64).astype(np.float32)
    col = (t % 64).astype(np.float32)
    inv = (np.float32(10000.0) ** (-(np.arange(32, dtype=np.float32)) / np.float32(32))).astype(np.float32)
    d = np.arange(128)
    j = d % 32
    pos = np.where((d < 64)[:, None], row[None, :], col[None, :]).astype(np.float32)
    ang = (pos * inv[j][:, None]).astype(np.float32)
    return np.stack([np.cos(ang), np.sin(ang)], 0).astype(np.float32)

def valid_mask(R0):
    t = np.arange(NE)
    row = R0 - 4 + t // 64
    return ((row >= 0) & (row < 128)).astype(np.float32)[None, :]

def bias_tables(rpb_l, R0):
    out = np.empty((8, 128, 5, 7, 128), np.float32)
    kk = np.arange(128)[:, None]
    qq = np.arange(128)[None, :]
    for ci, qt in enumerate((2, 3, 8, 16, 17)):
        r0 = R0 - 4 + 2 * qt
        r = r0 + qq // 64
        qc = qq % 64
        rs = np.clip(r - 4, 0, 120)
        ws = np.clip(qc - 8, 0, 48)
        for o in range(7):
            kt = qt - 3 + o
            kr = R0 - 4 + 2 * kt + kk // 64
            kc = kk % 64
            valid = (kr >= 0) & (kr < 128) & (kr >= rs) & (kr < rs + 8) & (kc >= ws) & (kc < ws + 16)
            dr = np.clip(kr - r + 7, 0, 14)
            dc = np.clip(kc - qc, -15, 15) + 15
            b = rpb_l[:, dr, dc]
            out[:, :, ci, o, :] = np.where(valid[None], b, NEGB)
    return out.reshape(8, 128, 5 * 896)

def layer_consts(inp, l):
    w_in = inp["w_in"][l]
    win = np.ascontiguousarray(w_in.reshape(16, 128, 40, 128).transpose(2, 1, 0, 3)).reshape(40, 128, 2048)
    wout = np.ascontiguousarray(inp["w_out"][l].reshape(16, 128, 2048).transpose(1, 0, 2)).reshape(128, 16 * 2048)
    chp = np.empty((128, 8, 34), np.float32)
    chp[:, :, :31] = inp["w_dw"][l].T.reshape(8, 128, 31).transpose(1, 0, 2)
    chp[:, :, 31] = inp["b_dw"][l].reshape(8, 128).T
    chp[:, :, 32] = inp["ln_g"][l].reshape(8, 128).T
    chp[:, :, 33] = inp["ln_b"][l].reshape(8, 128).T
    gqk = np.ascontiguousarray(np.stack([inp["g_q"][l], inp["g_k"][l]], 1))
    gmf = np.ascontiguousarray(np.stack([inp["g_mix"][l], inp["g_ffn"][l]], 0))
    wr = np.ascontiguousarray(inp["w_router"][l].reshape(16, 128, 32).transpose(1, 0, 2)).reshape(128, 512)
    br = np.ascontiguousarray(inp["b_router"][l][None, :])
    return dict(win=win, wout=wout, chp=chp.reshape(128, 8 * 34), gqk=gqk, gmf=gmf, wr=wr, br=br)

def core_inputs_B(x, ctx, mod_l, rpb_l, lc, ci):
    b, j = ci // 4, ci % 4
    R0 = 32 * j
    lo, hi = (R0 - 4) * 64, (R0 + 36) * 64
    xe = np.zeros((NE, D), np.float32)
    a, e = max(lo, 0), min(hi, 8192)
    xe[a - lo:e - lo] = x[b, a:e]
    idn, rmt = consts_B()
    m = dict(lc)
    m.update(xe=xe, cx=np.ascontiguousarray(ctx[b]), modv=np.ascontiguousarray(mod_l[[b, 2]]), cs=rope_tables(R0),
             rmt=rmt, idn=idn, vmask=valid_mask(R0), btab=bias_tables(rpb_l, R0))
    return m


def prep_expert_weights(wgu, wdn, bgu, bdn):
    n = wgu.shape[0]
    a = wgu.reshape(n, 16, 128, 2, 16, 128)
    a = np.ascontiguousarray(a.transpose(0, 4, 2, 1, 3, 5)).reshape(n, 16, 128, 16 * 256)
    b = np.ascontiguousarray(wdn.reshape(n, 16, 128, 2048).transpose(0, 2, 1, 3)).reshape(n, 128, 16 * 2048)
    c = np.ascontiguousarray(bgu.reshape(n, 2, 16, 128).transpose(0, 3, 1, 2)).reshape(n, 128, 32)
    return a, b, c, np.ascontiguousarray(bdn[:, None, :])


def _run(nc, in_maps):
    res = run_bass_kernel_spmd(nc, in_maps, core_ids=list(range(8)))
    return res.results


def kernel(x, c, ctx, c_ctx, w_ada, b_ada, g_mix, g_ffn, w_in, w_dw, b_dw, ln_g, ln_b, g_q, g_k, rpb,
           w_out, w_router, b_router, w_gate_up, b_gate_up, w_down, b_down):
    f = lambda a: np.asarray(a, dtype=np.float32)
    inp = dict(x=f(x), c=f(c), ctx=f(ctx), c_ctx=f(c_ctx), w_ada=f(w_ada), b_ada=f(b_ada), g_mix=f(g_mix), g_ffn=f(g_ffn),
               w_in=f(w_in), w_dw=f(w_dw), b_dw=f(b_dw), ln_g=f(ln_g), ln_b=f(ln_b), g_q=f(g_q), g_k=f(g_k), rpb=f(rpb),
               w_out=f(w_out), w_router=f(w_router), b_router=f(b_router), w_gate_up=f(w_gate_up), b_gate_up=f(b_gate_up),
               w_down=f(w_down), b_down=f(b_down))
    sT = np.ascontiguousarray(np.stack([inp["c"][0], inp["c"][1], inp["c_ctx"]], 0).T)
    resA = _run(build_A(), [{"sT": sT, "wa": np.ascontiguousarray(inp["w_ada"][:, :, i * NCOL:(i + 1) * NCOL]),
                             "ba": np.ascontiguousarray(inp["b_ada"][:, i * NCOL:(i + 1) * NCOL])} for i in range(8)])
    mod = np.concatenate([r["mod"] for r in resA], axis=2)
    x_cur, ctx_cur = inp["x"], inp["ctx"]
    for l in range(2):
        last = l == 1
        lcst = layer_consts(inp, l)
        resB = _run(build_B(last), [core_inputs_B(x_cur, ctx_cur, mod[l], inp["rpb"][l], lcst, ci) for ci in range(8)])
        x_mid = np.concatenate([r["x_mid"] for r in resB], 0)
        Gall = np.concatenate([r["G_o"] for r in resB], 0)
        Hall = np.concatenate([np.asarray(r["h2_o"]).view(np.uint16) for r in resB], 0)
        if not last:
            c_mid = np.concatenate([resB[0]["c_mid"], resB[4]["c_mid"]], 0)
            Gall = np.concatenate([Gall, resB[0]["Gc_o"], resB[4]["Gc_o"]], 0)
            Hall = np.concatenate([Hall, np.asarray(resB[0]["h2c_o"]).view(np.uint16),
                                   np.asarray(resB[4]["h2c_o"]).view(np.uint16)], 0)
        T = Gall.shape[0]
        sel = Gall > 0
        idx = [np.nonzero(sel[:, E])[0] for E in range(32)]
        counts = np.array([len(i) for i in idx])
        hot = set(int(e) for e in np.argsort(-counts, kind="stable")[:8])
        virt = []
        for E in range(32):
            if E in hot:
                hh = (len(idx[E]) + 1) // 2
                virt.append((E, idx[E][:hh]))
                virt.append((E, idx[E][hh:]))
            else:
                virt.append((E, idx[E]))
        vcounts = np.array([len(v[1]) for v in virt])
        order = np.argsort(-vcounts, kind="stable")
        NSL = len(virt) // 8
        caps = [max(128, int(-(-vcounts[order[8 * s:8 * s + 8]].max() // 128) * 128)) for s in range(NSL)]
        ns = int(sel.sum(1).max())
        in_maps = []
        for ci in range(8):
            vs = [int(order[8 * s + ci]) for s in range(NSL)]
            es = [virt[v][0] for v in vs]
            a, b, cc_, d_ = prep_expert_weights(inp["w_gate_up"][l, es], inp["w_down"][l, es],
                                                inp["b_gate_up"][l, es], inp["b_down"][l, es])
            m = dict(wgu=a, wdn=b, bgu=cc_, bdn=d_)
            for s in range(NSL):
                E, ii = virt[vs[s]]
                XT = np.zeros((128, 16, caps[s]), np.uint16)
                XT[:, :, :len(ii)] = Hall[ii].reshape(len(ii), 16, 128).transpose(2, 1, 0)
                gw = np.zeros((caps[s],), np.float32)
                gw[:len(ii)] = Gall[ii, E]
                m["XT%d" % s] = XT.reshape(128, 16 * caps[s]).view(ml_dtypes.bfloat16)
                m["gw%d" % s] = np.ascontiguousarray(gw.reshape(caps[s] // 128, 128).T)
            in_maps.append(m)
        resC = _run(build_C(caps), in_maps)
        del in_maps
        slot = np.cumsum(sel, axis=1) - 1
        y4 = np.zeros((ns, T, D), np.float32)
        for pos in range(len(virt)):
            E, ii = virt[int(order[pos])]
            y4[slot[ii, E], ii] = resC[pos % 8]["Y%d" % (pos // 8)][:len(ii)]
        del resC
        ntl = 16 if last else 17
        in_maps = []
        for ci in range(8):
            b = ci // 4
            xm = np.zeros((ntl * 128, D), np.float32)
            yy = np.zeros((ns, ntl * 128, D), np.float32)
            xm[:2048] = x_mid[ci * 2048:(ci + 1) * 2048]
            yy[:, :2048] = y4[:, ci * 2048:(ci + 1) * 2048]
            if not last and ci < 4:
                xm[2048:] = c_mid[ci * 128:(ci + 1) * 128]
                yy[:, 2048:] = y4[:, 16384 + ci * 128:16384 + (ci + 1) * 128]
            gtv = np.ascontiguousarray(np.stack([mod[l, b, 5 * D:6 * D], mod[l, 2, 5 * D:6 * D]], 0))
            in_maps.append(dict(xm=xm, y4=yy, gtv=gtv))
        resD = _run(build_D(ntl, 16, ns), in_maps)
        del in_maps, y4
        x_cur = np.concatenate([r["xo"][:2048] for r in resD], 0).reshape(2, 8192, D)
        if not last:
            ctx_cur = np.concatenate([resD[ci]["xo"][2048:] for ci in range(4)], 0).reshape(2, 256, D)
    return np.ascontiguousarray(x_cur, dtype=np.float32)
```

```python
from concourse.bass import IndirectOffsetOnAxis
from concourse.bass_utils import run_bass_kernel_spmd
import numpy as np
from contextlib import ExitStack
import concourse.bass as bass
import concourse.mybir as mybir

F32 = mybir.dt.float32
BF16 = mybir.dt.bfloat16
I32 = mybir.dt.int32
ALU = mybir.AluOpType
AF = mybir.ActivationFunctionType
AX = mybir.AxisListType


class KB:
    SEM_ROLL = 20000
    NDMA = 8

    def __init__(self, nc):
        self.nc = nc
        self.es = ExitStack()
        self.eng = {"pe": nc.tensor, "dve": nc.vector, "act": nc.scalar, "pool": nc.gpsimd, "sp": nc.sync}
        self.cur = {}
        self.cnt = {}
        self.nsem = 0
        for e in ("pe", "dve", "act", "pool"):
            self._roll(e)
        self.dsem = {}
        self.dcnt = {}
        self.dnext = {}
        for q in ("sp", "pool", "act"):
            self.dsem[q] = [self._newsem() for _ in range(self.NDMA)]
            self.dcnt[q] = [0] * self.NDMA
            self.dnext[q] = 0
        self.seen = {e: {} for e in self.eng}
        self.state = {}
        self.nt = 0
        self.out_deps = []

    def _newsem(self):
        self.nsem += 1
        return self.es.enter_context(self.nc.semaphore("s%d" % self.nsem))

    def _roll(self, e):
        self.cur[e] = self._newsem()
        self.cnt[e] = 0

    def sb(self, shape, dt, name=None):
        self.nt += 1
        return self.es.enter_context(self.nc.sbuf_tensor(name or ("t%d" % self.nt), list(shape), dt))

    def ps(self, shape, dt, name=None):
        self.nt += 1
        return self.es.enter_context(self.nc.psum_tensor(name or ("p%d" % self.nt), list(shape), dt))

    def dram(self, name, shape, dt, kind="Internal"):
        return self.nc.dram_tensor(name, list(shape), dt, kind=kind).ap()

    def _wait(self, e, deps):
        best = {}
        for d in deps:
            if d is None:
                continue
            sem, val = d
            k = id(sem)
            if k not in best or best[k][1] < val:
                best[k] = (sem, val)
        for k, (sem, val) in best.items():
            if self.seen[e].get(k, 0) >= val:
                continue
            self.eng[e].wait_ge(sem, val)
            self.seen[e][k] = val

    def _deps(self, reads, writes):
        deps = []
        for r in reads:
            st = self.state.get(r)
            if st:
                deps.append(st[0])
        for w in writes:
            st = self.state.get(w)
            if st:
                deps.append(st[0])
                deps.extend(st[1])
        return deps

    def _commit(self, my, reads, writes):
        for r in reads:
            st = self.state.setdefault(r, [None, []])
            st[1].append(my)
            if len(st[1]) > 64:
                st[1] = st[1][-64:]
        for w in writes:
            self.state[w] = [my, []]

    def op(self, e, fn, reads=(), writes=()):
        self._wait(e, self._deps(reads, writes))
        ins = fn(self.eng[e])
        if self.cnt[e] >= self.SEM_ROLL:
            self._roll(e)
        self.cnt[e] += 1
        ins.then_inc(self.cur[e], 1)
        my = (self.cur[e], self.cnt[e])
        self._commit(my, reads, writes)
        return my

    def dma(self, q, out, in_, reads=(), writes=(), is_output=False, **kw):
        i = self.dnext[q]
        self.dnext[q] = (i + 1) % self.NDMA
        sem = self.dsem[q][i]
        deps = self._deps(reads, writes)
        if self.dcnt[q][i] > 0:
            deps.append((sem, 16 * self.dcnt[q][i]))
        self._wait(q, deps)
        ins = self.eng[q].dma_start(out=out, in_=in_, **kw)
        self.dcnt[q][i] += 1
        ins.then_inc(sem, 16)
        my = (sem, 16 * self.dcnt[q][i])
        self._commit(my, reads, writes)
        if is_output:
            self.out_deps.append(my)
        return my

    def finish(self):
        self.seen["sp"] = {}
        self._wait("sp", self.out_deps)
        last = [(self.cur[e], self.cnt[e]) for e in ("pe", "dve", "act", "pool") if self.cnt[e] > 0]
        self._wait("sp", last)

    def close(self):
        self.es.close()


def _kb_barrier(self):
    deps = [(self.cur[e], self.cnt[e]) for e in ("pe", "dve", "act", "pool") if self.cnt[e] > 0]
    for q in self.dsem:
        for i, sem in enumerate(self.dsem[q]):
            if self.dcnt[q][i] > 0:
                deps.append((sem, 16 * self.dcnt[q][i]))
    for e in self.eng:
        self._wait(e, deps)
    self.state.clear()


KB.barrier = _kb_barrier


class Phase:
    def __init__(self, k):
        self.k = k
        self.es = ExitStack()

    def sb(self, shape, dt):
        self.k.nt += 1
        return self.es.enter_context(self.k.nc.sbuf_tensor("t%d" % self.k.nt, list(shape), dt))

    def ps(self, shape, dt):
        self.k.nt += 1
        return self.es.enter_context(self.k.nc.psum_tensor("p%d" % self.k.nt, list(shape), dt))

    def end(self):
        self.k.barrier()
        self.es.close()


def _kb_idma(self, out, in_, out_off=None, in_off=None, reads=(), writes=()):
    q = "pool"
    i = self.dnext[q]
    self.dnext[q] = (i + 1) % self.NDMA
    sem = self.dsem[q][i]
    deps = self._deps(reads, writes)
    if self.dcnt[q][i] > 0:
        deps.append((sem, 16 * self.dcnt[q][i]))
    self._wait(q, deps)
    ins = self.nc.gpsimd.indirect_dma_start(out=out, out_offset=out_off, in_=in_, in_offset=in_off)
    self.dcnt[q][i] += 1
    ins.then_inc(sem, 16)
    my = (sem, 16 * self.dcnt[q][i])
    self._commit(my, reads, writes)
    return my


KB.idma = _kb_idma


U32 = mybir.dt.uint32
D = 2048
NCX = 256
EPS = 1e-6
SCALE = 128 ** -0.5
NEGB = -30000.0
ALPHA = 1.702
LIM = 7.0
NEXP = 32
LCFG = [dict(NE=3072, NO=2560, CAP=1280, cls={4: 0, 5: 1, 18: 3, 19: 4}),
        dict(NE=2560, NO=2048, CAP=1024, cls={2: 0, 3: 1, 16: 3, 17: 4})]
OWN0 = 256
MAXT = (LCFG[0]["NO"] + NCX) // 128


def split_tiles(ntl, mx):
    out, t = [], 0
    while t < ntl:
        n = min(mx, ntl - t)
        out.append((t, n))
        t += n
    return out


def build_fused():
    nc = bass.Bass("TRN2", target_bir_lowering=False)

    def din(name, shape, dt=F32):
        return nc.dram_tensor(name, list(shape), dt, kind="ExternalInput").ap()

    xe0 = din("xe0", [LCFG[0]["NE"], D])
    cx0 = din("cx0", [NCX, D])
    sT = din("sT", [D, 2])
    w_ada = din("w_ada", [2, D, 6 * D])
    b_ada = din("b_ada", [2, 6 * D])
    rmt = din("rmt", [128, 128])
    idn = din("idn", [128, 128])
    tri = din("tri", [128, 128])
    trash = din("trash", [128, 4])
    LW = []
    for l in range(2):
        c = LCFG[l]
        LW.append(dict(
            gmf=din("gmf%d" % l, [2, D]), win=din("win%d" % l, [40, 128, 16 * 128]), wout=din("wout%d" % l, [128, 16 * D]),
            chp=din("chp%d" % l, [128, 8 * 34]), gqk=din("gqk%d" % l, [128, 2]), cs=din("cs%d" % l, [2, 128, c["NE"]]),
            vmask=din("vmask%d" % l, [1, c["NE"]]), btab=din("btab%d" % l, [8, 128, 5 * 896]),
            wr=din("wr%d" % l, [128, 16 * 32]), br=din("br%d" % l, [1, 32]), tv=din("tv%d" % l, [128, MAXT]),
            ce=din("ce%d" % l, [128, 32]),
            wgu=din("wgu%d" % l, [NEXP, 16, 128, 16 * 256]), wdn=din("wdn%d" % l, [NEXP, 128, 16 * D]),
            bgu=din("bgu%d" % l, [NEXP, 128, 32]), bdn=din("bdn%d" % l, [NEXP, 1, D])))
    x_out = nc.dram_tensor("x_out", [LCFG[1]["NO"], D], F32, kind="ExternalOutput").ap()

    k = KB(nc)
    mod_d = k.dram("mod_d", [2, 2, 6 * D], F32)
    x1_d = k.dram("x1_d", [LCFG[1]["NE"], D], F32)
    cx1_d = k.dram("cx1_d", [NCX, D], F32)

    ident_f = k.sb([128, 128], F32)
    ident_b = k.sb([128, 128], BF16)
    ones_f = k.sb([128, 128], F32)
    ones_b = k.sb([128, 128], BF16)
    rmt_s = k.sb([128, 128], F32)
    tri_s = k.sb([128, 128], F32)
    trash_s = k.sb([128, 4], F32)
    eps_s = k.sb([128, 1], F32)
    chp_s = k.sb([128, 8, 34], F32)
    gqk_s = k.sb([128, 2], F32)
    tv_s = k.sb([128, MAXT], F32)
    G_all = k.sb([128, MAXT, 32], F32)
    lg_all = k.sb([128, MAXT, 32], F32)
    m8_all = k.sb([128, MAXT, 8], F32)
    k.dma("sp", ident_f[:, :], idn, writes=[("c", 0)])
    k.dma("sp", rmt_s[:, :], rmt, writes=[("c", 1)])
    k.dma("sp", tri_s[:, :], tri, writes=[("c", 2)])
    k.dma("sp", trash_s[:, :], trash, writes=[("c", 3)])
    k.op("dve", lambda e: e.tensor_copy(out=ident_b[:, :], in_=ident_f[:, :]), reads=[("c", 0)], writes=[("c", 4)])
    k.op("dve", lambda e: e.memset(ones_f[:, :], 1.0), writes=[("c", 5)])
    k.op("dve", lambda e: e.memset(ones_b[:, :], 1.0), writes=[("c", 6)])
    k.op("dve", lambda e: e.memset(eps_s[:, :], EPS), writes=[("c", 7)])
    CONST = [("c", i) for i in range(8)]

    def rstd_ops(e_sum_ap, out_ap, n, deps_r, deps_w):
        k.op("act", lambda e: e.activation(out=out_ap, in_=e_sum_ap, func=AF.Sqrt, bias=eps_s[:, 0:1], scale=1.0 / n),
             reads=list(deps_r) + CONST, writes=deps_w)
        k.op("dve", lambda e: e.reciprocal(out=out_ap, in_=out_ap), reads=deps_w, writes=deps_w)

    def bc(ap_row):
        return ap_row.broadcast_to([128, D])

    ph = Phase(k)
    s_raw = ph.sb([128, 16, 2], F32)
    s_act = ph.sb([128, 16, 2], F32)
    wt = [ph.sb([128, 16, 512], F32) for _ in range(3)]
    btl = [ph.sb([2, 512], F32) for _ in range(2)]
    otl = [ph.sb([2, 512], F32) for _ in range(2)]
    ptA = [ph.ps([2, 512], F32) for _ in range(2)]
    k.dma("sp", s_raw[:, :, :], sT.rearrange("(kc p) b -> p kc b", p=128), writes=[("s_raw", 0)])
    k.op("act", lambda e: e.activation(out=s_act[:, :, :], in_=s_raw[:, :, :], func=AF.Silu),
         reads=[("s_raw", 0)], writes=[("s_act", 0)])
    jobs = [(l, cb) for l in range(2) for cb in range(24)]

    def load_wa(i):
        l, cb = jobs[i]
        k.dma("sp", wt[i % 3][:, :, :], w_ada[l, :, cb * 512:(cb + 1) * 512].rearrange("(kc p) c -> p kc c", p=128),
              writes=[("wt", i % 3)])
    load_wa(0)
    load_wa(1)
    for i, (l, cb) in enumerate(jobs):
        b = i % 2
        if i + 2 < len(jobs):
            load_wa(i + 2)
        k.dma("sp", btl[b][:, :], b_ada[l:l + 1, cb * 512:(cb + 1) * 512].broadcast_to([2, 512]), writes=[("bt", b)])

        def mm(e, i=i, b=b):
            for kc in range(16):
                ins = e.matmul(ptA[b][:, :], s_act[:, kc, :], wt[i % 3][:, kc, :], start=(kc == 0), stop=(kc == 15))
            return ins
        k.op("pe", mm, reads=[("s_act", 0), ("wt", i % 3)], writes=[("pt", b)])
        k.op("dve", lambda e, b=b: e.tensor_tensor(out=otl[b][:, :], in0=ptA[b][:, :], in1=btl[b][:, :], op=ALU.add),
             reads=[("pt", b), ("bt", b)], writes=[("ot", b)])
        k.dma("sp", mod_d[l, :, cb * 512:(cb + 1) * 512], otl[b][:, :], reads=[("ot", b)], writes=[("mod_d", l)])
    ph.end()

    def emit_layer(l, xe, cx, x_dst, cx_dst):
        cfg = LCFG[l]
        W = LW[l]
        last = l == 1
        NE, NO, CAP = cfg["NE"], cfg["NO"], cfg["CAP"]
        NT = NE + NCX
        NQ = NO + NCX
        CT0 = NE + 15
        VGW = NE + 15 + NCX + 15
        NTQ = (NO if last else NQ) // 128
        modv = mod_d[l]
        xmid_d = k.dram("xmid_d%d" % l, [NQ, D], F32)
        h2_d = k.dram("h2_d%d" % l, [NQ, D], BF16)
        conv_d = k.dram("conv_d%d" % l, [8, 128, NQ], F32)
        oT_d = k.dram("oT_d%d" % l, [16, 128, NQ], BF16)
        Xd = k.dram("Xd%d" % l, [NEXP * CAP + 128, D], BF16)
        Gd = k.dram("Gd%d" % l, [NEXP * CAP + 128, 16], F32)
        HALF = (NEXP // 2) * CAP
        YdA = k.dram("YdA%d" % l, [HALF + 128, D], F32)
        YdB = k.dram("YdB%d" % l, [HALF + 128, D], F32)

        lay = Phase(k)
        big = lay.sb([128, 16 * NT], BF16)
        hT = big[:, :].rearrange("p (kc t) -> p kc t", kc=16)
        k.dma("sp", chp_s[:, :, :], W["chp"].rearrange("p (c j) -> p c j", j=34), writes=[("lc", 0)])
        k.dma("sp", gqk_s[:, :], W["gqk"], writes=[("lc", 1)])
        k.dma("sp", tv_s[:, :], W["tv"], writes=[("lc", 2)])
        LC = [("lc", i) for i in range(3)]

        ph = Phase(k)
        rows = [ph.sb([128, D], F32) for _ in range(5)]
        xt = [ph.sb([128, D], F32) for _ in range(2)]
        sq = ph.sb([128, D], F32)
        tmpf = ph.sb([128, D], F32)
        hb = [ph.sb([128, D], BF16) for _ in range(2)]
        ss = [ph.sb([128, 2], F32) for _ in range(2)]
        ptr = [ph.ps([128, 1024], F32) for _ in range(2)]
        k.dma("sp", rows[4][:, :], bc(W["gmf"][0:1, :]), writes=[("row", 4)])
        for j, r in enumerate((0, 1)):
            k.dma("sp", rows[2 * j][:, :], bc(modv[r:r + 1, D:2 * D]), reads=[("mod_d", l)], writes=[("row", 2 * j)])
            k.dma("sp", rows[2 * j + 1][:, :], bc(modv[r:r + 1, 0:D]), reads=[("mod_d", l)], writes=[("row", 2 * j + 1)])
            k.op("dve", lambda e, j=j: e.scalar_tensor_tensor(out=rows[2 * j][:, :], in0=rows[2 * j][:, :], scalar=1.0,
                                                              in1=rows[4][:, :], op0=ALU.add, op1=ALU.mult),
                 reads=[("row", 4)], writes=[("row", 2 * j)])
        tiles0 = [(xe, t, t * 128, 0) for t in range(NE // 128)] + [(cx, t, NE + t * 128, 1) for t in range(NCX // 128)]

        def load_x(i):
            src, t, col, which = tiles0[i]
            k.dma("sp", xt[i % 2][:, :], src[t * 128:(t + 1) * 128, :], reads=[("xsrc", l)], writes=[("xt", i % 2)])
        load_x(0)
        for i, (src, t, col, which) in enumerate(tiles0):
            b = i % 2
            if i + 1 < len(tiles0):
                load_x(i + 1)
            k.op("act", lambda e: e.activation(out=sq[:, :], in_=xt[b][:, :], func=AF.Square),
                 reads=[("xt", b)], writes=[("sq", 0)])
            k.op("dve", lambda e: e.tensor_reduce(out=ss[b][:, 0:1], in_=sq[:, :], axis=AX.X, op=ALU.add),
                 reads=[("sq", 0)], writes=[("ss", b)])
            rstd_ops(ss[b][:, 0:1], ss[b][:, 1:2], D, [("ss", b)], [("ss", b)])
            A, Bv = rows[2 * which], rows[2 * which + 1]
            k.op("dve", lambda e: e.scalar_tensor_tensor(out=tmpf[:, :], in0=xt[b][:, :], scalar=ss[b][:, 1:2],
                                                         in1=A[:, :], op0=ALU.mult, op1=ALU.mult),
                 reads=[("xt", b), ("ss", b), ("row", 2 * which)], writes=[("tmpf", 0)])
            k.op("pool", lambda e: e.tensor_tensor(out=hb[b][:, :], in0=tmpf[:, :], in1=Bv[:, :], op=ALU.add),
                 reads=[("tmpf", 0), ("row", 2 * which + 1)], writes=[("hb", b)])
            pv = ptr[b][:, :].bitcast(BF16)

            def tr(e):
                for kc in range(16):
                    ins = e.transpose(pv[:, kc * 128:(kc + 1) * 128], hb[b][:, kc * 128:(kc + 1) * 128], ident_b[:, :])
                return ins
            k.op("pe", tr, reads=[("hb", b)] + CONST, writes=[("ptr", b)])
            k.op("act", lambda e: e.copy(out=hT[:, :, col:col + 128], in_=pv.rearrange("p (kc t) -> p kc t", kc=16)),
                 reads=[("ptr", b)], writes=[("hT", col // 128)])
        ph.end()

        def load_w(wb, chunk, slot):
            k.dma("pool", wb[slot][:, :, :], W["win"][chunk].rearrange("p (kc n) -> p kc n", kc=16), writes=[("wb", slot)])

        def mm_fm(ps_ap, wb, slot, c0, n, ps_key):
            def f(e):
                for kc in range(16):
                    ins = e.matmul(ps_ap, wb[slot][:, kc, :], hT[:, kc, c0:c0 + n], start=(kc == 0), stop=(kc == 15))
                return ins
            k.op("pe", f, reads=[("wb", slot)] + [("hT", i) for i in range(c0 // 128, (c0 + n) // 128)], writes=[ps_key])

        EXT_BLOCKS = [(i * 512, 512) for i in range(NE // 512)]
        OWN_BLOCKS = [(OWN0 + i * 512, 512) for i in range(NO // 512)]
        CTX_BLOCK = [(NE, NCX)]

        ph = Phase(k)
        wb = [ph.sb([128, 16, 128], BF16) for _ in range(4)]
        vg = [ph.sb([128, VGW], F32) for _ in range(2)]
        acc = [ph.sb([128, NQ], F32) for _ in range(2)]
        sig = [ph.sb([128, 512], F32) for _ in range(2)]
        vm = ph.sb([128, NE], F32)
        pa = [ph.ps([128, 512], F32) for _ in range(2)]
        pg = [ph.ps([128, 512], F32) for _ in range(2)]
        k.dma("sp", vm[:, :], W["vmask"].broadcast_to([128, NE]), writes=[("vm", 0)])
        for b in range(2):
            k.op("pool", lambda e: e.memset(vg[b][:, NE:VGW], 0.0), writes=[("vg", b)])
        blocks = EXT_BLOCKS + ([] if last else CTX_BLOCK)
        load_w(wb, 0, 0)
        load_w(wb, 8, 1)
        it = 0
        for cc in range(8):
            sa, sg = (2 * cc) % 4, (2 * cc + 1) % 4
            if cc + 1 < 8:
                load_w(wb, cc + 1, (2 * cc + 2) % 4)
                load_w(wb, 8 + cc + 1, (2 * cc + 3) % 4)
            vb = cc % 2
            for (c0, n) in blocks:
                pb = it % 2
                it += 1
                mm_fm(pa[pb][:, :n], wb, sa, c0, n, ("pa", pb))
                mm_fm(pg[pb][:, :n], wb, sg, c0, n, ("pg", pb))
                k.op("act", lambda e: e.activation(out=sig[pb][:, :n], in_=pg[pb][:, :n], func=AF.Sigmoid),
                     reads=[("pg", pb)], writes=[("sig", pb)])
                if c0 < NE:
                    k.op("pool", lambda e: e.tensor_tensor(out=sig[pb][:, :n], in0=sig[pb][:, :n], in1=vm[:, c0:c0 + n], op=ALU.mult),
                         reads=[("vm", 0)], writes=[("sig", pb)])
                    dst = vg[vb][:, c0:c0 + n]
                else:
                    dst = vg[vb][:, CT0:CT0 + n]
                k.op("dve", lambda e: e.tensor_tensor(out=dst, in0=pa[pb][:, :n], in1=sig[pb][:, :n], op=ALU.mult),
                     reads=[("pa", pb), ("sig", pb)], writes=[("vg", vb)])
            segs = [(0, NO, OWN0)] + ([] if last else [(NO, NCX, CT0)])
            for j in range(31):
                for (o0, n, v0) in segs:
                    src = vg[vb][:, v0 + j - 15:v0 + j - 15 + n]
                    if j == 0:
                        k.op("dve", lambda e: e.tensor_scalar(out=acc[vb][:, o0:o0 + n], in0=src, scalar1=chp_s[:, cc, 0:1],
                                                              scalar2=chp_s[:, cc, 31:32], op0=ALU.mult, op1=ALU.add),
                             reads=[("vg", vb)] + LC, writes=[("acc", vb, o0)])
                    else:
                        k.op("dve", lambda e: e.scalar_tensor_tensor(out=acc[vb][:, o0:o0 + n], in0=src, scalar=chp_s[:, cc, j:j + 1],
                                                                     in1=acc[vb][:, o0:o0 + n], op0=ALU.mult, op1=ALU.add),
                             reads=[("vg", vb)], writes=[("acc", vb, o0)])
            nq = NO if last else NQ
            k.dma("sp", conv_d[cc, :, 0:nq], acc[vb][:, 0:nq], reads=[("acc", vb, 0), ("acc", vb, NO)], writes=[("conv_d", cc)])
        ph.end()

        ph = Phase(k)
        wb = [ph.sb([128, 16, 128], BF16) for _ in range(3)]
        csb = [ph.sb([128, 2, 512], F32) for _ in range(2)]
        qT = ph.sb([128, NQ], BF16)
        kT = ph.sb([128, NT], BF16)
        Vh = ph.sb([128, NT // 128, 128], BF16)
        bt = ph.sb([128, 5, 896], F32)
        sqe = ph.sb([128, 512], F32)
        rs = ph.sb([128, 512], F32)
        xn = ph.sb([128, 512], F32)
        t1 = ph.sb([128, 512], F32)
        t2 = ph.sb([128, 512], F32)
        stmp = ph.sb([128, 896], F32)
        PT = [ph.sb([128, 9 * 128], BF16) for _ in range(2)]
        rec = ph.sb([128, 128], F32)
        oTh = ph.sb([128, NQ], BF16)
        pj = [ph.ps([128, 512], F32) for _ in range(2)]
        psr = ph.ps([128, 512], F32)
        pst = ph.ps([128, 1536], F32)
        po = ph.ps([128, 512], F32)
        pji = [0]
        csi = [0]

        def qk_block(wslot, c0, n, gcol, dst_ap, dst_key, rope_c0):
            pb = pji[0] % 2
            pji[0] += 1
            X = pj[pb][:, :n]
            if rope_c0 is not None:
                cb_ = csi[0] % 2
                csi[0] += 1
                k.dma("sp", csb[cb_][:, :, :n], W["cs"][:, :, rope_c0:rope_c0 + n].rearrange("a p t -> p a t"), writes=[("cs", cb_)])
            mm_fm(X, wb, wslot, c0, n, ("pj", pb))
            k.op("act", lambda e: e.activation(out=sqe[:, :n], in_=X, func=AF.Square), reads=[("pj", pb)], writes=[("sqe", 0)])
            k.op("pe", lambda e: e.matmul(psr[:, :n], ones_f[:, :], sqe[:, :n], start=True, stop=True),
                 reads=[("sqe", 0)] + CONST, writes=[("psr", 0)])
            rstd_ops(psr[:, :n], rs[:, :n], 128, [("psr", 0)], [("rs", 0)])
            k.op("dve", lambda e: e.scalar_tensor_tensor(out=xn[:, :n], in0=X, scalar=gqk_s[:, gcol:gcol + 1], in1=rs[:, :n],
                                                         op0=ALU.mult, op1=ALU.mult),
                 reads=[("pj", pb), ("rs", 0)] + LC, writes=[("xn", 0)])
            if rope_c0 is None:
                k.op("act", lambda e: e.copy(out=dst_ap, in_=xn[:, :n]), reads=[("xn", 0)], writes=[dst_key])
                return
            k.op("pe", lambda e: e.matmul(psr[:, :n], rmt_s[:, :], xn[:, :n], start=True, stop=True),
                 reads=[("xn", 0), ("rs", 0)] + CONST, writes=[("psr", 0)])
            k.op("pool", lambda e: e.tensor_tensor(out=t1[:, :n], in0=xn[:, :n], in1=csb[cb_][:, 0, :n], op=ALU.mult),
                 reads=[("xn", 0), ("cs", cb_)], writes=[("t1", 0)])
            k.op("dve", lambda e: e.tensor_tensor(out=t2[:, :n], in0=psr[:, :n], in1=csb[cb_][:, 1, :n], op=ALU.mult),
                 reads=[("psr", 0), ("cs", cb_)], writes=[("t2", 0)])
            k.op("dve", lambda e: e.tensor_tensor(out=dst_ap, in0=t1[:, :n], in1=t2[:, :n], op=ALU.add),
                 reads=[("t1", 0), ("t2", 0)], writes=[dst_key])

        NKT = NE // 128
        CT_A, CT_B = NKT, NKT + 1

        def attend(qcol, slots, bias_cls, o_lo, o_hi, pti):
            P = PT[pti]

            def sc(e):
                for (s, kt) in slots:
                    ins = e.matmul(pst[:, s * 128:(s + 1) * 128], kT[:, kt * 128:(kt + 1) * 128], qT[:, qcol:qcol + 128],
                                   start=True, stop=True)
                return ins
            k.op("pe", sc, reads=[("kT", 0), ("qT", 0)], writes=[("pst", 0)])
            if bias_cls is not None:
                a, b_ = o_lo * 128, o_hi * 128
                k.op("dve", lambda e: e.scalar_tensor_tensor(out=stmp[:, a:b_], in0=pst[:, a:b_], scalar=SCALE,
                                                             in1=bt[:, bias_cls, a:b_], op0=ALU.mult, op1=ALU.add),
                     reads=[("pst", 0), ("bt", 0)], writes=[("stmp", 0)])
                k.op("act", lambda e: e.activation(out=P[:, a:b_], in_=stmp[:, a:b_], func=AF.Exp),
                     reads=[("stmp", 0)], writes=[("PT", pti)])
            k.op("act", lambda e: e.activation(out=P[:, 896:1152], in_=pst[:, 896:1152], func=AF.Exp, scale=SCALE),
                 reads=[("pst", 0)], writes=[("PT", pti)])

            def pv(e):
                for i, (s, kt) in enumerate(slots):
                    e.matmul(po[:, 0:128], Vh[:, kt, :], P[:, s * 128:(s + 1) * 128], start=(i == 0), stop=(i == len(slots) - 1))
                for i, (s, kt) in enumerate(slots):
                    ins = e.matmul(po[:, 128:256], ones_b[:, :], P[:, s * 128:(s + 1) * 128], start=(i == 0),
                                   stop=(i == len(slots) - 1))
                return ins
            k.op("pe", pv, reads=[("PT", pti), ("Vh", 0)] + CONST, writes=[("po", 0)])
            k.op("dve", lambda e: e.reciprocal(out=rec[:, :], in_=po[:, 128:256]), reads=[("po", 0)], writes=[("rec", 0)])
            k.op("dve", lambda e: e.tensor_tensor(out=oTh[:, qcol:qcol + 128], in0=po[:, 0:128], in1=rec[:, :], op=ALU.mult),
                 reads=[("po", 0), ("rec", 0)], writes=[("oTh", 0)])

        load_w(wb, 16, 0)
        wi = 0
        for h in range(8):
            chunks = [16 + h, 24 + h, 32 + h]
            nxt = chunks[1:] + ([16 + h + 1] if h < 7 else [])
            k.dma("sp", bt[:, :, :], W["btab"][h].rearrange("p (c n) -> p c n", c=5), writes=[("bt", 0)])
            sq_, wi = wi % 3, wi + 1
            load_w(wb, nxt[0], wi % 3)
            for i, (c0, n) in enumerate(OWN_BLOCKS):
                qk_block(sq_, c0, n, 0, qT[:, i * 512:i * 512 + n], ("qT", 0), c0)
            if not last:
                qk_block(sq_, NE, NCX, 0, qT[:, NO:NQ], ("qT", 0), None)
            sk_, wi = wi % 3, wi + 1
            load_w(wb, nxt[1], wi % 3)
            for (c0, n) in EXT_BLOCKS:
                qk_block(sk_, c0, n, 1, kT[:, c0:c0 + n], ("kT", 0), c0)
            qk_block(sk_, NE, NCX, 1, kT[:, NE:NT], ("kT", 0), None)
            sv_, wi = wi % 3, wi + 1
            if len(nxt) > 2:
                load_w(wb, nxt[2], wi % 3)
            for g0 in range(0, NT // 128, 4):
                gn = min(4, NT // 128 - g0)
                pb = pji[0] % 2
                pji[0] += 1

                def vmm(e):
                    for t in range(gn):
                        col = (g0 + t) * 128
                        for kc in range(16):
                            ins = e.matmul(pj[pb][:, t * 128:(t + 1) * 128], hT[:, kc, col:col + 128], wb[sv_][:, kc, :],
                                           start=(kc == 0), stop=(kc == 15))
                    return ins
                k.op("pe", vmm, reads=[("wb", sv_)] + [("hT", g0 + t) for t in range(gn)], writes=[("pj", pb)])
                k.op("act", lambda e: e.copy(out=Vh[:, g0:g0 + gn, :], in_=pj[pb][:, :gn * 128].rearrange("p (t d) -> p t d", d=128)),
                     reads=[("pj", pb)], writes=[("Vh", 0)])
            ai = 0
            for qt in range(2, 2 + NO // 128):
                slots = [(o, qt - 3 + o) for o in range(7) if 0 <= qt - 3 + o <= NKT - 1]
                o_lo, o_hi = slots[0][0], slots[-1][0] + 1
                attend((qt - 2) * 128, slots + [(7, CT_A), (8, CT_B)], cfg["cls"].get(qt, 2), o_lo, o_hi, ai % 2)
                ai += 1
            if not last:
                for t in range(2):
                    attend(NO + t * 128, [(7, CT_A), (8, CT_B)], None, 0, 0, ai % 2)
                    ai += 1
            nq = NO if last else NQ
            k.dma("sp", oT_d[8 + h, :, 0:nq], oTh[:, 0:nq], reads=[("oTh", 0)], writes=[("oT_d", 8 + h)])
        ph.end()

        ph = Phase(k)
        cv = [ph.sb([128, 8, 512], F32) for _ in range(2)]
        cq = ph.sb([128, 8, 512], F32)
        mean = ph.sb([128, 512], F32)
        var = ph.sb([128, 512], F32)
        d1 = [ph.sb([128, 512], F32) for _ in range(2)]
        ob = [ph.sb([128, 8, 512], BF16) for _ in range(2)]
        p1 = ph.ps([128, 512], F32)
        p2 = ph.ps([128, 512], F32)
        lnblocks = [(i * 512, 512) for i in range(NO // 512)] + ([] if last else [(NO, NCX)])

        def load_cv(i):
            c0, n = lnblocks[i]
            k.dma("sp", cv[i % 2][:, :, :n], conv_d[:, :, c0:c0 + n].rearrange("c p t -> p c t"),
                  reads=[("conv_d", c) for c in range(8)], writes=[("cv", i % 2)])
        load_cv(0)
        for i, (c0, n) in enumerate(lnblocks):
            b = i % 2
            if i + 1 < len(lnblocks):
                load_cv(i + 1)
            k.op("act", lambda e: e.activation(out=cq[:, :, :n], in_=cv[b][:, :, :n], func=AF.Square),
                 reads=[("cv", b)], writes=[("cq", 0)])

            def s1(e):
                for c in range(8):
                    ins = e.matmul(p1[:, :n], ones_f[:, :], cv[b][:, c, :n], start=(c == 0), stop=(c == 7))
                return ins

            def s2(e):
                for c in range(8):
                    ins = e.matmul(p2[:, :n], ones_f[:, :], cq[:, c, :n], start=(c == 0), stop=(c == 7))
                return ins
            k.op("pe", s1, reads=[("cv", b)] + CONST, writes=[("p1", 0)])
            k.op("pe", s2, reads=[("cq", 0)] + CONST, writes=[("p2", 0)])
            k.op("dve", lambda e: e.tensor_scalar(out=mean[:, :n], in0=p1[:, :n], scalar1=1.0 / 1024, scalar2=None, op0=ALU.mult),
                 reads=[("p1", 0)], writes=[("mean", 0)])
            k.op("dve", lambda e: e.tensor_tensor(out=var[:, :n], in0=mean[:, :n], in1=mean[:, :n], op=ALU.mult),
                 reads=[("mean", 0)], writes=[("var", 0)])
            k.op("dve", lambda e: e.scalar_tensor_tensor(out=var[:, :n], in0=p2[:, :n], scalar=1.0 / 1024, in1=var[:, :n],
                                                         op0=ALU.mult, op1=ALU.subtract),
                 reads=[("p2", 0), ("var", 0)], writes=[("var", 0)])
            k.op("act", lambda e: e.activation(out=var[:, :n], in_=var[:, :n], func=AF.Sqrt, bias=eps_s[:, 0:1], scale=1.0),
                 reads=[("var", 0)] + CONST, writes=[("var", 0)])
            k.op("dve", lambda e: e.reciprocal(out=var[:, :n], in_=var[:, :n]), reads=[("var", 0)], writes=[("var", 0)])
            for c in range(8):
                db = c % 2
                eng = "dve" if c % 2 == 0 else "pool"
                k.op(eng, lambda e: e.tensor_tensor(out=d1[db][:, :n], in0=cv[b][:, c, :n], in1=mean[:, :n], op=ALU.subtract),
                     reads=[("cv", b), ("mean", 0)], writes=[("d1", db)])
                k.op(eng, lambda e: e.tensor_tensor(out=d1[db][:, :n], in0=d1[db][:, :n], in1=var[:, :n], op=ALU.mult),
                     reads=[("var", 0), ("d1", db)], writes=[("d1", db)])
                k.op("act", lambda e: e.activation(out=ob[b][:, c, :n], in_=d1[db][:, :n], func=AF.Silu,
                                                   bias=chp_s[:, c, 33:34], scale=chp_s[:, c, 32:33]),
                     reads=[("d1", db)] + LC, writes=[("ob", b)])
            k.dma("sp", oT_d[0:8, :, c0:c0 + n].rearrange("c p t -> p c t"), ob[b][:, :, :n], reads=[("ob", b)],
                  writes=[("oT_d", c) for c in range(8)])
        ph.end()

        ph = Phase(k)
        wo = big[:, 0:16 * D].rearrange("p (kc n) -> p kc n", kc=16)
        rows = [ph.sb([128, D], F32) for _ in range(4)]
        xt = [ph.sb([128, D], F32) for _ in range(2)]
        oTt = [ph.sb([128, 16, 128], BF16) for _ in range(2)]
        xm = ph.sb([128, D], F32)
        h2f = ph.sb([128, D], F32)
        h2b = ph.sb([128, D], BF16)
        h2T = ph.sb([128, 16, 128], F32)
        ss4 = ph.sb([128, 2], F32)
        wr_s = ph.sb([128, 16, 32], F32)
        br_s = ph.sb([128, 32], F32)
        negm = ph.sb([128, 1], F32)
        msk = ph.sb([128, 32], F32)
        ex = ph.sb([128, 32], F32)
        gs = ph.sb([128, 2], F32)
        pw = ph.ps([128, D], F32)
        pt2 = ph.ps([128, 1024], F32)
        pl = ph.ps([128, 32], F32)
        k.dma("pool", wo, W["wout"].rearrange("p (kc n) -> p kc n", kc=16), writes=[("wo", 0)])
        k.dma("sp", wr_s[:, :, :], W["wr"].rearrange("p (kc n) -> p kc n", kc=16), writes=[("wr", 0)])
        k.dma("sp", br_s[:, :], W["br"].broadcast_to([128, 32]), writes=[("wr", 1)])

        def load_rows(r):
            k.dma("sp", rows[0][:, :], bc(modv[r:r + 1, 2 * D:3 * D]), writes=[("row", 0)])
            k.dma("sp", rows[3][:, :], bc(W["gmf"][1:2, :]), writes=[("row", 3)])
            k.dma("sp", rows[1][:, :], bc(modv[r:r + 1, 4 * D:5 * D]), writes=[("row", 1)])
            k.dma("sp", rows[2][:, :], bc(modv[r:r + 1, 3 * D:4 * D]), writes=[("row", 2)])
            k.op("dve", lambda e: e.scalar_tensor_tensor(out=rows[1][:, :], in0=rows[1][:, :], scalar=1.0, in1=rows[3][:, :],
                                                         op0=ALU.add, op1=ALU.mult), reads=[("row", 3)], writes=[("row", 1)])
        tiles4 = [(xe, OWN0 // 128 + t, t * 128, 0) for t in range(NO // 128)]
        if not last:
            tiles4 += [(cx, t, NO + t * 128, 1) for t in range(NCX // 128)]

        def load4(i):
            src, st, col, which = tiles4[i]
            k.dma("sp", xt[i % 2][:, :], src[st * 128:(st + 1) * 128, :], writes=[("xt", i % 2)])
            k.dma("sp", oTt[i % 2][:, :, :], oT_d[:, :, col:col + 128].rearrange("c p t -> p c t"),
                  reads=[("oT_d", c) for c in range(16)], writes=[("oTt", i % 2)])
        load_rows(0)
        load4(0)
        for i, (src, st, col, which) in enumerate(tiles4):
            b = i % 2
            if which == 1 and tiles4[i - 1][3] == 0:
                load_rows(1)
            if i + 1 < len(tiles4):
                load4(i + 1)

            def mo(e):
                for nb in range(4):
                    for kc in range(16):
                        ins = e.matmul(pw[:, nb * 512:(nb + 1) * 512], oTt[b][:, kc, :], wo[:, kc, nb * 512:(nb + 1) * 512],
                                       start=(kc == 0), stop=(kc == 15))
                return ins
            k.op("pe", mo, reads=[("oTt", b), ("wo", 0)], writes=[("pw", 0)])
            k.op("dve", lambda e: e.tensor_tensor(out=xm[:, :], in0=pw[:, :], in1=rows[0][:, :], op=ALU.mult),
                 reads=[("pw", 0), ("row", 0)], writes=[("xm", 0)])
            k.op("pool", lambda e: e.tensor_tensor(out=xm[:, :], in0=xm[:, :], in1=xt[b][:, :], op=ALU.add),
                 reads=[("xt", b), ("xm", 0)], writes=[("xm", 0)])
            k.dma("sp", xmid_d[col:col + 128, :], xm[:, :], reads=[("xm", 0)], writes=[("xmid_d", i)])
            k.op("act", lambda e: e.activation(out=h2f[:, :], in_=xm[:, :], func=AF.Square), reads=[("xm", 0)], writes=[("h2f", 0)])
            k.op("dve", lambda e: e.tensor_reduce(out=ss4[:, 0:1], in_=h2f[:, :], axis=AX.X, op=ALU.add),
                 reads=[("h2f", 0)], writes=[("ss", 0)])
            rstd_ops(ss4[:, 0:1], ss4[:, 1:2], D, [("ss", 0)], [("ss", 0)])
            k.op("dve", lambda e: e.scalar_tensor_tensor(out=h2f[:, :], in0=xm[:, :], scalar=ss4[:, 1:2], in1=rows[1][:, :],
                                                         op0=ALU.mult, op1=ALU.mult),
                 reads=[("xm", 0), ("ss", 0), ("row", 1)], writes=[("h2f", 0)])
            k.op("pool", lambda e: e.tensor_tensor(out=h2f[:, :], in0=h2f[:, :], in1=rows[2][:, :], op=ALU.add),
                 reads=[("h2f", 0), ("row", 2)], writes=[("h2f", 0)])
            k.op("act", lambda e: e.copy(out=h2b[:, :], in_=h2f[:, :]), reads=[("h2f", 0)], writes=[("h2b", 0)])
            k.dma("sp", h2_d[col:col + 128, :], h2b[:, :], reads=[("h2b", 0)], writes=[("h2_d", i)])
            for half in range(2):
                def trf(e):
                    for j in range(8):
                        kc = half * 8 + j
                        ins = e.transpose(pt2[:, j * 128:(j + 1) * 128], h2f[:, kc * 128:(kc + 1) * 128], ident_f[:, :])
                    return ins
                k.op("pe", trf, reads=[("h2f", 0)] + CONST, writes=[("pt2", 0)])
                k.op("act", lambda e: e.copy(out=h2T[:, half * 8:(half + 1) * 8, :], in_=pt2[:, :].rearrange("p (j t) -> p j t", j=8)),
                     reads=[("pt2", 0)], writes=[("h2T", half)])

            def rl(e):
                for kc in range(16):
                    ins = e.matmul(pl[:, :], h2T[:, kc, :], wr_s[:, kc, :], start=(kc == 0), stop=(kc == 15))
                return ins
            k.op("pe", rl, reads=[("h2T", 0), ("h2T", 1), ("wr", 0)], writes=[("pl", 0)])
            lg = lg_all[:, i, :]
            m8 = m8_all[:, i, :]
            k.op("dve", lambda e: e.tensor_tensor(out=lg, in0=pl[:, :], in1=br_s[:, :], op=ALU.add),
                 reads=[("pl", 0), ("wr", 1)], writes=[("lg", i)])
            k.op("dve", lambda e: e.max(out=m8, in_=lg), reads=[("lg", i)], writes=[("m8", i)])
            k.op("dve", lambda e: e.tensor_scalar(out=negm[:, :], in0=m8[:, 0:1], scalar1=-1.0, scalar2=None, op0=ALU.mult),
                 reads=[("m8", i)], writes=[("negm", 0)])
            k.op("dve", lambda e: e.tensor_scalar(out=msk[:, :], in0=lg, scalar1=m8[:, 3:4], scalar2=None, op0=ALU.is_ge),
                 reads=[("lg", i), ("m8", i)], writes=[("msk", 0)])
            k.op("act", lambda e: e.activation(out=ex[:, :], in_=lg, func=AF.Exp, bias=negm[:, 0:1], scale=1.0),
                 reads=[("lg", i), ("negm", 0)], writes=[("ex", 0)])
            k.op("dve", lambda e: e.tensor_tensor(out=ex[:, :], in0=ex[:, :], in1=msk[:, :], op=ALU.mult),
                 reads=[("ex", 0), ("msk", 0)], writes=[("ex", 0)])
            k.op("dve", lambda e: e.tensor_reduce(out=gs[:, 0:1], in_=ex[:, :], axis=AX.X, op=ALU.add),
                 reads=[("ex", 0)], writes=[("gs", 0)])
            k.op("dve", lambda e: e.reciprocal(out=gs[:, 1:2], in_=gs[:, 0:1]), reads=[("gs", 0)], writes=[("gs", 0)])
            k.op("dve", lambda e: e.tensor_scalar(out=G_all[:, i, :], in0=ex[:, :], scalar1=gs[:, 1:2], scalar2=tv_s[:, i:i + 1],
                                                  op0=ALU.mult, op1=ALU.mult),
                 reads=[("ex", 0), ("gs", 0)] + LC, writes=[("G", i)])
        ph.end()
        lay.end()

        ph = Phase(k)
        NS = NTQ * 4
        ce_s = ph.sb([128, 32], F32)
        sel = ph.sb([128, NTQ, 32], F32)
        rank = ph.sb([128, NTQ, 32], F32)
        carry = ph.sb([128, 32], F32)
        okm = ph.sb([128, NTQ, 32], F32)
        oh = ph.sb([128, NTQ, 4, 32], F32)
        pr = ph.sb([128, NTQ, 4, 32], F32)
        dk = ph.sb([128, NS], F32)
        vk = ph.sb([128, NS], F32)
        gk = ph.sb([128, NS], F32)
        du = ph.sb([128, NS], U32)
        gsrc = ph.sb([128, NS, 16], F32)
        zb = ph.sb([128, 8192], BF16)
        h2t = [ph.sb([128, D], BF16) for _ in range(2)]
        pp1 = ph.ps([128, 32], F32)
        pp2 = ph.ps([128, 32], F32)
        k.dma("sp", ce_s[:, :], W["ce"], writes=[("ce", 0)])
        k.op("pool", lambda e: e.memset(zb[:, :], 0.0), writes=[("zb", 0)])
        k.op("pool", lambda e: e.memset(gsrc[:, :, :], 0.0), writes=[("gsrc", 0)])
        k.op("dve", lambda e: e.memset(carry[:, :], 0.0), writes=[("carry", 0)])
        nx = (NEXP * CAP + 128) * D
        xflat = Xd.rearrange("r d -> (r d)")
        step = 128 * 8192
        o = 0
        while o < nx:
            n = min(step, nx - o)
            k.dma("sp", xflat[o:o + n].rearrange("(p f) -> p f", p=128), zb[:, :n // 128], reads=[("zb", 0)], writes=[("Xd", 0)])
            o += n
        gflat = Gd.rearrange("r d -> (r d)")
        ng = (NEXP * CAP + 128) * 16
        og = 0
        while og < ng:
            n_ = min(128 * 4096, ng - og)
            k.dma("sp", gflat[og:og + n_].rearrange("(p f) -> p f", p=128), zb[:, :].bitcast(F32)[:, :n_ // 128],
                  reads=[("zb", 0)], writes=[("Gd", 0)])
            og += n_
        k.dma("sp", YdA[HALF:HALF + 128, :], zb[:, :].bitcast(F32)[:, :D], reads=[("zb", 0)], writes=[("Yd", "t")])
        k.dma("sp", YdB[HALF:HALF + 128, :], zb[:, :].bitcast(F32)[:, :D], reads=[("zb", 0)], writes=[("Yd", "t")])
        k.barrier()
        GA = [("G", i) for i in range(NTQ)]
        k.op("dve", lambda e: e.tensor_scalar(out=sel[:, :, :], in0=G_all[:, 0:NTQ, :], scalar1=0.0, scalar2=None, op0=ALU.is_gt),
             reads=GA, writes=[("sel", 0)])
        for i in range(NTQ):
            k.op("pe", lambda e: e.matmul(pp1[:, :], tri_s[:, :], sel[:, i, :], start=True, stop=True),
                 reads=[("sel", 0)] + CONST, writes=[("pp1", 0)])
            k.op("pe", lambda e: e.matmul(pp2[:, :], ones_f[:, :], sel[:, i, :], start=True, stop=True),
                 reads=[("sel", 0)] + CONST, writes=[("pp2", 0)])
            k.op("dve", lambda e: e.tensor_tensor(out=rank[:, i, :], in0=pp1[:, :], in1=carry[:, :], op=ALU.add),
                 reads=[("pp1", 0), ("carry", 0)], writes=[("rank", i)])
            k.op("dve", lambda e: e.tensor_tensor(out=carry[:, :], in0=pp2[:, :], in1=carry[:, :], op=ALU.add),
                 reads=[("pp2", 0), ("carry", 0)], writes=[("carry", 0)])
        RA = [("rank", i) for i in range(NTQ)]
        k.op("dve", lambda e: e.scalar_tensor_tensor(out=okm[:, :, :], in0=rank[:, :, :], scalar=float(CAP), in1=sel[:, :, :],
                                                     op0=ALU.is_lt, op1=ALU.mult), reads=RA + [("sel", 0)], writes=[("okm", 0)])
        k.op("dve", lambda e: e.tensor_tensor(out=rank[:, :, :], in0=rank[:, :, :],
                                              in1=ce_s[:, :].unsqueeze(1).broadcast_to([128, NTQ, 32]), op=ALU.add),
             reads=RA + [("ce", 0), ("okm", 0)], writes=[("destf", 0)])
        LGA = [("lg", i) for i in range(NTQ)] + [("m8", i) for i in range(NTQ)]
        k.op("dve", lambda e: e.tensor_tensor(out=oh[:, :, :, :],
                                              in0=lg_all[:, 0:NTQ, :].unsqueeze(2).broadcast_to([128, NTQ, 4, 32]),
                                              in1=m8_all[:, 0:NTQ, 0:4].unsqueeze(3).broadcast_to([128, NTQ, 4, 32]),
                                              op=ALU.is_equal), reads=LGA, writes=[("oh", 0)])
        for src3, dst, key in ((rank, dk, "dk"), (okm, vk, "vk"), (G_all[:, 0:NTQ, :], gk, "gk")):
            srcap = src3[:, :, :]
            k.op("dve", lambda e: e.tensor_tensor(out=pr[:, :, :, :], in0=oh[:, :, :, :],
                                                  in1=srcap.unsqueeze(2).broadcast_to([128, NTQ, 4, 32]), op=ALU.mult),
                 reads=[("oh", 0), ("destf", 0), ("okm", 0)] + GA, writes=[("pr", 0)])
            k.op("dve", lambda e: e.tensor_reduce(out=dst[:, :], in_=pr[:, :, :, :].rearrange("p t k e -> p (t k) e"),
                                                  axis=AX.X, op=ALU.add), reads=[("pr", 0)], writes=[(key, 0)])
        k.op("dve", lambda e: e.tensor_scalar(out=dk[:, :], in0=dk[:, :], scalar1=trash_s[:, l:l + 1], scalar2=None, op0=ALU.subtract),
             reads=[("dk", 0)] + CONST, writes=[("dk", 0)])
        k.op("dve", lambda e: e.tensor_tensor(out=dk[:, :], in0=dk[:, :], in1=vk[:, :], op=ALU.mult),
             reads=[("dk", 0), ("vk", 0)], writes=[("dk", 0)])
        k.op("dve", lambda e: e.tensor_scalar(out=dk[:, :], in0=dk[:, :], scalar1=trash_s[:, l:l + 1], scalar2=None, op0=ALU.add),
             reads=[("dk", 0)], writes=[("dk", 0)])
        k.op("dve", lambda e: e.tensor_copy(out=du[:, :], in_=dk[:, :]), reads=[("dk", 0)], writes=[("du", 0)])
        k.op("dve", lambda e: e.tensor_copy(out=gsrc[:, :, 0:1], in_=gk[:, :].unsqueeze(2)), reads=[("gk", 0), ("gsrc", 0)],
             writes=[("gsrc", 0)])
        for i in range(NTQ):
            b = i % 2
            k.dma("sp", h2t[b][:, :], h2_d[i * 128:(i + 1) * 128, :], reads=[("h2_d", i)], writes=[("h2t", b)])
            for kk in range(4):
                s = i * 4 + kk
                k.idma(Xd[:, :], h2t[b][:, :], out_off=IndirectOffsetOnAxis(ap=du[:, s:s + 1], axis=0),
                       reads=[("h2t", b), ("du", 0), ("Xd", 0)], writes=[("Xd", 1)])
                k.idma(Gd[:, :], gsrc[:, s, :], out_off=IndirectOffsetOnAxis(ap=du[:, s:s + 1], axis=0),
                       reads=[("gsrc", 0), ("du", 0), ("Gd", 0)], writes=[("Gd", 1)])
        duA, duB = k_duA[l], k_duB[l]
        av = ph.sb([128, NS], F32)
        tA = ph.sb([128, NS], F32)
        k.op("dve", lambda e: e.tensor_scalar(out=av[:, :], in0=dk[:, :], scalar1=float(HALF), scalar2=None, op0=ALU.is_lt),
             reads=[("dk", 0)], writes=[("av", 0)])
        k.op("dve", lambda e: e.tensor_scalar(out=tA[:, :], in0=dk[:, :], scalar1=trash_s[:, 2 + l:3 + l], scalar2=None, op0=ALU.subtract),
             reads=[("dk", 0)], writes=[("tA", 0)])
        k.op("dve", lambda e: e.tensor_tensor(out=tA[:, :], in0=tA[:, :], in1=av[:, :], op=ALU.mult),
             reads=[("av", 0), ("tA", 0)], writes=[("tA", 0)])
        k.op("dve", lambda e: e.tensor_scalar(out=tA[:, :], in0=tA[:, :], scalar1=trash_s[:, 2 + l:3 + l], scalar2=None, op0=ALU.add),
             reads=[("tA", 0)], writes=[("tA", 0)])
        k.op("dve", lambda e: e.tensor_copy(out=duA[:, :NS], in_=tA[:, :]), reads=[("tA", 0)], writes=[("duk", l)])
        k.op("dve", lambda e: e.tensor_scalar(out=av[:, :], in0=av[:, :], scalar1=-1.0, scalar2=1.0, op0=ALU.mult, op1=ALU.add),
             reads=[("av", 0), ("tA", 0)], writes=[("av", 0)])
        k.op("dve", lambda e: e.tensor_scalar(out=tA[:, :], in0=dk[:, :], scalar1=trash_s[:, l:l + 1], scalar2=None, op0=ALU.subtract),
             reads=[("dk", 0), ("duk", l)], writes=[("tA", 0)])
        k.op("dve", lambda e: e.tensor_tensor(out=tA[:, :], in0=tA[:, :], in1=av[:, :], op=ALU.mult),
             reads=[("av", 0), ("tA", 0)], writes=[("tA", 0)])
        k.op("dve", lambda e: e.tensor_scalar(out=tA[:, :], in0=tA[:, :], scalar1=trash_s[:, 2 + l:3 + l], scalar2=None, op0=ALU.add),
             reads=[("tA", 0)], writes=[("tA", 0)])
        k.op("dve", lambda e: e.tensor_copy(out=duB[:, :NS], in_=tA[:, :]), reads=[("tA", 0)], writes=[("dukB", l)])
        ph.end()

        ph = Phase(k)
        ntl = CAP // 128
        xrow = [ph.sb([128, D], BF16) for _ in range(2)]
        xts = [ph.sb([128, 16, CAP], BF16)]
        act = ph.sb([128, 16, CAP], BF16)
        wg = [ph.sb([128, 16, 256], BF16) for _ in range(3)]
        wd = [ph.sb([128, 16, 512], BF16) for _ in range(2)]
        gt_ = [ph.sb([128, 512], F32) for _ in range(2)]
        st_ = [ph.sb([128, 512], F32) for _ in range(2)]
        lt_ = [ph.sb([128, 512], F32) for _ in range(2)]
        tt_ = [ph.sb([128, 512], F32) for _ in range(2)]
        ys = [ph.sb([128, 512], F32) for _ in range(3)]
        bg_s = [ph.sb([128, 32], F32) for _ in range(2)]
        bd_s = [ph.sb([128, D], F32) for _ in range(2)]
        gw_s = [ph.sb([128, ntl, 16], F32) for _ in range(2)]
        pxt = ph.ps([128, 1024], F32)
        pA = [ph.ps([128, 512], F32) for _ in range(2)]
        pB = [ph.ps([128, 512], F32) for _ in range(2)]
        pD = [ph.ps([128, 512], F32) for _ in range(1)]
        gu_jobs = [(e, j) for e in range(NEXP) for j in range(16)]
        dn_jobs = [(e, nb) for e in range(NEXP) for nb in range(4)]

        def load_wg(i):
            e, j = gu_jobs[i]
            k.dma("pool", wg[i % 3][:, :, :], W["wgu"][e, j].rearrange("p (kc n) -> p kc n", kc=16), writes=[("wg", i % 3)])

        def load_wd(i):
            e, nb = dn_jobs[i]
            k.dma("pool", wd[i % 2][:, :, :], W["wdn"][e].rearrange("p (kc n) -> p kc n", kc=16)[:, :, nb * 512:(nb + 1) * 512],
                  writes=[("wd", i % 2)])

        def load_xt(e):
            eb = e % 2
            for t in range(ntl):
                r0 = e * CAP + t * 128
                xb_ = (e * ntl + t) % 2
                k.dma("sp", xrow[xb_][:, :], Xd[r0:r0 + 128, :], reads=[("Xd", 1)], writes=[("xrow", xb_)])
                pv = pxt[:, :].bitcast(BF16)

                def tr(e_):
                    for kc in range(16):
                        ins = e_.transpose(pv[:, kc * 128:(kc + 1) * 128], xrow[xb_][:, kc * 128:(kc + 1) * 128], ident_b[:, :])
                    return ins
                k.op("pe", tr, reads=[("xrow", xb_)] + CONST, writes=[("pxt", 0)])
                k.op("act", lambda e_: e_.copy(out=xts[0][:, :, t * 128:(t + 1) * 128], in_=pv.rearrange("p (kc t) -> p kc t", kc=16)),
                     reads=[("pxt", 0)], writes=[("xts", 0)])
            k.dma("sp", bg_s[eb][:, :], W["bgu"][e], writes=[("bg", eb)])
            k.dma("sp", bd_s[eb][:, :], W["bdn"][e].broadcast_to([128, D]), writes=[("bd", eb)])
            k.dma("sp", gw_s[eb][:, :, :], Gd[e * CAP:(e + 1) * CAP, :].rearrange("(t p) c -> p t c", p=128),
                  reads=[("Gd", 1)], writes=[("gw", eb)])

        load_wg(0)
        load_wg(1)
        load_wd(0)
        load_xt(0)
        it = 0
        yi = 0
        nblocks = [(a * 128, n * 128) for a, n in split_tiles(ntl, 4)]
        for e in range(NEXP):
            eb = e % 2
            for j in range(16):
                i = e * 16 + j
                if i + 2 < len(gu_jobs):
                    load_wg(i + 2)
                ws = wg[i % 3]
                for (c0, n) in nblocks:
                    pb = it % 2
                    it += 1

                    def mm(e_, ps, off):
                        for kc in range(16):
                            ins = e_.matmul(ps[:, :n], ws[:, kc, off:off + 128], xts[0][:, kc, c0:c0 + n], start=(kc == 0), stop=(kc == 15))
                        return ins
                    k.op("pe", lambda e_: mm(e_, pA[pb], 0), reads=[("wg", i % 3), ("xts", 0)], writes=[("pA", pb)])
                    k.op("pe", lambda e_: mm(e_, pB[pb], 128), reads=[("wg", i % 3), ("xts", 0)], writes=[("pB", pb)])
                    k.op("dve", lambda e_: e_.tensor_scalar(out=gt_[pb][:, :n], in0=pA[pb][:, :n], scalar1=bg_s[eb][:, j:j + 1],
                                                            scalar2=LIM, op0=ALU.add, op1=ALU.min),
                         reads=[("pA", pb), ("bg", eb)], writes=[("g", pb)])
                    k.op("act", lambda e_: e_.activation(out=st_[pb][:, :n], in_=gt_[pb][:, :n], func=AF.Sigmoid, scale=ALPHA),
                         reads=[("g", pb)], writes=[("s", pb)])
                    k.op("dve", lambda e_: e_.tensor_scalar(out=lt_[pb][:, :n], in0=pB[pb][:, :n], scalar1=bg_s[eb][:, 16 + j:17 + j],
                                                            scalar2=LIM, op0=ALU.add, op1=ALU.min),
                         reads=[("pB", pb), ("bg", eb)], writes=[("l", pb)])
                    k.op("pool", lambda e_: e_.tensor_scalar(out=lt_[pb][:, :n], in0=lt_[pb][:, :n], scalar1=-LIM, scalar2=1.0,
                                                             op0=ALU.max, op1=ALU.add), reads=[("l", pb)], writes=[("l", pb)])
                    k.op("pool", lambda e_: e_.tensor_tensor(out=tt_[pb][:, :n], in0=gt_[pb][:, :n], in1=st_[pb][:, :n], op=ALU.mult),
                         reads=[("g", pb), ("s", pb)], writes=[("t", pb)])
                    k.op("dve", lambda e_: e_.tensor_tensor(out=act[:, j, c0:c0 + n], in0=tt_[pb][:, :n], in1=lt_[pb][:, :n], op=ALU.mult),
                         reads=[("t", pb), ("l", pb)], writes=[("act", j)])
            if e + 1 < NEXP:
                load_xt(e + 1)
            for nb in range(4):
                i = e * 4 + nb
                if i + 1 < len(dn_jobs):
                    load_wd(i + 1)
                wds = wd[i % 2]
                for t in range(ntl):
                    yb = yi % 3
                    yi += 1

                    def mmd(e_):
                        for kc in range(16):
                            ins = e_.matmul(pD[0][:, :], act[:, kc, t * 128:(t + 1) * 128], wds[:, kc, :], start=(kc == 0), stop=(kc == 15))
                        return ins
                    k.op("pe", mmd, reads=[("wd", i % 2)] + [("act", j) for j in range(16)], writes=[("pD", 0)])
                    k.op("dve", lambda e_: e_.tensor_tensor(out=ys[yb][:, :], in0=pD[0][:, :], in1=bd_s[eb][:, nb * 512:(nb + 1) * 512],
                                                            op=ALU.add), reads=[("pD", 0), ("bd", eb)], writes=[("ys", yb)])
                    k.op("act", lambda e_: e_.mul(out=ys[yb][:, :], in_=ys[yb][:, :], mul=gw_s[eb][:, t, 0:1]),
                         reads=[("ys", yb), ("gw", eb)], writes=[("ys", yb)])
                    Yt = YdA if e < NEXP // 2 else YdB
                    r0_ = (e % (NEXP // 2)) * CAP + t * 128
                    k.dma("sp", Yt[r0_:r0_ + 128, nb * 512:(nb + 1) * 512], ys[yb][:, :],
                          reads=[("ys", yb)], writes=[("Yd", e)])
        ph.end()

        ph = Phase(k)
        gts = [ph.sb([128, D], F32) for _ in range(2)]
        xb = [ph.sb([128, D], F32) for _ in range(2)]
        yb4 = [[ph.sb([128, D], F32) for _ in range(8)] for _ in range(2)]
        for r in range(2):
            k.dma("sp", gts[r][:, :], bc(modv[r:r + 1, 5 * D:6 * D]), writes=[("gt", r)])

        def loadc(i):
            b = i % 2
            k.dma("sp", xb[b][:, :], xmid_d[i * 128:(i + 1) * 128, :], writes=[("xb", b)])
            for s_ in range(4):
                k.idma(yb4[b][s_][:, :], YdA[:, :], in_off=IndirectOffsetOnAxis(ap=k_duA[l][:, i * 4 + s_:i * 4 + s_ + 1], axis=0),
                       writes=[("yb", b, s_)])
                k.idma(yb4[b][4 + s_][:, :], YdB[:, :], in_off=IndirectOffsetOnAxis(ap=k_duB[l][:, i * 4 + s_:i * 4 + s_ + 1], axis=0),
                       writes=[("yb", b, 4 + s_)])
        loadc(0)
        for i in range(NTQ):
            b = i % 2
            if i + 1 < NTQ:
                loadc(i + 1)
            r = 0 if i < NO // 128 else 1
            for s_ in range(1, 8):
                eng = "dve" if s_ % 2 == 1 else "pool"
                k.op(eng, lambda e: e.tensor_tensor(out=yb4[b][0][:, :], in0=yb4[b][0][:, :], in1=yb4[b][s_][:, :], op=ALU.add),
                     reads=[("yb", b, s_)], writes=[("yb", b, 0)])
            k.op("pool", lambda e: e.tensor_tensor(out=yb4[b][0][:, :], in0=yb4[b][0][:, :], in1=gts[r][:, :], op=ALU.mult),
                 reads=[("gt", r)], writes=[("yb", b, 0)])
            k.op("dve", lambda e: e.tensor_tensor(out=xb[b][:, :], in0=xb[b][:, :], in1=yb4[b][0][:, :], op=ALU.add),
                 reads=[("yb", b, 0)], writes=[("xb", b)])
            if i < NO // 128:
                if last:
                    k.dma("sp", x_dst[i * 128:(i + 1) * 128, :], xb[b][:, :], reads=[("xb", b)], is_output=True)
                else:
                    k.dma("sp", x_dst[i * 128:(i + 1) * 128, :], xb[b][:, :], reads=[("xb", b)], writes=[("xsrc", l + 1)])
            else:
                t = i - NO // 128
                k.dma("sp", cx_dst[t * 128:(t + 1) * 128, :], xb[b][:, :], reads=[("xb", b)], writes=[("xsrc", l + 1)])
        ph.end()

    k_duA = [k.sb([128, MAXT * 4], U32) for _ in range(2)]
    k_duB = [k.sb([128, MAXT * 4], U32) for _ in range(2)]
    emit_layer(0, xe0, cx0, x1_d, cx1_d)
    emit_layer(1, x1_d, cx1_d, x_out, None)
    k.finish()
    k.close()
    return nc


def consts_F():
    idn = np.eye(128, dtype=np.float32)
    Rm = np.zeros((128, 128), np.float32)
    for base in (0, 64):
        for j in range(32):
            Rm[base + j, base + 32 + j] = -1.0
            Rm[base + 32 + j, base + j] = 1.0
    tri = (np.arange(128)[:, None] < np.arange(128)[None, :]).astype(np.float32)
    trash = np.stack([NEXP * LCFG[l]["CAP"] + np.arange(128) for l in range(2)]
                     + [(NEXP // 2) * LCFG[l]["CAP"] + np.arange(128) for l in range(2)], 1).astype(np.float32)
    return dict(idn=idn, rmt=np.ascontiguousarray(Rm.T), tri=tri, trash=trash)

def rope_tables(Rext0, NE):
    t = np.arange(NE)
    row = (Rext0 + t // 64).astype(np.float32)
    col = (t % 64).astype(np.float32)
    inv = (np.float32(10000.0) ** (-(np.arange(32, dtype=np.float32)) / np.float32(32))).astype(np.float32)
    d = np.arange(128)
    pos = np.where((d < 64)[:, None], row[None, :], col[None, :]).astype(np.float32)
    ang = (pos * inv[d % 32][:, None]).astype(np.float32)
    return np.stack([np.cos(ang), np.sin(ang)], 0).astype(np.float32)

def valid_mask(Rext0, NE):
    row = Rext0 + np.arange(NE) // 64
    return ((row >= 0) & (row < 128)).astype(np.float32)[None, :]

def bias_tables(rpb_l, Rext0, qts):
    out = np.empty((8, 128, 5, 7, 128), np.float32)
    kk = np.arange(128)[:, None]
    qq = np.arange(128)[None, :]
    for ci, qt in enumerate(qts):
        r = Rext0 + 2 * qt + qq // 64
        qc = qq % 64
        rs = np.clip(r - 4, 0, 120)
        ws = np.clip(qc - 8, 0, 48)
        for o in range(7):
            kt = qt - 3 + o
            kr = Rext0 + 2 * kt + kk // 64
            kc = kk % 64
            valid = (kr >= 0) & (kr < 128) & (kr >= rs) & (kr < rs + 8) & (kc >= ws) & (kc < ws + 16)
            dr = np.clip(kr - r + 7, 0, 14)
            dc = np.clip(kc - qc, -15, 15) + 15
            out[:, :, ci, o, :] = np.where(valid[None], rpb_l[:, dr, dc], NEGB)
    return out.reshape(8, 128, 5 * 896)

def prep_expert_weights(wgu, wdn, bgu, bdn):
    n = wgu.shape[0]
    a = wgu.reshape(n, 16, 128, 2, 16, 128)
    a = np.ascontiguousarray(a.transpose(0, 4, 2, 1, 3, 5)).reshape(n, 16, 128, 16 * 256)
    b = np.ascontiguousarray(wdn.reshape(n, 16, 128, 2048).transpose(0, 2, 1, 3)).reshape(n, 128, 16 * 2048)
    c = np.ascontiguousarray(bgu.reshape(n, 2, 16, 128).transpose(0, 3, 1, 2)).reshape(n, 128, 32)
    return a, b, c, np.ascontiguousarray(bdn[:, None, :])

def shared_inputs(inp):
    m = consts_F()
    m["w_ada"] = np.ascontiguousarray(inp["w_ada"]); m["b_ada"] = np.ascontiguousarray(inp["b_ada"])
    for l in range(2):
        w_in = inp["w_in"][l]
        m["win%d" % l] = np.ascontiguousarray(w_in.reshape(16, 128, 40, 128).transpose(2, 1, 0, 3)).reshape(40, 128, 2048)
        m["wout%d" % l] = np.ascontiguousarray(inp["w_out"][l].reshape(16, 128, 2048).transpose(1, 0, 2)).reshape(128, 16 * 2048)
        chp = np.empty((128, 8, 34), np.float32)
        chp[:, :, :31] = inp["w_dw"][l].T.reshape(8, 128, 31).transpose(1, 0, 2)
        chp[:, :, 31] = inp["b_dw"][l].reshape(8, 128).T
        chp[:, :, 32] = inp["ln_g"][l].reshape(8, 128).T
        chp[:, :, 33] = inp["ln_b"][l].reshape(8, 128).T
        m["chp%d" % l] = chp.reshape(128, 8 * 34)
        m["gqk%d" % l] = np.ascontiguousarray(np.stack([inp["g_q"][l], inp["g_k"][l]], 1))
        m["gmf%d" % l] = np.ascontiguousarray(np.stack([inp["g_mix"][l], inp["g_ffn"][l]], 0))
        m["wr%d" % l] = np.ascontiguousarray(inp["w_router"][l].reshape(16, 128, 32).transpose(1, 0, 2)).reshape(128, 512)
        m["br%d" % l] = np.ascontiguousarray(inp["b_router"][l][None, :])
        m["ce%d" % l] = np.ascontiguousarray(np.broadcast_to((np.arange(32) * LCFG[l]["CAP"]).astype(np.float32)[None, :], (128, 32)))
        a, b, c, d = prep_expert_weights(inp["w_gate_up"][l], inp["w_down"][l], inp["b_gate_up"][l], inp["b_down"][l])
        m["wgu%d" % l], m["wdn%d" % l], m["bgu%d" % l], m["bdn%d" % l] = a, b, c, d
    return m

def core_inputs(inp, shared, ci):
    b, j = ci // 4, ci % 4
    R0 = 32 * j
    m = dict(shared)
    NE0 = LCFG[0]["NE"]
    lo, hi = (R0 - 8) * 64, (R0 + 40) * 64
    xe = np.zeros((NE0, D), np.float32)
    a, e = max(lo, 0), min(hi, 8192)
    xe[a - lo:e - lo] = inp["x"][b, a:e]
    m["xe0"] = xe
    m["cx0"] = np.ascontiguousarray(inp["ctx"][b])
    m["sT"] = np.ascontiguousarray(np.stack([inp["c"][b], inp["c_ctx"]], 1))
    for l in range(2):
        cfg = LCFG[l]
        Rext0 = R0 - 8 if l == 0 else R0 - 4
        m["cs%d" % l] = rope_tables(Rext0, cfg["NE"])
        m["vmask%d" % l] = valid_mask(Rext0, cfg["NE"])
        qts = sorted(cfg["cls"].keys())
        qts = [qts[0], qts[1], 8, qts[2], qts[3]]
        m["btab%d" % l] = bias_tables(inp["rpb"][l], Rext0, qts)
        tv = np.zeros((128, MAXT), np.float32)
        nlat = cfg["NO"] // 128
        tok = np.arange(nlat * 128)
        row = Rext0 + 4 + tok // 64
        tv[:, :nlat] = ((row >= 0) & (row < 128)).astype(np.float32).reshape(nlat, 128).T
        if l == 0:
            tv[:, nlat:nlat + 2] = 1.0
        m["tv%d" % l] = tv
    return m


def kernel(x, c, ctx, c_ctx, w_ada, b_ada, g_mix, g_ffn, w_in, w_dw, b_dw, ln_g, ln_b, g_q, g_k, rpb,
           w_out, w_router, b_router, w_gate_up, b_gate_up, w_down, b_down):
    f = lambda a: np.asarray(a, dtype=np.float32)
    inp = dict(x=f(x), c=f(c), ctx=f(ctx), c_ctx=f(c_ctx), w_ada=f(w_ada), b_ada=f(b_ada), g_mix=f(g_mix), g_ffn=f(g_ffn),
               w_in=f(w_in), w_dw=f(w_dw), b_dw=f(b_dw), ln_g=f(ln_g), ln_b=f(ln_b), g_q=f(g_q), g_k=f(g_k), rpb=f(rpb),
               w_out=f(w_out), w_router=f(w_router), b_router=f(b_router), w_gate_up=f(w_gate_up), b_gate_up=f(b_gate_up),
               w_down=f(w_down), b_down=f(b_down))
    shared = shared_inputs(inp)
    in_maps = [core_inputs(inp, shared, ci) for ci in range(8)]
    res = run_bass_kernel_spmd(build_fused(), in_maps, core_ids=list(range(8)))
    out = np.concatenate([r["x_out"] for r in res.results], 0).reshape(2, 8192, D)
    return np.ascontiguousarray(out, dtype=np.float32)
```
